# Optimizing a Trainium2 kernel written in Bass

```python
import numpy as np
import jax
import jax.numpy as jnp
from jax import lax

D_MODEL = 1024
BATCH = 16
SEQ = 2048
DEPTH = 2

MLSTM_HEADS = 4
MLSTM_QK_DIM = 64
MLSTM_V_DIM = 128
MLSTM_CHUNK = 64
MLSTM_WIDTH = MLSTM_HEADS * MLSTM_V_DIM
SCONV_WIDTH = D_MODEL - MLSTM_WIDTH
SCONV_TAPS = 3
CONF_WIDTH = D_MODEL
CONF_TAPS = 31
N_EXPERTS = 32
TOP_K = 4
D_EXPERT = D_MODEL
SWIGLU_LIMIT = 7.0
SWIGLU_ALPHA = 1.702
EXPERT_ROWS = 128
LN_EPS = 1e-5
HEAD_NORM_EPS = 1e-6
DEEPNORM_ALPHA = (2 * DEPTH) ** 0.25
DEEPNORM_BETA = (8 * DEPTH) ** -0.25
N_EVEN = (DEPTH + 1) // 2
N_ODD = DEPTH // 2
AB_SPLIT_SIZES = (MLSTM_HEADS * MLSTM_QK_DIM, MLSTM_HEADS * MLSTM_QK_DIM, MLSTM_WIDTH, MLSTM_WIDTH,
                  MLSTM_HEADS, MLSTM_HEADS, SCONV_WIDTH, SCONV_WIDTH, SCONV_WIDTH)
AB_IN_WIDTH = sum(AB_SPLIT_SIZES)

kernel_name = 'hybrid_mlstm_shortconv_conformer_moe_deepnorm'


def layer_norm(x, g, b, eps=LN_EPS):
    xf = x.astype(jnp.float32)
    mu = jnp.mean(xf, axis=-1, keepdims=True)
    var = jnp.mean(jnp.square(xf - mu), axis=-1, keepdims=True)
    y = (xf - mu) * lax.rsqrt(var + eps) * g.astype(jnp.float32) + b.astype(jnp.float32)
    return y.astype(x.dtype)


def causal_depthwise_conv(u, w):
    taps, ch = w.shape
    return lax.conv_general_dilated(u, w[:, None, :].astype(u.dtype), window_strides=(1,),
                                    padding=[(taps - 1, 0)],
                                    dimension_numbers=('NWC', 'WIO', 'NWC'),
                                    feature_group_count=ch)


def mlstm_chunkwise(q, k, v, i_pre, f_pre):
    Bn, S, H, dk = q.shape
    dv = v.shape[-1]
    L = MLSTM_CHUNK
    NC = S // L
    q = q.reshape(Bn, NC, L, H, dk).transpose(0, 3, 1, 2, 4)
    k = k.reshape(Bn, NC, L, H, dk).transpose(0, 3, 1, 2, 4) * (dk ** -0.5)
    v = v.reshape(Bn, NC, L, H, dv).transpose(0, 3, 1, 2, 4)
    ig = i_pre.reshape(Bn, NC, L, H).transpose(0, 3, 1, 2)
    logf = jax.nn.log_sigmoid(f_pre).reshape(Bn, NC, L, H).transpose(0, 3, 1, 2)
    b = jnp.cumsum(logf, axis=-1)
    g = b[..., -1]
    a = g[..., None] - b + ig

    def step(carry, inp):
        C, n, m = carry
        k_c, v_c, a_c, g_c = inp
        m_new = jnp.maximum(g_c + m, jnp.max(a_c, axis=-1))
        decay = jnp.exp(g_c + m - m_new)
        w = jnp.exp(a_c - m_new[..., None])
        C_new = decay[..., None, None] * C + jnp.einsum('bhl,bhlk,bhlv->bhkv', w, k_c, v_c)
        n_new = decay[..., None] * n + jnp.einsum('bhl,bhlk->bhk', w, k_c)
        return (C_new, n_new, m_new), (C, n, m)

    init = (jnp.zeros((Bn, H, dk, dv), jnp.float32), jnp.zeros((Bn, H, dk), jnp.float32),
            jnp.zeros((Bn, H), jnp.float32))
    xs = (k.transpose(2, 0, 1, 3, 4), v.transpose(2, 0, 1, 3, 4), a.transpose(2, 0, 1, 3), g.transpose(2, 0, 1))
    _, (C_prev, n_prev, m_prev) = lax.scan(step, init, xs)
    C_prev = C_prev.transpose(1, 2, 0, 3, 4)
    n_prev = n_prev.transpose(1, 2, 0, 3)
    m_prev = m_prev.transpose(1, 2, 0)

    causal = jnp.tril(jnp.ones((L, L), dtype=bool))
    dmat = b[..., :, None] - b[..., None, :] + ig[..., None, :]
    dmat = jnp.where(causal, dmat, -jnp.inf)
    e_inter = b + m_prev[..., None]
    m_t = jnp.maximum(e_inter, jnp.max(dmat, axis=-1))
    w_intra = jnp.exp(dmat - m_t[..., None])
    w_inter = jnp.exp(e_inter - m_t)
    s = jnp.einsum('bhctk,bhcsk->bhcts', q, k) * w_intra
    num = w_inter[..., None] * jnp.einsum('bhctk,bhckv->bhctv', q, C_prev) + jnp.einsum('bhcts,bhcsv->bhctv', s, v)
    den = w_inter * jnp.einsum('bhctk,bhck->bhct', q, n_prev) + jnp.sum(s, axis=-1)
    h = num / jnp.maximum(jnp.abs(den), jnp.exp(-m_t))[..., None]
    return h.transpose(0, 2, 3, 1, 4).reshape(Bn, S, H, dv)


def mixer_ab(x, w_in, b_igate, b_fgate, conv_w, head_gain, w_out):
    Bn, S, _ = x.shape
    H, dk, dv = MLSTM_HEADS, MLSTM_QK_DIM, MLSTM_V_DIM
    f32 = jnp.float32
    z = x @ w_in
    cuts = np.cumsum(AB_SPLIT_SIZES)[:-1].tolist()
    q, k, v, o_pre, i_pre, f_pre, sb, sc, sx = jnp.split(z, cuts, axis=-1)
    h = mlstm_chunkwise(q.reshape(Bn, S, H, dk).astype(f32), k.reshape(Bn, S, H, dk).astype(f32),
                        v.reshape(Bn, S, H, dv).astype(f32), (i_pre + b_igate).astype(f32),
                        (f_pre + b_fgate).astype(f32))
    mu = jnp.mean(h, axis=-1, keepdims=True)
    var = jnp.mean(jnp.square(h - mu), axis=-1, keepdims=True)
    h = (h - mu) * lax.rsqrt(var + HEAD_NORM_EPS) * head_gain.reshape(H, dv).astype(f32)
    h = (jax.nn.sigmoid(o_pre.astype(f32)) * h.reshape(Bn, S, MLSTM_WIDTH)).astype(x.dtype)
    y = sb * causal_depthwise_conv(sc * sx, conv_w)
    return jnp.concatenate([h, y], axis=-1) @ w_out


def conformer_conv(x, w_pw1, w_dw, b_dw, ln_g, ln_b, w_pw2):
    a, gate = jnp.split(x @ w_pw1, 2, axis=-1)
    u = a * jax.nn.sigmoid(gate)
    u = causal_depthwise_conv(u, w_dw) + b_dw
    u = jax.nn.silu(layer_norm(u, ln_g, ln_b))
    return u @ w_pw2


def moe(x, router_w, router_b, w_up, b_up, w_down, b_down):
    Bn, S, D = x.shape
    T = Bn * S
    E, K, R = N_EXPERTS, TOP_K, EXPERT_ROWS
    xt = x.reshape(T, D)
    logits = (xt @ router_w + router_b).astype(jnp.float32)
    top_vals, top_idx = lax.top_k(logits, K)
    gates = jax.nn.softmax(top_vals, axis=-1)
    A = T * K
    e_flat = top_idx.reshape(A).astype(jnp.int32)
    tok_flat = jnp.repeat(jnp.arange(T, dtype=jnp.int32), K)
    g_flat = gates.reshape(A)
    order = jnp.argsort(e_flat)
    e_sorted = e_flat[order]
    counts = jnp.bincount(e_flat, length=E).astype(jnp.int32)
    starts = jnp.cumsum(counts) - counts
    padded = ((counts + R - 1) // R) * R
    pends = jnp.cumsum(padded)
    pstarts = pends - padded
    dest = pstarts[e_sorted] + (jnp.arange(A, dtype=jnp.int32) - starts[e_sorted])
    P = -(-(A + E * (R - 1)) // R) * R
    nb = P // R
    row_tok = jnp.full((P,), T, jnp.int32).at[dest].set(tok_flat[order])
    row_gate = jnp.zeros((P,), x.dtype).at[dest].set(g_flat[order].astype(x.dtype))
    group_expert = jnp.minimum(jnp.searchsorted(pends, jnp.arange(nb, dtype=jnp.int32) * R, side='right'),
                               E - 1).astype(jnp.int32)
    x_pad = jnp.concatenate([xt, jnp.zeros((1, D), xt.dtype)], axis=0)
    xs = x_pad[row_tok].reshape(nb, R, D)

    def expert_rows(args):
        xb, e = args
        hb = xb @ w_up[e] + b_up[e]
        glu, lin = jnp.split(hb, 2, axis=-1)
        glu = jnp.minimum(glu, SWIGLU_LIMIT)
        lin = jnp.clip(lin, -SWIGLU_LIMIT, SWIGLU_LIMIT)
        act = glu * jax.nn.sigmoid(SWIGLU_ALPHA * glu) * (lin + 1.0)
        return act @ w_down[e] + b_down[e]

    ys = lax.map(expert_rows, (xs, group_expert)).reshape(P, D)
    out = jnp.zeros((T + 1, D), x.dtype).at[row_tok].add(ys * row_gate[:, None])[:T]
    return out.reshape(Bn, S, D)


def setup_inputs(seed: int = 0) -> dict:
    key = jax.random.key(seed)
    ks = jax.random.split(key, 24)
    f32 = jnp.float32

    def nrm(k, shape, scale):
        return jax.random.normal(k, shape, f32) * scale

    D, H, E, F = D_MODEL, MLSTM_HEADS, N_EXPERTS, D_EXPERT
    beta = DEEPNORM_BETA
    x = nrm(ks[0], (BATCH, SEQ, D), 1.0)
    col_scale = jnp.concatenate([
        jnp.ones((2 * H * MLSTM_QK_DIM,), f32), jnp.full((MLSTM_WIDTH,), beta, f32),
        jnp.ones((MLSTM_WIDTH + 2 * H + 2 * SCONV_WIDTH,), f32), jnp.full((SCONV_WIDTH,), beta, f32)])
    ab_w_in = nrm(ks[1], (N_EVEN, D, AB_IN_WIDTH), D ** -0.5) * col_scale
    ab_b_igate = nrm(ks[2], (N_EVEN, H), 0.1)
    ab_b_fgate = jnp.linspace(3.0, 6.0, H, dtype=f32) + nrm(ks[3], (N_EVEN, H), 0.1)
    ab_conv_w = nrm(ks[4], (N_EVEN, SCONV_TAPS, SCONV_WIDTH), SCONV_TAPS ** -0.5)
    ab_head_gain = 1.0 + nrm(ks[5], (N_EVEN, MLSTM_WIDTH), 0.02)
    ab_w_out = nrm(ks[6], (N_EVEN, MLSTM_WIDTH + SCONV_WIDTH, D), (MLSTM_WIDTH + SCONV_WIDTH) ** -0.5 * beta)
    glu_scale = jnp.concatenate([jnp.full((CONF_WIDTH,), beta, f32), jnp.ones((CONF_WIDTH,), f32)])
    cf_w_pw1 = nrm(ks[7], (N_ODD, D, 2 * CONF_WIDTH), D ** -0.5) * glu_scale
    cf_w_dw = nrm(ks[8], (N_ODD, CONF_TAPS, CONF_WIDTH), CONF_TAPS ** -0.5)
    cf_b_dw = nrm(ks[9], (N_ODD, CONF_WIDTH), 0.02)
    cf_ln_g = 1.0 + nrm(ks[10], (N_ODD, CONF_WIDTH), 0.02)
    cf_ln_b = nrm(ks[11], (N_ODD, CONF_WIDTH), 0.02)
    cf_w_pw2 = nrm(ks[12], (N_ODD, CONF_WIDTH, D), CONF_WIDTH ** -0.5 * beta)
    router_w = nrm(ks[13], (DEPTH, D, E), D ** -0.5)
    router_b = nrm(ks[14], (DEPTH, E), 0.01)
    exp_w_up = nrm(ks[15], (DEPTH, E, D, 2 * F), D ** -0.5 * beta)
    exp_b_up = nrm(ks[16], (DEPTH, E, 2 * F), 0.02)
    exp_w_down = nrm(ks[17], (DEPTH, E, F, D), F ** -0.5 * beta)
    exp_b_down = nrm(ks[18], (DEPTH, E, D), 0.02)
    post_ln_g = 1.0 + nrm(ks[19], (DEPTH, 2, D), 0.02)
    post_ln_b = nrm(ks[20], (DEPTH, 2, D), 0.02)
    return {'x': x, 'ab_w_in': ab_w_in, 'ab_b_igate': ab_b_igate, 'ab_b_fgate': ab_b_fgate,
            'ab_conv_w': ab_conv_w, 'ab_head_gain': ab_head_gain, 'ab_w_out': ab_w_out,
            'cf_w_pw1': cf_w_pw1, 'cf_w_dw': cf_w_dw, 'cf_b_dw': cf_b_dw, 'cf_ln_g': cf_ln_g,
            'cf_ln_b': cf_ln_b, 'cf_w_pw2': cf_w_pw2, 'router_w': router_w, 'router_b': router_b,
            'exp_w_up': exp_w_up, 'exp_b_up': exp_b_up, 'exp_w_down': exp_w_down, 'exp_b_down': exp_b_down,
            'post_ln_g': post_ln_g, 'post_ln_b': post_ln_b}


def reference(x, ab_w_in, ab_b_igate, ab_b_fgate, ab_conv_w, ab_head_gain, ab_w_out,
              cf_w_pw1, cf_w_dw, cf_b_dw, cf_ln_g, cf_ln_b, cf_w_pw2,
              router_w, router_b, exp_w_up, exp_b_up, exp_w_down, exp_b_down,
              post_ln_g, post_ln_b):
    for layer in range(DEPTH):
        j = layer // 2
        if layer % 2 == 0:
            h = mixer_ab(x, ab_w_in[j], ab_b_igate[j], ab_b_fgate[j], ab_conv_w[j], ab_head_gain[j], ab_w_out[j])
        else:
            h = conformer_conv(x, cf_w_pw1[j], cf_w_dw[j], cf_b_dw[j], cf_ln_g[j], cf_ln_b[j], cf_w_pw2[j])
        x = layer_norm(DEEPNORM_ALPHA * x + h, post_ln_g[layer, 0], post_ln_b[layer, 0])
        f = moe(x, router_w[layer], router_b[layer], exp_w_up[layer], exp_b_up[layer],
                exp_w_down[layer], exp_b_down[layer])
        x = layer_norm(DEEPNORM_ALPHA * x + f, post_ln_g[layer, 1], post_ln_b[layer, 1])
    return x
```

```python
import contextlib
import math
import os
import numpy as np
import concourse.bass as bass
import concourse.mybir as mybir
from concourse.bass_utils import run_bass_kernel_spmd
from concourse.alu_op_type import AluOpType as ALU

F32 = mybir.dt.float32
BF16 = mybir.dt.bfloat16
I32 = mybir.dt.int32
U32 = mybir.dt.uint32
AF = mybir.ActivationFunctionType

D = 1024
NTOK = 4096
NTILE = 32
SEQ = 2048
NE = 32
CAP = 640
NTC = CAP // 128
NSLOT = NE * CAP
ALPHA = 4.0 ** 0.25
LN_EPS = 1e-5
HN_EPS = 1e-6


class Buf:
    __slots__ = ("name", "writer", "readers", "sem", "dcount")

    def __init__(self, name):
        self.name = name
        self.writer = None
        self.readers = []
        self.sem = None
        self.dcount = 0


class T:
    __slots__ = ("t", "b")

    def __init__(self, t, b):
        self.t = t
        self.b = b


class Sched:
    def __init__(self, nc):
        self.nc = nc
        self.engs = {"pe": nc.tensor, "act": nc.scalar, "dve": nc.vector, "pool": nc.gpsimd, "sp": nc.sync}
        self.sems = {}
        self.count = {k: 0 for k in self.engs}
        self.seen = {k: {} for k in self.engs}
        self._ctx = []
        self.nsem = 0
        self.dma_bufs = []
        for k in self.engs:
            self.sems[k] = self.new_sem("s_" + k)

    def new_sem(self, name):
        cm = self.nc.semaphore("%s_%d" % (name, self.nsem))
        s = cm.__enter__()
        self._ctx.append(cm)
        self.nsem += 1
        return s

    def close(self):
        for cm in reversed(self._ctx):
            cm.__exit__(None, None, None)

    def _wait(self, eng, tok):
        if tok is None:
            return
        sem, val, pe = tok
        key = id(sem)
        if self.seen[eng].get(key, 0) >= val:
            return
        if pe == "pe" and eng == "pe":
            return
        self.engs[eng].wait_ge(sem, val)
        self.seen[eng][key] = val

    def _deps(self, eng, reads, writes, is_dma=False, after=()):
        toks = list(after)
        for b in reads:
            toks.append(b.writer)
        for b in writes:
            if b.writer is not None and not (is_dma and b.writer[2] == "dma"):
                toks.append(b.writer)
            toks.extend(b.readers)
        best = {}
        for t in toks:
            if t is None:
                continue
            k = id(t[0])
            if k not in best or best[k][1] < t[1]:
                best[k] = t
        for t in best.values():
            self._wait(eng, t)

    def _commit(self, tok, reads, writes):
        for b in writes:
            b.writer = tok
            b.readers = []
        for b in reads:
            b.readers.append(tok)
            if len(b.readers) > 16:
                last = {}
                for r in b.readers:
                    last[id(r[0])] = r
                b.readers = list(last.values())

    def op(self, eng, fn, reads=(), writes=(), after=(), sig=True):
        self._deps(eng, reads, writes, after=after)
        inst = fn(self.engs[eng])
        if not sig:
            return None
        self.count[eng] += 1
        inst.then_inc(self.sems[eng], 1)
        tok = (self.sems[eng], self.count[eng], eng)
        self._commit(tok, reads, writes)
        return tok

    def dma(self, q, fn, reads=(), writes=(), after=(), sembuf=None):
        sb = sembuf if sembuf is not None else writes[0]
        if sb.sem is None:
            sb.sem = self.new_sem("d_" + sb.name)
            self.dma_bufs.append(sb)
        self._deps(q, reads, writes, is_dma=True, after=after)
        inst = fn(self.engs[q])
        sb.dcount += 16
        inst.then_inc(sb.sem, 16)
        tok = (sb.sem, sb.dcount, "dma")
        self._commit(tok, reads, writes)
        return tok

    def barrier(self, engines=None):
        for e in (engines or self.engs):
            for k in self.engs:
                if k != e and self.count[k] > 0:
                    self._wait(e, (self.sems[k], self.count[k], k))
            for b in self.dma_bufs:
                if b.dcount > 0:
                    self._wait(e, (b.sem, b.dcount, "dma"))


class Ctx:
    pass


def build_program(stages=("ab", "moe0", "cf", "moe1"), debug=False):
    nc = bass.Bass("TRN2", target_bir_lowering=False)
    S = Sched(nc)
    G = Ctx()
    es_top = contextlib.ExitStack()

    def din(name, shape, dt=F32):
        return nc.dram_tensor(name, list(shape), dt, kind="ExternalInput").ap()

    def dscratch(name, shape, dt=F32, out=False):
        kind = "ExternalOutput" if out else "Internal"
        return nc.dram_tensor(name, list(shape), dt, kind=kind).ap()

    first = stages[0]
    x0 = din("x", [NTOK, D]) if first == "ab" else None
    W = {}
    if "ab" in stages:
        W["w_in"] = din("ab_w_in", [D, 3080])
        W["w_out"] = din("ab_w_out", [D, D])
        W["ab_bif"] = din("ab_bif", [8])
        W["ab_conv"] = din("ab_conv", [128, 12])
        W["ab_hg"] = din("ab_hg", [512])
    if "cf" in stages:
        W["pw1"] = din("cf_w_pw1", [D, 2048])
        W["pw2"] = din("cf_w_pw2", [D, D])
        W["cf_dw"] = din("cf_dw", [128, 31 * 8])
        W["cf_vec"] = din("cf_vec", [128, 24])
    W["router_w"] = din("router_w", [2, D, NE])
    W["router_b"] = din("router_b", [2, NE])
    W["up"] = din("exp_w_up", [2, NE, D, 2 * D])
    W["dn"] = din("exp_w_down", [2, NE, D, D])
    W["bup"] = din("bup", [128, 2 * NE * 16])
    W["bdn"] = din("exp_b_down", [2, NE, D])
    W["lng"] = din("post_ln_g", [2, 2, D])
    W["lnb"] = din("post_ln_b", [2, 2, D])

    last = stages[-1]
    xs = {}
    names = {"ab": "x1", "moe0": "x2", "cf": "x3", "moe1": "out"}
    for st in ("ab", "moe0", "cf", "moe1"):
        nm = names[st]
        if st in stages:
            xs[nm] = T(dscratch(nm, [NTOK, D], F32, out=(debug or st == last)), Buf(nm))
    prev = {"moe0": "x1", "cf": "x2", "moe1": "x3", "route0": "x1", "route1": "x3"}
    if first != "ab":
        nm = prev[first]
        xs[nm] = T(din(nm, [NTOK, D]), Buf(nm))
    else:
        xs["x0"] = T(x0, Buf("x0"))
    xg = T(dscratch("xg", [NSLOT, D], BF16, out=debug), Buf("xg"))
    yg = T(dscratch("yg", [NSLOT, D], F32, out=debug), Buf("yg"))

    uid = [0]

    def sbt(es, name, shape, dt):
        uid[0] += 1
        name = "sb%d_%s" % (uid[0], name)
        return T(es.enter_context(nc.sbuf_tensor(name, list(shape), dt)), Buf(name))

    def pst(es, name, shape, dt):
        uid[0] += 1
        name = "ps%d_%s" % (uid[0], name)
        return T(es.enter_context(nc.psum_tensor(name, list(shape), dt)), Buf(name))

    ident_f = sbt(es_top, "ident_f", [128, 128], F32)
    ident_b = sbt(es_top, "ident_b", [128, 128], BF16)
    uincl_f = sbt(es_top, "uincl_f", [128, 128], F32)
    ustr_b = sbt(es_top, "ustr_b", [128, 128], BF16)
    ones_f = sbt(es_top, "ones_f", [128, 128], F32)
    ones_b = sbt(es_top, "ones_b", [128, 128], BF16)
    iota_e = sbt(es_top, "iota_e", [128, NE], F32)
    ecap = sbt(es_top, "ecap", [128, NE], F32)
    dest_i = sbt(es_top, "dest_i", [128, NTILE * 4], I32)
    gate_t = sbt(es_top, "gate_t", [128, NTILE * 4], F32)
    dest_b = [Buf("dest%d" % i) for i in range(NTILE)]
    gate_b = [Buf("gate%d" % i) for i in range(NTILE)]
    cbase = sbt(es_top, "cbase", [128, NE], F32)
    zero_b = sbt(es_top, "zero_b", [128, 1024], BF16)
    tmpc = sbt(es_top, "tmpc", [128, 128], F32)

    def consts():
        S.op("pool", lambda e: e.memset(ones_f.t[:], 1.0), writes=[ones_f.b])
        S.op("pool", lambda e: e.memset(ones_b.t[:], 1.0), writes=[ones_b.b])
        S.op("pool", lambda e: e.memset(zero_b.t[:], 0.0), writes=[zero_b.b])
        S.op("pool", lambda e: e.affine_select(out=ident_f.t[:], in_=ones_f.t[:], pattern=[[-1, 128]],
                                                compare_op=ALU.is_equal, fill=0.0, base=0, channel_multiplier=1),
             reads=[ones_f.b], writes=[ident_f.b])
        S.op("dve", lambda e: e.tensor_copy(out=ident_b.t[:], in_=ident_f.t[:]), reads=[ident_f.b], writes=[ident_b.b])
        S.op("pool", lambda e: e.affine_select(out=uincl_f.t[:], in_=ones_f.t[:], pattern=[[1, 128]],
                                                compare_op=ALU.is_ge, fill=0.0, base=0, channel_multiplier=-1),
             reads=[ones_f.b], writes=[uincl_f.b])
        S.op("pool", lambda e: e.affine_select(out=tmpc.t[:], in_=ones_f.t[:], pattern=[[1, 128]],
                                                compare_op=ALU.is_gt, fill=0.0, base=0, channel_multiplier=-1),
             reads=[ones_f.b], writes=[tmpc.b])
        S.op("dve", lambda e: e.tensor_copy(out=ustr_b.t[:], in_=tmpc.t[:]), reads=[tmpc.b], writes=[ustr_b.b])
        S.op("pool", lambda e: e.iota(iota_e.t[:], pattern=[[1, NE]], base=0, channel_multiplier=0,
                                      allow_small_or_imprecise_dtypes=True), writes=[iota_e.b])
        S.op("dve", lambda e: e.tensor_scalar(out=ecap.t[:], in0=iota_e.t[:], scalar1=float(CAP), scalar2=None,
                                              op0=ALU.mult), reads=[iota_e.b], writes=[ecap.b])

    consts()
    breg = nc.gpsimd.to_reg(NSLOT - 1)
    zf = None
    for i in range(NSLOT // 128):
        zf = S.dma("sp", lambda e, i=i: e.dma_start(
            out=xg.t[i * 128:(i + 1) * 128, :], in_=zero_b.t[:]),
            reads=[zero_b.b], writes=[xg.b])
    G.xg_zero_tok = zf

    def make_epi(es, layer, which, nbuf=2):
        E = Ctx()
        E.lng = sbt(es, "lng%d%d" % (layer, which), [128, D], F32)
        E.lnb = sbt(es, "lnb%d%d" % (layer, which), [128, D], F32)
        S.dma("sp", lambda e: e.dma_start(out=E.lng.t[:], in_=W["lng"][layer, which].partition_broadcast(128)), writes=[E.lng.b])
        S.dma("sp", lambda e: e.dma_start(out=E.lnb.t[:], in_=W["lnb"][layer, which].partition_broadcast(128)), writes=[E.lnb.b])
        E.xt = [sbt(es, "epx%d%d_%d" % (layer, which, i), [128, D], F32) for i in range(nbuf)]
        E.r = [sbt(es, "epr%d%d_%d" % (layer, which, i), [128, D], F32) for i in range(nbuf)]
        E.nbuf = nbuf
        E.sts = [sbt(es, "epst%d%d" % (layer, which), [128, 12], F32) for i in range(nbuf)]
        E.mvs = [sbt(es, "epmv%d%d" % (layer, which), [128, 2], F32) for i in range(nbuf)]
        E.eps = sbt(es, "epeps%d%d" % (layer, which), [128, 1], F32)
        S.op("dve", lambda e: e.memset(E.eps.t[:], LN_EPS), writes=[E.eps.b])
        return E

    def epi_load(E, i, xin, slot=None):
        sl = (i if slot is None else slot) % E.nbuf
        xt = E.xt[sl]
        S.dma("sp", lambda e: e.dma_start(out=xt.t[:], in_=xin.t[i * 128:(i + 1) * 128, :]), reads=[xin.b], writes=[xt.b])

    def epilogue(E, i, xin, xout, add_fn, slot=None, preloaded=False):
        sl = (i if slot is None else slot) % E.nbuf
        xt = E.xt[sl]
        r = E.r[sl]
        est, emv = E.sts[sl], E.mvs[sl]
        if not preloaded:
            S.dma("sp", lambda e: e.dma_start(out=xt.t[:], in_=xin.t[i * 128:(i + 1) * 128, :]), reads=[xin.b], writes=[xt.b])
        src = add_fn(r, xt)
        if src is None:
            src, h0, h1, flat = r, r.t[:, 0:512], r.t[:, 512:1024], r.t[:]
        else:
            h0, h1, flat = src.t[:, 0, :], src.t[:, 1, :], src.t[:].rearrange("p a b -> p (a b)")
        S.op("dve", lambda e: e.bn_stats(out=est.t[:, 0:6], in_=h0), reads=[src.b], writes=[est.b])
        S.op("dve", lambda e: e.bn_stats(out=est.t[:, 6:12], in_=h1), reads=[src.b], writes=[est.b])
        S.op("dve", lambda e: e.bn_aggr(out=emv.t[:], in_=est.t[:]), reads=[est.b], writes=[emv.b])
        S.op("act", lambda e: e.activation(out=emv.t[:, 1:2], in_=emv.t[:, 1:2], func=AF.Ln, bias=E.eps.t[:, 0:1], scale=1.0),
             reads=[emv.b, E.eps.b], writes=[emv.b])
        S.op("act", lambda e: e.activation(out=emv.t[:, 1:2], in_=emv.t[:, 1:2], func=AF.Exp, scale=-0.5),
             reads=[emv.b], writes=[emv.b])
        S.op("dve", lambda e: e.tensor_scalar(out=r.t[:], in0=flat, scalar1=emv.t[:, 0:1], scalar2=emv.t[:, 1:2],
                                              op0=ALU.subtract, op1=ALU.mult), reads=[src.b, r.b, emv.b], writes=[r.b])
        S.op("dve", lambda e: e.tensor_tensor(out=r.t[:], in0=r.t[:], in1=E.lng.t[:], op=ALU.mult),
             reads=[r.b, E.lng.b], writes=[r.b])
        S.op("dve", lambda e: e.tensor_tensor(out=r.t[:], in0=r.t[:], in1=E.lnb.t[:], op=ALU.add),
             reads=[r.b, E.lnb.b], writes=[r.b])
        S.dma("pool", lambda e: e.dma_start(out=xout.t[i * 128:(i + 1) * 128, :], in_=r.t[:]), reads=[r.b], writes=[xout.b], sembuf=r.b)
        return r

    def make_router(es, layer, pbanks, ptr_f, nbuf=2):
        R = Ctx()
        R.rw = sbt(es, "rw%d" % layer, [128, 8, NE], F32)
        R.rb = sbt(es, "rb%d" % layer, [128, NE], F32)
        S.dma("sp", lambda e: e.dma_start(out=R.rw.t[:], in_=W["router_w"][layer].rearrange("(ko p) n -> p ko n", p=128)),
              writes=[R.rw.b])
        S.dma("sp", lambda e: e.dma_start(out=R.rb.t[:], in_=W["router_b"][layer].partition_broadcast(128)), writes=[R.rb.b])
        S.op("dve", lambda e: e.memset(cbase.t[:], 0.0), writes=[cbase.b])
        R.xT = sbt(es, "rxT%d" % layer, [128, 8, 128], F32)
        R.xb = [sbt(es, "rxb%d_%d" % (layer, i), [128, D], BF16) for i in range(nbuf)]
        R.nbuf = nbuf
        R.lg = sbt(es, "rlg%d" % layer, [128, NE], F32)
        R.v8 = sbt(es, "rv8%d" % layer, [128, 8], F32)
        R.i8 = sbt(es, "ri8%d" % layer, [128, 8], U32)
        R.i8f = sbt(es, "ri8f%d" % layer, [128, 8], F32)
        R.mk = sbt(es, "rmk%d" % layer, [128, 4, NE], F32)
        R.msum = sbt(es, "rms%d" % layer, [128, NE], F32)
        R.msb = sbt(es, "rmsb%d" % layer, [128, NE], BF16)
        R.pos = sbt(es, "rpos%d" % layer, [128, NE], F32)
        R.junk = sbt(es, "rjk%d" % layer, [128, NE], F32)
        R.dst = sbt(es, "rdst%d" % layer, [128, 4], F32)
        R.val = sbt(es, "rval%d" % layer, [128, 4], F32)
        R.ex = sbt(es, "rex%d" % layer, [128, 4], F32)
        R.nv0 = sbt(es, "rnv%d" % layer, [128, 1], F32)
        R.ssum = sbt(es, "rss%d" % layer, [128, 1], F32)
        R.pT = ptr_f
        R.pS = pbanks
        return R

    def route_tile(R, i, r):
        for ko in range(8):
            S.op("pe", lambda e, ko=ko: e.transpose(R.pT.t[:, ko * 128:(ko + 1) * 128], r.t[:, ko * 128:(ko + 1) * 128], ident_f.t[:]),
                 reads=[r.b, ident_f.b], writes=[R.pT.b], sig=(ko == 7))
        S.op("act", lambda e: e.copy(out=R.xT.t[:].rearrange("p a b -> p (a b)"), in_=R.pT.t[:]), reads=[R.pT.b], writes=[R.xT.b])
        for ko in range(8):
            S.op("pe", lambda e, ko=ko: e.matmul(R.pS.t[:, 0:NE], lhsT=R.xT.t[:, ko, :], rhs=R.rw.t[:, ko, :], start=(ko == 0), stop=(ko == 7)),
                 reads=[R.xT.b, R.rw.b], writes=[R.pS.b], sig=(ko == 7))
        S.op("dve", lambda e: e.tensor_tensor(out=R.lg.t[:], in0=R.pS.t[:, 0:NE], in1=R.rb.t[:], op=ALU.add),
             reads=[R.pS.b, R.rb.b], writes=[R.lg.b])
        S.op("dve", lambda e: e.max(out=R.v8.t[:], in_=R.lg.t[:]), reads=[R.lg.b], writes=[R.v8.b])
        S.op("dve", lambda e: e.max_index(out=R.i8.t[:], in_max=R.v8.t[:], in_values=R.lg.t[:]), reads=[R.lg.b, R.v8.b], writes=[R.i8.b])
        S.op("dve", lambda e: e.tensor_copy(out=R.i8f.t[:], in_=R.i8.t[:]), reads=[R.i8.b], writes=[R.i8f.b])
        S.op("dve", lambda e: e.tensor_tensor(out=R.mk.t[:], in0=iota_e.t[:].unsqueeze(1).to_broadcast([128, 4, NE]),
                                              in1=R.i8f.t[:, 0:4].unsqueeze(2).to_broadcast([128, 4, NE]), op=ALU.is_equal),
             reads=[iota_e.b, R.i8f.b], writes=[R.mk.b])
        S.op("dve", lambda e: e.tensor_reduce(out=R.msum.t[:], in_=R.mk.t[:].rearrange("p k e -> p e k"), axis=mybir.AxisListType.X, op=ALU.add),
             reads=[R.mk.b], writes=[R.msum.b])
        S.op("dve", lambda e: e.tensor_copy(out=R.msb.t[:], in_=R.msum.t[:]), reads=[R.msum.b], writes=[R.msb.b])
        S.op("pe", lambda e: e.matmul(R.pS.t[:, 64:64 + NE], lhsT=ustr_b.t[:], rhs=R.msb.t[:], start=True, stop=True),
             reads=[ustr_b.b, R.msb.b, R.lg.b], writes=[R.pS.b], sig=False)
        S.op("pe", lambda e: e.matmul(R.pS.t[:, 128:128 + NE], lhsT=ones_b.t[:], rhs=R.msb.t[:], start=True, stop=True),
             reads=[ones_b.b, R.msb.b, R.lg.b], writes=[R.pS.b])
        S.op("dve", lambda e: e.tensor_tensor(out=R.pos.t[:], in0=R.pS.t[:, 64:64 + NE], in1=cbase.t[:], op=ALU.add),
             reads=[R.pS.b, cbase.b], writes=[R.pos.b])
        S.op("dve", lambda e: e.tensor_tensor(out=cbase.t[:], in0=R.pS.t[:, 128:128 + NE], in1=cbase.t[:], op=ALU.add),
             reads=[R.pS.b, cbase.b], writes=[cbase.b])
        S.op("dve", lambda e: e.tensor_scalar(out=R.junk.t[:], in0=R.pos.t[:], scalar1=float(CAP), scalar2=1.0e6,
                                              op0=ALU.is_ge, op1=ALU.mult), reads=[R.pos.b], writes=[R.junk.b])
        S.op("dve", lambda e: e.tensor_tensor(out=R.pos.t[:], in0=R.pos.t[:], in1=R.junk.t[:], op=ALU.add),
             reads=[R.pos.b, R.junk.b], writes=[R.pos.b])
        S.op("dve", lambda e: e.tensor_tensor(out=R.pos.t[:], in0=R.pos.t[:], in1=ecap.t[:], op=ALU.add),
             reads=[R.pos.b, ecap.b], writes=[R.pos.b])
        S.op("dve", lambda e: e.tensor_tensor(out=R.mk.t[:], in0=R.mk.t[:], in1=R.pos.t[:].unsqueeze(1).to_broadcast([128, 4, NE]), op=ALU.mult),
             reads=[R.mk.b, R.pos.b], writes=[R.mk.b])
        S.op("dve", lambda e: e.tensor_reduce(out=R.dst.t[:], in_=R.mk.t[:], axis=mybir.AxisListType.X, op=ALU.add),
             reads=[R.mk.b], writes=[R.dst.b])
        S.op("dve", lambda e: e.tensor_copy(out=dest_i.t[:, i * 4:(i + 1) * 4], in_=R.dst.t[:]), reads=[R.dst.b], writes=[dest_b[i]])
        S.op("dve", lambda e: e.tensor_scalar(out=R.nv0.t[:], in0=R.v8.t[:, 0:1], scalar1=-1.0, scalar2=None, op0=ALU.mult),
             reads=[R.v8.b], writes=[R.nv0.b])
        S.op("act", lambda e: e.activation(out=R.ex.t[:], in_=R.v8.t[:, 0:4], func=AF.Exp, bias=R.nv0.t[:, 0:1], scale=1.0),
             reads=[R.v8.b, R.nv0.b], writes=[R.ex.b])
        S.op("dve", lambda e: e.reduce_sum(out=R.ssum.t[:], in_=R.ex.t[:], axis=mybir.AxisListType.X), reads=[R.ex.b], writes=[R.ssum.b])
        S.op("dve", lambda e: e.reciprocal(out=R.ssum.t[:], in_=R.ssum.t[:]), reads=[R.ssum.b], writes=[R.ssum.b])
        S.op("dve", lambda e: e.tensor_scalar(out=R.val.t[:], in0=R.dst.t[:], scalar1=float(NSLOT), scalar2=None, op0=ALU.is_lt),
             reads=[R.dst.b], writes=[R.val.b])
        S.op("dve", lambda e: e.tensor_scalar(out=R.ex.t[:], in0=R.ex.t[:], scalar1=R.ssum.t[:, 0:1], scalar2=None, op0=ALU.mult),
             reads=[R.ex.b, R.ssum.b], writes=[R.ex.b])
        S.op("dve", lambda e: e.tensor_tensor(out=gate_t.t[:, i * 4:(i + 1) * 4], in0=R.ex.t[:], in1=R.val.t[:], op=ALU.mult),
             reads=[R.ex.b, R.val.b], writes=[gate_b[i]])
        xb = R.xb[i % R.nbuf]
        S.op("act", lambda e: e.copy(out=xb.t[:], in_=r.t[:]), reads=[r.b], writes=[xb.b])
        for k in range(4):
            S.dma("pool", lambda e, k=k: e.indirect_dma_start(
                out=xg.t[:, :], out_offset=bass.IndirectOffsetOnAxis(ap=dest_i.t[:, i * 4 + k:i * 4 + k + 1], axis=0),
                in_=xb.t[:, :], in_offset=None, bounds_check=breg, oob_is_err=False),
                reads=[xb.b, dest_b[i]], writes=[xg.b], after=[G.xg_zero_tok], sembuf=xb.b)

    def moe_phase(layer, xin, xout):
        with contextlib.ExitStack() as es:
            NR = 10
            ring = [sbt(es, "ring%d" % i, [128, 8, 512], BF16) for i in range(NR)]
            stg = [sbt(es, "stg%d" % i, [128, 2, 512], F32) for i in range(8)]
            xl = [sbt(es, "xl%d" % i, [128, NTC, D], BF16) for i in range(2)]
            xgT = [sbt(es, "xgT%d" % i, [128, 8, CAP], BF16) for i in range(2)]
            actT = sbt(es, "actT", [128, 8, CAP], BF16)
            gsb = [sbt(es, "gsb%d" % i, [128, CAP], F32) for i in range(2)]
            sgb = [sbt(es, "sgb%d" % i, [128, CAP], F32) for i in range(2)]
            t1b = [sbt(es, "t1b%d" % i, [128, CAP], F32) for i in range(2)]
            ysb = [sbt(es, "ysb%d" % i, [128, D], F32) for i in range(2)]
            bdn = [sbt(es, "bdn%d" % i, [128, D], F32) for i in range(2)]
            bup = sbt(es, "bup", [128, NE * 16], F32)
            bup1 = sbt(es, "bup1", [128, NE * 16], F32)
            pg = pst(es, "pg", [128, 2, 512], F32)
            pl = pst(es, "pl", [128, 2, 512], F32)
            py = pst(es, "py", [128, 2, 512], F32)
            ptrs = [pst(es, "ptr%d" % i, [128, 8, 128], BF16) for i in range(2)]
            pyb = [Buf("py_h0"), Buf("py_h1")]

            S.dma("sp", lambda e: e.dma_start(out=bup.t[:], in_=W["bup"][:, layer * NE * 16:(layer + 1) * NE * 16]), writes=[bup.b])
            S.op("dve", lambda e: e.tensor_scalar(out=bup1.t[:], in0=bup.t[:], scalar1=1.0, scalar2=None, op0=ALU.add),
                 reads=[bup.b], writes=[bup1.b])

            corder = [("up", 0), ("up", 2), ("up", 1), ("up", 3), ("dn", 0), ("dn", 1)]
            nchunks = NE * 6
            pcount = [0]

            cast_q = []
            step = [0]

            def emit_casts(pred, limit=99):
                n = 0
                k = 0
                while k < len(cast_q) and n < limit:
                    if pred(cast_q[k]):
                        cast_q.pop(k)[3]()
                        n += 1
                    else:
                        break

            def tick():
                step[0] += 1
                emit_casts(lambda c: c[0] <= step[0], limit=2)

            def flush_chunk(j):
                emit_casts(lambda c: c[1] <= j)

            def load_chunk(j):
                if j >= nchunks:
                    return
                e_, c = divmod(j, 6)
                kind, gi = corder[c]
                src = W["up"][layer, e_] if kind == "up" else W["dn"][layer, e_]
                slot = ring[j % NR]
                for q in range(4):
                    si = pcount[0] % len(stg)
                    st = stg[si]
                    pcount[0] += 1
                    while any(c[2] == si for c in cast_q):
                        cast_q.pop(0)[3]()
                    S.dma("sp", lambda e: e.dma_start(
                        out=st.t[:], in_=src[q * 256:(q + 1) * 256, gi * 512:(gi + 1) * 512].rearrange("(ko p) f -> p ko f", p=128)),
                        writes=[st.b])

                    def emit(st=st, q=q, slot=slot):
                        S.op("act", lambda e: e.copy(out=slot.t[:, q * 2:(q + 1) * 2, :], in_=st.t[:]), reads=[st.b], writes=[slot.b])
                    cast_q.append([step[0] + 2 + q // 2, j, si, emit])

            def load_x(e_):
                if e_ >= NE:
                    return
                S.dma("sp", lambda e: e.dma_start(out=xl[e_ % 2].t[:], in_=xg.t[e_ * CAP:(e_ + 1) * CAP, :].rearrange("(t p) d -> p t d", p=128)),
                      reads=[xg.b], writes=[xl[e_ % 2].b])

            def load_bdn(e_):
                if e_ >= NE:
                    return
                S.dma("sp", lambda e: e.dma_start(out=bdn[e_ % 2].t[:], in_=W["bdn"][layer, e_].partition_broadcast(128)),
                      writes=[bdn[e_ % 2].b])

            def trans_tile(e_, tt):
                if e_ >= NE:
                    return
                src = xl[e_ % 2]
                dst = xgT[e_ % 2]
                ptr = ptrs[tt % 2]
                for ko in range(8):
                    S.op("pe", lambda e, ko=ko: e.transpose(ptr.t[:, ko, :], src.t[:, tt, ko * 128:(ko + 1) * 128], ident_b.t[:]),
                         reads=[src.b, ident_b.b], writes=[ptr.b], sig=(ko == 7))
                S.op("act", lambda e: e.copy(out=dst.t[:, :, tt * 128:(tt + 1) * 128], in_=ptr.t[:]),
                     reads=[ptr.b], writes=[dst.b])

            def transposes(e_):
                for tt in range(NTC):
                    trans_tile(e_, tt)

            def down_tile(e_, tt):
                base = e_ * 6
                wd = [ring[(base + 4) % NR], ring[(base + 5) % NR]]
                y_ = ysb[tt % 2]
                for half in range(2):
                    for fo in range(8):
                        S.op("pe", lambda e, fo=fo: e.matmul(
                            py.t[:, half, :], lhsT=actT.t[:, fo, tt * 128:(tt + 1) * 128], rhs=wd[half].t[:, fo, :],
                            start=(fo == 0), stop=(fo == 7)),
                            reads=[actT.b, wd[half].b], writes=[pyb[half]], sig=(fo == 7))
                    S.op("dve", lambda e: e.tensor_tensor(out=y_.t[:, half * 512:(half + 1) * 512], in0=py.t[:, half, :],
                                                          in1=bdn[e_ % 2].t[:, half * 512:(half + 1) * 512], op=ALU.add),
                         reads=[pyb[half], bdn[e_ % 2].b] + ([y_.b] if half else []), writes=[y_.b])
                S.dma("pool", lambda e: e.dma_start(out=yg.t[e_ * CAP + tt * 128:e_ * CAP + (tt + 1) * 128, :], in_=y_.t[:]),
                      reads=[y_.b], writes=[yg.b], sembuf=y_.b)

            for j in range(NR):
                load_chunk(j)
                flush_chunk(j)
            load_x(0)
            load_bdn(0)
            transposes(0)
            load_x(1)
            load_bdn(1)
            HN = CAP // 2
            for e_ in range(NE):
                xT = xgT[e_ % 2]
                base = e_ * 6
                flush_chunk(base + 1)
                for j in range(8):
                    if j == 4:
                        flush_chunk(base + 3)
                    tick()
                    cg = ring[(base + (0 if j < 4 else 2)) % NR]
                    cl = ring[(base + (1 if j < 4 else 3)) % NR]
                    col = (j % 4) * 128
                    for (pp, cw) in ((pg, cg), (pl, cl)):
                        for half in range(2):
                            for ko in range(8):
                                S.op("pe", lambda e, pp=pp, cw=cw, half=half, ko=ko: e.matmul(
                                    pp.t[:, half, 0:HN], lhsT=cw.t[:, ko, col:col + 128], rhs=xT.t[:, ko, half * HN:(half + 1) * HN],
                                    start=(ko == 0), stop=(ko == 7)),
                                    reads=[cw.b, xT.b], writes=[pp.b], sig=(half == 1 and ko == 7))
                    g_ = gsb[j % 2]
                    s_ = sgb[j % 2]
                    t_ = t1b[j % 2]
                    bcol = e_ * 16 + j
                    S.op("dve", lambda e, g_=g_, bcol=bcol: e.tensor_scalar(
                        out=g_.t[:].rearrange("p (a b) -> p a b", a=2), in0=pg.t[:, :, 0:HN], scalar1=bup.t[:, bcol:bcol + 1], scalar2=7.0,
                        op0=ALU.add, op1=ALU.min), reads=[pg.b, bup.b], writes=[g_.b])
                    S.op("act", lambda e, g_=g_, s_=s_: e.activation(out=s_.t[:], in_=g_.t[:], func=AF.Sigmoid, scale=1.702),
                         reads=[g_.b], writes=[s_.b])
                    S.op("dve", lambda e, t_=t_, bcol=bcol: e.tensor_scalar(
                        out=t_.t[:].rearrange("p (a b) -> p a b", a=2), in0=pl.t[:, :, 0:HN], scalar1=bup1.t[:, bcol + 8:bcol + 9], scalar2=-6.0,
                        op0=ALU.add, op1=ALU.max), reads=[pl.b, bup1.b], writes=[t_.b])
                    S.op("dve", lambda e, t_=t_, g_=g_: e.scalar_tensor_tensor(out=t_.t[:], in0=t_.t[:], scalar=8.0, in1=g_.t[:],
                                                                         op0=ALU.min, op1=ALU.mult), reads=[t_.b, g_.b], writes=[t_.b])
                    S.op("dve", lambda e, t_=t_, s_=s_, j=j: e.tensor_tensor(out=actT.t[:, j, :], in0=t_.t[:], in1=s_.t[:], op=ALU.mult),
                         reads=[t_.b, s_.b], writes=[actT.b])
                    if j == 3:
                        load_chunk(base + 0 + NR)
                        load_chunk(base + 1 + NR)
                    if j == 7:
                        load_chunk(base + 2 + NR)
                        load_chunk(base + 3 + NR)
                flush_chunk(base + 5)
                trans_tile(e_ + 1, 0)
                trans_tile(e_ + 1, 1)
                tick()
                down_tile(e_, 0)
                trans_tile(e_ + 1, 2)
                tick()
                down_tile(e_, 1)
                trans_tile(e_ + 1, 3)
                tick()
                down_tile(e_, 2)
                trans_tile(e_ + 1, 4)
                tick()
                down_tile(e_, 3)
                tick()
                down_tile(e_, 4)
                load_x(e_ + 2)
                load_chunk(base + 4 + NR)
                load_chunk(base + 5 + NR)
                load_bdn(e_ + 2)
            flush_chunk(nchunks)
        S.barrier()
        with contextlib.ExitStack() as es:
            E = make_epi(es, layer, 1)
            yk = [[sbt(es, "yk%d_%d" % (b, k), [128, D], F32) for k in range(4)] for b in range(2)]
            dg = [[sbt(es, "dg%d_%d" % (b, k), [128, 128], F32) for k in range(4)] for b in range(2)]
            dal = sbt(es, "dal", [128, 128], F32)
            S.op("dve", lambda e: e.tensor_scalar(out=dal.t[:], in0=ident_f.t[:], scalar1=ALPHA, scalar2=None, op0=ALU.mult),
                 reads=[ident_f.b], writes=[dal.b])
            pacc = [pst(es, "pacc%d" % b, [128, 2, 512], F32) for b in range(2)]

            def issue_gathers(i):
                b = i % 2
                for k in range(4):
                    if i < 2:
                        S.op("pool", lambda e, k=k: e.memset(yk[b][k].t[:], 0.0), writes=[yk[b][k].b])
                    S.dma("pool", lambda e, k=k: e.indirect_dma_start(
                        out=yk[b][k].t[:, :], out_offset=None, in_=yg.t[:, :],
                        in_offset=bass.IndirectOffsetOnAxis(ap=dest_i.t[:, i * 4 + k:i * 4 + k + 1], axis=0),
                        bounds_check=breg, oob_is_err=False), reads=[yg.b, dest_b[i]], writes=[yk[b][k].b])
                for k in range(4):
                    S.op("dve", lambda e, k=k: e.tensor_scalar(out=dg[b][k].t[:], in0=ident_f.t[:], scalar1=gate_t.t[:, i * 4 + k:i * 4 + k + 1],
                                                               scalar2=None, op0=ALU.mult), reads=[ident_f.b, gate_b[i]], writes=[dg[b][k].b])

            issue_gathers(0)
            epi_load(E, 0, xin)
            for i in range(NTILE):
                b = i % 2
                if i + 1 < NTILE:
                    issue_gathers(i + 1)
                    epi_load(E, i + 1, xin)

                def add_fn(r, xt, b=b, i=i):
                    ps = pacc[b]
                    for half in range(2):
                        cs = slice(half * 512, (half + 1) * 512)
                        S.op("pe", lambda e: e.matmul(ps.t[:, half, :], lhsT=dal.t[:], rhs=xt.t[:, cs], start=True, stop=False),
                             reads=[dal.b, xt.b], writes=[ps.b], sig=False)
                        for k in range(4):
                            S.op("pe", lambda e, k=k: e.matmul(ps.t[:, half, :], lhsT=dg[b][k].t[:], rhs=yk[b][k].t[:, cs], start=False, stop=(k == 3)),
                                 reads=[dal.b, xt.b] + [t_.b for t_ in dg[b]] + [t_.b for t_ in yk[b]], writes=[ps.b],
                                 sig=(half == 1 and k == 3))
                    return ps
                epilogue(E, i, xin, xout, add_fn, preloaded=True)
        S.barrier()

    G.S = S
    G.nc = nc
    G.es_top = es_top
    G.xs = xs
    G.W = W
    G.sbt = sbt
    G.pst = pst
    G.make_epi = make_epi
    G.epilogue = epilogue
    G.make_router = make_router
    G.route_tile = route_tile
    G.consts = dict(ident_f=ident_f, ident_b=ident_b, uincl_f=uincl_f, ones_f=ones_f, ones_b=ones_b)

    for st in stages:
        if st == "ab":
            mixer_ab_phase(G, xs["x0"], xs["x1"])
        elif st == "moe0":
            moe_phase(0, xs["x1"], xs["x2"])
        elif st == "cf":
            conformer_phase(G, xs["x2"], xs["x3"])
        elif st == "moe1":
            moe_phase(1, xs["x3"], xs["out"])
        elif st == "route0":
            route_only_phase(G, 0, xs["x1"])
        elif st == "route1":
            route_only_phase(G, 1, xs["x3"])

    S.barrier(engines=["sp"])
    es_top.close()
    S.close()
    return nc


def route_only_phase(G, layer, xin):
    S = G.S
    with contextlib.ExitStack() as es:
        pT = G.pst(es, "rpT", [128, 1024], F32)
        pS = G.pst(es, "rpS", [128, 512], F32)
        R = G.make_router(es, layer, pS, pT)
        rt = [G.sbt(es, "rot%d" % i, [128, D], F32) for i in range(2)]
        for i in range(NTILE):
            r = rt[i % 2]
            S.dma("sp", lambda e: e.dma_start(out=r.t[:], in_=xin.t[i * 128:(i + 1) * 128, :]), reads=[xin.b], writes=[r.b])
            G.route_tile(R, i, r)
    S.barrier()


def load_weight_bf16(G, es_stage, dst, src, ncols, nm):
    S = G.S
    stg = [G.sbt(es_stage, "wst_%s%d" % (nm, i), [128, ncols], F32) for i in range(2)]
    for ko in range(8):
        st = stg[ko % 2]
        S.dma("sp", lambda e: e.dma_start(out=st.t[:], in_=src[ko * 128:(ko + 1) * 128, :]), writes=[st.b])
        eng = "pool" if ko % 2 == 0 else "act"
        if eng == "pool":
            S.op("pool", lambda e: e.tensor_copy(out=dst.t[:, ko, :], in_=st.t[:]), reads=[st.b], writes=[dst.b])
        else:
            S.op("act", lambda e: e.copy(out=dst.t[:, ko, :], in_=st.t[:]), reads=[st.b], writes=[dst.b])


def build_xT(G, X, xin, blk):
    S = G.S
    c = G.consts
    for tl in range(4):
        i = blk * 4 + tl
        S.dma("sp", lambda e: e.dma_start(out=X.xl.t[:], in_=xin.t[i * 128:(i + 1) * 128, :]), reads=[xin.b], writes=[X.xl.b])
        S.op("act", lambda e: e.copy(out=X.xlb.t[:], in_=X.xl.t[:]), reads=[X.xl.b], writes=[X.xlb.b])
        for ko in range(8):
            S.op("pe", lambda e: e.transpose(X.PT.t[:, ko * 128:(ko + 1) * 128], X.xlb.t[:, ko * 128:(ko + 1) * 128], c["ident_b"].t[:]),
                 reads=[X.xlb.b, c["ident_b"].b], writes=[X.PT.b], sig=(ko == 7))
        S.op("dve", lambda e: e.tensor_copy(out=X.xT.t[:, :, tl * 128:(tl + 1) * 128], in_=X.PT.t[:].rearrange("p (a b) -> p a b", a=8)),
             reads=[X.PT.b], writes=[X.xT.b])


def mixer_ab_phase(G, xin, xout):
    S, W, sbt, pst = G.S, G.W, G.sbt, G.pst
    c = G.consts
    ident_b, uincl_f, ones_f = c["ident_b"], c["uincl_f"], c["ones_f"]
    with contextlib.ExitStack() as es:
        w_in = sbt(es, "w_in", [128, 8, 3080], BF16)
        w_out = sbt(es, "w_out", [128, 8, D], BF16)
        with contextlib.ExitStack() as es2:
            load_weight_bf16(G, es2, w_in, W["w_in"], 3080, "in")
            load_weight_bf16(G, es2, w_out, W["w_out"], D, "out")
            S.barrier()
        PAB = pst(es, "PAB", [128, 1024], F32)
        PC = pst(es, "PC", [128, 512], F32)
        PD = pst(es, "PD", [128, 512], F32)
        PT = pst(es, "PT", [128, 1024], BF16)
        PE_ = pst(es, "PE", [128, 512], F32)
        PF = pst(es, "PF", [128, 512], F32)
        PG = pst(es, "PG", [128, 512], F32)
        E = G.make_epi(es, 0, 0)
        bif = sbt(es, "bif", [128, 8], F32)
        cw = sbt(es, "cw", [128, 12], F32)
        hg = sbt(es, "hg", [128, 512], F32)
        S.dma("sp", lambda e: e.dma_start(out=bif.t[:], in_=W["ab_bif"].partition_broadcast(128)), writes=[bif.b])
        S.dma("sp", lambda e: e.dma_start(out=cw.t[:], in_=W["ab_conv"]), writes=[cw.b])
        S.dma("sp", lambda e: e.dma_start(out=hg.t[:], in_=W["ab_hg"].partition_broadcast(128)), writes=[hg.b])
        maskT = sbt(es, "maskT", [128, 512], F32)
        for h in range(4):
            S.op("pool", lambda e: e.tensor_copy(out=maskT.t[:, h * 128:(h + 1) * 128], in_=uincl_f.t[:]), reads=[uincl_f.b], writes=[maskT.b])
        tmpc = sbt(es, "tmpc", [128, 512], F32)
        y1 = sbt(es, "y1", [128, 512], F32)
        LNK = math.log(0.125)
        lnk = sbt(es, "lnk", [128, 1], F32)
        S.op("pool", lambda e: e.memset(lnk.t[:], LNK), writes=[lnk.b])
        PS = []
        for sq in range(2):
            P = Ctx()
            X = Ctx()
            X.xl = sbt(es, "xl", [128, D], F32)
            X.xlb = sbt(es, "xlb", [128, D], BF16)
            X.xT = sbt(es, "xT", [128, 8, 512], BF16)
            X.PT = PT
            P.X = X
            P.R = G.make_router(es, 0, PE_, PAB, nbuf=1)
            P.u = [sbt(es, "u%d" % i, [128, 514], F32) for i in range(4)]
            P.yT = sbt(es, "yT", [128, 4, 512], BF16)
            for nm, shp, dt in (("fi", [128, 8], F32), ("e1", [128, 4], F32), ("lf", [128, 4], F32), ("qsc", [128, 4], F32),
                                ("ksc", [128, 4], F32), ("t4", [128, 4], F32), ("eg", [128, 4], F32), ("qs", [128, 256], BF16),
                                ("ks", [128, 256], BF16), ("vp", [128, 4, 129], BF16), ("so", [128, 512], F32),
                                ("qkT", [64, 8, 128], BF16), ("SmT", [128, 512], BF16), ("Cst", [64, 4, 129], F32),
                                ("Cb", [64, 4, 129], BF16), ("dn", [128, 4], F32), ("hh", [128, 512], F32), ("hst", [128, 24], F32),
                                ("hmv", [128, 8], F32), ("hrs", [128, 4], F32), ("hA", [128, 512], BF16), ("hAT", [128, 4, 128], BF16)):
                setattr(P, nm, sbt(es, nm, shp, dt))
            S.op("pool", lambda e: e.memset(P.vp.t[:], 1.0), writes=[P.vp.b])
            PS.append(P)

        def stream(sq):
            P = PS[sq]
            X, R, u, yTb = P.X, P.R, P.u, P.yT
            fi, e1, lf, qsc, ksc, t4, eg, qs, ks, vp, so = P.fi, P.e1, P.lf, P.qsc, P.ksc, P.t4, P.eg, P.qs, P.ks, P.vp, P.so
            qkT, SmT, Cst, Cb, dn, hh, hst, hmv, hrs, hA, hAT = P.qkT, P.SmT, P.Cst, P.Cb, P.dn, P.hh, P.hst, P.hmv, P.hrs, P.hA, P.hAT
            for blk in range(sq * 4, sq * 4 + 4):
                if blk % 4 == 0:
                    for cc in range(4):
                        S.op("pool", lambda e: e.memset(u[cc].t[:, 0:2], 0.0), writes=[u[cc].b])
                    S.op("pool", lambda e: e.memset(Cst.t[:], 0.0), writes=[Cst.b])
                    S.op("pool", lambda e: e.memset(Cb.t[:], 0.0), writes=[Cb.b])
                build_xT(G, X, xin, blk)
                yield
                xT = X.xT
                for cc in range(4):
                    for (bank, c0) in ((PE_, 1544 + 512 + cc * 128), (PF, 1544 + 1024 + cc * 128), (PG, 1544 + cc * 128)):
                        for ko in range(8):
                            S.op("pe", lambda e: e.matmul(bank.t[:], lhsT=w_in.t[:, ko, c0:c0 + 128], rhs=xT.t[:, ko, :], start=(ko == 0), stop=(ko == 7)),
                                 reads=[w_in.b, xT.b], writes=[bank.b], sig=(ko == 7))
                    uu = u[cc]
                    S.op("act", lambda e: e.copy(out=tmpc.t[:], in_=PE_.t[:]), reads=[PE_.b], writes=[tmpc.b])
                    S.op("dve", lambda e: e.tensor_tensor(out=uu.t[:, 2:514], in0=tmpc.t[:], in1=PF.t[:], op=ALU.mult),
                         reads=[tmpc.b, PF.b], writes=[uu.b])
                    S.op("dve", lambda e: e.tensor_scalar(out=y1.t[:], in0=uu.t[:, 2:514], scalar1=cw.t[:, 8 + cc:9 + cc], scalar2=None, op0=ALU.mult),
                         reads=[uu.b, cw.b], writes=[y1.b])
                    S.op("dve", lambda e: e.scalar_tensor_tensor(out=y1.t[:], in0=uu.t[:, 1:513], scalar=cw.t[:, 4 + cc:5 + cc], in1=y1.t[:],
                                                                 op0=ALU.mult, op1=ALU.add), reads=[uu.b, cw.b, y1.b], writes=[y1.b])
                    S.op("dve", lambda e: e.scalar_tensor_tensor(out=y1.t[:], in0=uu.t[:, 0:512], scalar=cw.t[:, cc:cc + 1], in1=y1.t[:],
                                                                 op0=ALU.mult, op1=ALU.add), reads=[uu.b, cw.b, y1.b], writes=[y1.b])
                    S.op("dve", lambda e: e.tensor_tensor(out=yTb.t[:, cc, :], in0=y1.t[:], in1=PG.t[:], op=ALU.mult),
                         reads=[y1.b, PG.b], writes=[yTb.b])
                    S.op("dve", lambda e: e.tensor_copy(out=uu.t[:, 0:2], in_=uu.t[:, 512:514]), reads=[uu.b], writes=[uu.b])
                    yield
                yield
                for tl in range(4):
                    i = blk * 4 + tl
                    tc0 = tl * 128
                    for (out_ap, bank, c0, c1) in ((PAB.t[:, 0:512], PAB, 0, 512), (PAB.t[:, 512:1024], PAB, 512, 1024),
                                                   (PC.t[:], PC, 1024, 1536), (PD.t[:, 0:8], PD, 1536, 1544)):
                        for ko in range(8):
                            S.op("pe", lambda e: e.matmul(out_ap, lhsT=xT.t[:, ko, tc0:tc0 + 128], rhs=w_in.t[:, ko, c0:c1], start=(ko == 0), stop=(ko == 7)),
                                 reads=[w_in.b, xT.b], writes=[bank.b], sig=(ko == 7 and c0 != 0))
                    S.op("dve", lambda e: e.tensor_tensor(out=fi.t[:], in0=PD.t[:, 0:8], in1=bif.t[:], op=ALU.add), reads=[PD.b, bif.b], writes=[fi.b])
                    S.op("act", lambda e: e.activation(out=e1.t[:], in_=fi.t[:, 4:8], func=AF.Exp, scale=-1.0), reads=[fi.b], writes=[e1.b])
                    S.op("act", lambda e: e.activation(out=lf.t[:], in_=e1.t[:], func=AF.Ln, bias=1.0), reads=[e1.b], writes=[lf.b])
                    S.op("pe", lambda e: e.matmul(PD.t[:, 16:20], lhsT=uincl_f.t[:], rhs=lf.t[:], start=True, stop=True),
                         reads=[uincl_f.b, lf.b], writes=[PD.b], sig=False)
                    S.op("pe", lambda e: e.matmul(PD.t[:, 20:24], lhsT=ones_f.t[:], rhs=lf.t[:], start=True, stop=True),
                         reads=[ones_f.b, lf.b], writes=[PD.b])
                    S.op("act", lambda e: e.activation(out=qsc.t[:], in_=PD.t[:, 16:20], func=AF.Exp, scale=-1.0), reads=[PD.b], writes=[qsc.b])
                    S.op("dve", lambda e: e.tensor_tensor(out=t4.t[:], in0=PD.t[:, 16:20], in1=fi.t[:, 0:4], op=ALU.add), reads=[PD.b, fi.b], writes=[t4.b])
                    S.op("act", lambda e: e.activation(out=ksc.t[:], in_=t4.t[:], func=AF.Exp, bias=lnk.t[:, 0:1], scale=1.0), reads=[t4.b, lnk.b], writes=[ksc.b])
                    S.op("act", lambda e: e.activation(out=eg.t[:], in_=PD.t[:, 20:24], func=AF.Exp, scale=-1.0), reads=[PD.b], writes=[eg.b])
                    for h in range(4):
                        S.op("dve", lambda e: e.tensor_scalar(out=qs.t[:, h * 64:(h + 1) * 64], in0=PAB.t[:, h * 64:(h + 1) * 64], scalar1=qsc.t[:, h:h + 1],
                                                              scalar2=None, op0=ALU.mult), reads=[PAB.b, qsc.b], writes=[qs.b])
                        S.op("dve", lambda e: e.tensor_scalar(out=ks.t[:, h * 64:(h + 1) * 64], in0=PAB.t[:, 256 + h * 64:256 + (h + 1) * 64],
                                                              scalar1=ksc.t[:, h:h + 1], scalar2=None, op0=ALU.mult), reads=[PAB.b, ksc.b], writes=[ks.b])
                    S.op("act", lambda e: e.copy(out=vp.t[:, :, 0:128], in_=PAB.t[:, 512:1024].rearrange("p (a b) -> p a b", a=4)),
                         reads=[PAB.b], writes=[vp.b])
                    S.op("act", lambda e: e.activation(out=so.t[:], in_=PC.t[:], func=AF.Sigmoid), reads=[PC.b], writes=[so.b])
                    yield
                    PT64 = PT.t[0:64, :].rearrange("p (a b) -> p a b", a=8)
                    for h in range(4):
                        S.op("pe", lambda e: e.transpose(PT64[:, h, :], qs.t[:, h * 64:(h + 1) * 64], ident_b.t[:]),
                             reads=[qs.b, ident_b.b], writes=[PT.b], sig=False)
                    for h in range(4):
                        S.op("pe", lambda e: e.transpose(PT64[:, 4 + h, :], ks.t[:, h * 64:(h + 1) * 64], ident_b.t[:]),
                             reads=[ks.b, qs.b, ident_b.b], writes=[PT.b], sig=(h == 3))
                    S.op("act", lambda e: e.copy(out=qkT.t[:], in_=PT64), reads=[PT.b], writes=[qkT.b])
                    yield
                    for h in range(4):
                        S.op("pe", lambda e: e.matmul(PE_.t[:, h * 128:(h + 1) * 128], lhsT=qkT.t[:, 4 + h, :], rhs=qkT.t[:, h, :], start=True, stop=True),
                             reads=[qkT.b], writes=[PE_.b], sig=(h == 3))
                    S.op("dve", lambda e: e.tensor_tensor(out=SmT.t[:], in0=PE_.t[:], in1=maskT.t[:], op=ALU.mult), reads=[PE_.b, maskT.b], writes=[SmT.b])
                    yield
                    for h in range(4):
                        bank = PF if h < 2 else PG
                        o_ap = bank.t[:, (h % 2) * 129:(h % 2 + 1) * 129]
                        S.op("pe", lambda e: e.matmul(o_ap, lhsT=SmT.t[:, h * 128:(h + 1) * 128], rhs=vp.t[:, h, :], start=True, stop=False),
                             reads=[SmT.b, vp.b], writes=[bank.b], sig=False)
                        S.op("pe", lambda e: e.matmul(o_ap, lhsT=qkT.t[:, h, :], rhs=Cb.t[:, h, :], start=False, stop=True),
                             reads=[qkT.b, Cb.b, SmT.b, vp.b], writes=[bank.b], sig=(h % 2 == 1))
                    for h in range(4):
                        o_ap = PAB.t[0:64, (h // 2) * 512 + (h % 2) * 129:(h // 2) * 512 + (h % 2 + 1) * 129]
                        S.op("pe", lambda e: e.matmul(o_ap, lhsT=ks.t[:, h * 64:(h + 1) * 64], rhs=vp.t[:, h, :], start=True, stop=True),
                             reads=[ks.b, vp.b], writes=[PAB.b], sig=(h == 3))
                    for h in range(4):
                        o_ap = PAB.t[0:64, (h // 2) * 512 + (h % 2) * 129:(h // 2) * 512 + (h % 2 + 1) * 129]
                        S.op("dve", lambda e: e.tensor_scalar(out=Cst.t[:, h, :], in0=Cst.t[:, h, :], scalar1=eg.t[0:64, h:h + 1], scalar2=None, op0=ALU.mult),
                             reads=[Cst.b, eg.b], writes=[Cst.b])
                        S.op("dve", lambda e: e.scalar_tensor_tensor(out=Cst.t[:, h, :], in0=o_ap, scalar=eg.t[0:64, h:h + 1], in1=Cst.t[:, h, :],
                                                                     op0=ALU.mult, op1=ALU.add), reads=[PAB.b, eg.b, Cst.b], writes=[Cst.b])
                    S.op("act", lambda e: e.copy(out=Cb.t[:], in_=Cst.t[:]), reads=[Cst.b], writes=[Cb.b])
                    for bi, bank in enumerate((PF, PG)):
                        den = bank.t[:, 0:258].rearrange("p (h c) -> p h c", c=129)[:, :, 128]
                        S.op("act", lambda e: e.activation(out=dn.t[:, 2 * bi:2 * bi + 2], in_=den, func=AF.Abs), reads=[bank.b], writes=[dn.b])
                    S.op("dve", lambda e: e.tensor_scalar(out=dn.t[:], in0=dn.t[:], scalar1=1.0, scalar2=None, op0=ALU.max), reads=[dn.b], writes=[dn.b])
                    S.op("dve", lambda e: e.reciprocal(out=dn.t[:], in_=dn.t[:]), reads=[dn.b], writes=[dn.b])
                    for h in range(4):
                        bank = PF if h < 2 else PG
                        S.op("dve", lambda e: e.tensor_scalar(out=hh.t[:, h * 128:(h + 1) * 128], in0=bank.t[:, (h % 2) * 129:(h % 2) * 129 + 128],
                                                              scalar1=dn.t[:, h:h + 1], scalar2=None, op0=ALU.mult), reads=[bank.b, dn.b], writes=[hh.b])
                    for h in range(4):
                        S.op("dve", lambda e: e.bn_stats(out=hst.t[:, h * 6:(h + 1) * 6], in_=hh.t[:, h * 128:(h + 1) * 128]), reads=[hh.b], writes=[hst.b])
                    for h in range(4):
                        S.op("dve", lambda e: e.bn_aggr(out=hmv.t[:, h * 2:(h + 1) * 2], in_=hst.t[:, h * 6:(h + 1) * 6]), reads=[hst.b], writes=[hmv.b])
                    S.op("act", lambda e: e.activation(out=hrs.t[:], in_=hmv.t[:].rearrange("p (h c) -> p h c", c=2)[:, :, 1], func=AF.Sqrt, bias=HN_EPS),
                         reads=[hmv.b], writes=[hrs.b])
                    S.op("dve", lambda e: e.reciprocal(out=hrs.t[:], in_=hrs.t[:]), reads=[hrs.b], writes=[hrs.b])
                    for h in range(4):
                        S.op("dve", lambda e: e.tensor_scalar(out=hh.t[:, h * 128:(h + 1) * 128], in0=hh.t[:, h * 128:(h + 1) * 128],
                                                              scalar1=hmv.t[:, 2 * h:2 * h + 1], scalar2=hrs.t[:, h:h + 1], op0=ALU.subtract, op1=ALU.mult),
                             reads=[hh.b, hmv.b, hrs.b], writes=[hh.b])
                    S.op("dve", lambda e: e.tensor_tensor(out=hh.t[:], in0=hh.t[:], in1=hg.t[:], op=ALU.mult), reads=[hh.b, hg.b], writes=[hh.b])
                    S.op("dve", lambda e: e.tensor_tensor(out=hA.t[:], in0=hh.t[:], in1=so.t[:], op=ALU.mult), reads=[hh.b, so.b], writes=[hA.b])
                    for cq in range(4):
                        S.op("pe", lambda e: e.transpose(PT.t[:, cq * 128:(cq + 1) * 128], hA.t[:, cq * 128:(cq + 1) * 128], ident_b.t[:]),
                             reads=[hA.b, ident_b.b], writes=[PT.b], sig=(cq == 3))
                    S.op("act", lambda e: e.copy(out=hAT.t[:], in_=PT.t[:, 0:512].rearrange("p (a b) -> p a b", a=4)), reads=[PT.b], writes=[hAT.b])
                    yield
                    for half, bank in enumerate((PC, PD)):
                        for cq in range(8):
                            lhsT = hAT.t[:, cq, :] if cq < 4 else yTb.t[:, cq - 4, tc0:tc0 + 128]
                            S.op("pe", lambda e: e.matmul(bank.t[:], lhsT=lhsT, rhs=w_out.t[:, cq, half * 512:(half + 1) * 512], start=(cq == 0), stop=(cq == 7)),
                                 reads=[hAT.b, yTb.b, w_out.b], writes=[bank.b], sig=(cq == 7))

                    def add_fn(r, xt):
                        S.op("dve", lambda e: e.scalar_tensor_tensor(out=r.t[:, 0:512], in0=xt.t[:, 0:512], scalar=ALPHA, in1=PC.t[:], op0=ALU.mult, op1=ALU.add),
                             reads=[xt.b, PC.b], writes=[r.b])
                        S.op("dve", lambda e: e.scalar_tensor_tensor(out=r.t[:, 512:1024], in0=xt.t[:, 512:1024], scalar=ALPHA, in1=PD.t[:], op0=ALU.mult, op1=ALU.add),
                             reads=[xt.b, PD.b, r.b], writes=[r.b])
                    r = G.epilogue(E, i, xin, xout, add_fn, slot=sq)
                    yield
                    G.route_tile(R, i, r)
                    yield

        gens = [stream(0), stream(1)]
        live = [True, True]
        while any(live):
            for q in range(2):
                if live[q]:
                    try:
                        next(gens[q])
                    except StopIteration:
                        live[q] = False
    S.barrier()


def conformer_phase(G, xin, xout):
    S, W, sbt, pst = G.S, G.W, G.sbt, G.pst
    c = G.consts
    ident_b, ident_f, ones_f = c["ident_b"], c["ident_f"], c["ones_f"]
    with contextlib.ExitStack() as es:
        pw1 = sbt(es, "pw1", [128, 8, 2048], BF16)
        pw2 = sbt(es, "pw2", [128, 8, D], BF16)
        with contextlib.ExitStack() as es2:
            load_weight_bf16(G, es2, pw1, W["pw1"], 2048, "p1")
            load_weight_bf16(G, es2, pw2, W["pw2"], D, "p2")
            S.barrier()
        cfdw = sbt(es, "cfdw", [128, 248], F32)
        cfv = sbt(es, "cfv", [128, 24], F32)
        S.dma("sp", lambda e: e.dma_start(out=cfdw.t[:], in_=W["cf_dw"]), writes=[cfdw.b])
        S.dma("sp", lambda e: e.dma_start(out=cfv.t[:], in_=W["cf_vec"]), writes=[cfv.b])
        diag = sbt(es, "diag", [128, 248, 128], BF16)
        for q in range(248):
            S.op("dve", lambda e: e.tensor_scalar(out=diag.t[:, q, :], in0=ident_f.t[:], scalar1=cfdw.t[:, q:q + 1], scalar2=None, op0=ALU.mult),
                 reads=[ident_f.b, cfdw.b], writes=[diag.b])
        onesm = sbt(es, "onesm", [128, 128], F32)
        S.op("pool", lambda e: e.memset(onesm.t[:], 1.0 / 1024.0), writes=[onesm.b])
        X = Ctx()
        X.xl = sbt(es, "xl", [128, D], F32)
        X.xlb = sbt(es, "xlb", [128, D], BF16)
        X.xT = sbt(es, "xT", [128, 8, 512], BF16)
        PAB = pst(es, "PAB", [128, 1024], F32)
        PC = pst(es, "PC", [128, 512], F32)
        PD = pst(es, "PD", [128, 512], F32)
        PT = pst(es, "PT", [128, 1024], BF16)
        PE_ = pst(es, "PE", [128, 512], F32)
        PF = pst(es, "PF", [128, 512], F32)
        PG = pst(es, "PG", [128, 512], F32)
        X.PT = PT
        E = G.make_epi(es, 1, 0, nbuf=2)
        R = G.make_router(es, 1, PE_, PAB, nbuf=1)
        ub = [sbt(es, "ub%d" % i, [128, 542], BF16) for i in range(8)]
        cv = [sbt(es, "cv%d" % i, [128, 512], F32) for i in range(8)]
        sg = sbt(es, "sg", [128, 512], F32)
        sq = sbt(es, "sq", [128, 512], F32)
        m2 = sq
        rstd = sbt(es, "rstd", [128, 512], F32)
        nmr = sbt(es, "nmr", [128, 512], F32)
        nn = sg
        vT = sbt(es, "vT", [128, 8, 512], BF16)
        print("CF sbuf bytes remaining", G.nc.sbuf_bytes_remaining)
        for blk in range(8):
            if blk % 4 == 0:
                for cc in range(8):
                    S.op("pool", lambda e: e.memset(ub[cc].t[:, 0:30], 0.0), writes=[ub[cc].b])
            build_xT(G, X, xin, blk)
            xT = X.xT
            def ag(cc):
                ba, bg = (PE_, PF) if cc % 2 == 0 else (PC, PD)
                for (bank, c0) in ((ba, cc * 128), (bg, 1024 + cc * 128)):
                    for ko in range(8):
                        S.op("pe", lambda e: e.matmul(bank.t[:], lhsT=pw1.t[:, ko, c0:c0 + 128], rhs=xT.t[:, ko, :], start=(ko == 0), stop=(ko == 7)),
                             reads=[pw1.b, xT.b], writes=[bank.b], sig=(ko == 7))

            def rest(cc):
                ba, bg = (PE_, PF) if cc % 2 == 0 else (PC, PD)
                S.op("act", lambda e: e.activation(out=sg.t[:], in_=bg.t[:], func=AF.Sigmoid), reads=[bg.b], writes=[sg.b])
                S.op("dve", lambda e: e.tensor_tensor(out=ub[cc].t[:, 30:542], in0=ba.t[:], in1=sg.t[:], op=ALU.mult),
                     reads=[ba.b, sg.b], writes=[ub[cc].b])
                for j in range(31):
                    S.op("pe", lambda e: e.matmul(PG.t[:], lhsT=diag.t[:, j * 8 + cc, :], rhs=ub[cc].t[:, j:j + 512], start=(j == 0), stop=(j == 30)),
                         reads=[diag.b, ub[cc].b], writes=[PG.b], sig=(j == 30))
                S.op("act", lambda e: e.activation(out=cv[cc].t[:], in_=PG.t[:], func=AF.Identity, bias=cfv.t[:, cc:cc + 1], scale=1.0),
                     reads=[PG.b, cfv.b], writes=[cv[cc].b])
                S.op("act", lambda e: e.activation(out=sq.t[:], in_=cv[cc].t[:], func=AF.Square), reads=[cv[cc].b], writes=[sq.b])
                S.op("pe", lambda e: e.matmul(PAB.t[:, 0:512], lhsT=onesm.t[:], rhs=cv[cc].t[:], start=(cc == 0), stop=(cc == 7)),
                     reads=[onesm.b, cv[cc].b], writes=[PAB.b])
                S.op("pe", lambda e: e.matmul(PAB.t[:, 512:1024], lhsT=onesm.t[:], rhs=sq.t[:], start=(cc == 0), stop=(cc == 7)),
                     reads=[onesm.b, sq.b], writes=[PAB.b])
                S.op("dve", lambda e: e.tensor_copy(out=ub[cc].t[:, 0:30], in_=ub[cc].t[:, 512:542]), reads=[ub[cc].b], writes=[ub[cc].b])

            ag(0)
            for cc in range(8):
                if cc + 1 < 8:
                    ag(cc + 1)
                rest(cc)
            S.op("act", lambda e: e.activation(out=m2.t[:], in_=PAB.t[:, 0:512], func=AF.Square), reads=[PAB.b], writes=[m2.b])
            S.op("dve", lambda e: e.tensor_tensor(out=m2.t[:], in0=PAB.t[:, 512:1024], in1=m2.t[:], op=ALU.subtract), reads=[PAB.b, m2.b], writes=[m2.b])
            S.op("act", lambda e: e.activation(out=m2.t[:], in_=m2.t[:], func=AF.Sqrt, bias=LN_EPS), reads=[m2.b], writes=[m2.b])
            S.op("dve", lambda e: e.reciprocal(out=rstd.t[:], in_=m2.t[:]), reads=[m2.b], writes=[rstd.b])
            S.op("dve", lambda e: e.scalar_tensor_tensor(out=nmr.t[:], in0=PAB.t[:, 0:512], scalar=-1.0, in1=rstd.t[:], op0=ALU.mult, op1=ALU.mult),
                 reads=[PAB.b, rstd.b], writes=[nmr.b])
            for cc in range(8):
                S.op("dve", lambda e: e.tensor_tensor(out=nn.t[:], in0=cv[cc].t[:], in1=rstd.t[:], op=ALU.mult), reads=[cv[cc].b, rstd.b], writes=[nn.b])
                S.op("dve", lambda e: e.tensor_tensor(out=nn.t[:], in0=nn.t[:], in1=nmr.t[:], op=ALU.add), reads=[nn.b, nmr.b], writes=[nn.b])
                S.op("dve", lambda e: e.tensor_scalar(out=nn.t[:], in0=nn.t[:], scalar1=cfv.t[:, 8 + cc:9 + cc], scalar2=cfv.t[:, 16 + cc:17 + cc],
                                                      op0=ALU.mult, op1=ALU.add), reads=[nn.b, cfv.b], writes=[nn.b])
                S.op("act", lambda e: e.activation(out=vT.t[:, cc, :], in_=nn.t[:], func=AF.Silu), reads=[nn.b], writes=[vT.b])
            rts = {}

            def outproj_epi(tl):
                i = blk * 4 + tl
                tc0 = tl * 128
                for half, bank in enumerate((PC, PD)):
                    for cq in range(8):
                        S.op("pe", lambda e: e.matmul(bank.t[:], lhsT=vT.t[:, cq, tc0:tc0 + 128], rhs=pw2.t[:, cq, half * 512:(half + 1) * 512],
                                                      start=(cq == 0), stop=(cq == 7)), reads=[vT.b, pw2.b], writes=[bank.b], sig=(cq == 7))

                def add_fn(r, xt):
                    S.op("dve", lambda e: e.scalar_tensor_tensor(out=r.t[:, 0:512], in0=xt.t[:, 0:512], scalar=ALPHA, in1=PC.t[:], op0=ALU.mult, op1=ALU.add),
                         reads=[xt.b, PC.b], writes=[r.b])
                    S.op("dve", lambda e: e.scalar_tensor_tensor(out=r.t[:, 512:1024], in0=xt.t[:, 512:1024], scalar=ALPHA, in1=PD.t[:], op0=ALU.mult, op1=ALU.add),
                         reads=[xt.b, PD.b, r.b], writes=[r.b])
                rts[tl] = G.epilogue(E, i, xin, xout, add_fn)

            outproj_epi(0)
            for tl in range(4):
                if tl + 1 < 4:
                    outproj_epi(tl + 1)
                G.route_tile(R, blk * 4 + tl, rts[tl])
    S.barrier()


def host_weights(inp, stages):
    f = np.ascontiguousarray
    m = {}
    if "ab" in stages:
        m["ab_w_in"] = f(inp["ab_w_in"][0])
        m["ab_w_out"] = f(inp["ab_w_out"][0])
        m["ab_bif"] = f(np.concatenate([inp["ab_b_igate"][0], inp["ab_b_fgate"][0]]))
        m["ab_conv"] = f(inp["ab_conv_w"][0].reshape(3, 4, 128).transpose(2, 0, 1).reshape(128, 12))
        m["ab_hg"] = f(inp["ab_head_gain"][0])
    if "cf" in stages:
        m["cf_w_pw1"] = f(inp["cf_w_pw1"][0])
        m["cf_w_pw2"] = f(inp["cf_w_pw2"][0])
        m["cf_dw"] = f(inp["cf_w_dw"][0].reshape(31, 8, 128).transpose(2, 0, 1).reshape(128, 248))
        vec = np.stack([inp["cf_b_dw"][0], inp["cf_ln_g"][0], inp["cf_ln_b"][0]])
        m["cf_vec"] = f(vec.reshape(3, 8, 128).transpose(2, 0, 1).reshape(128, 24))
    m["router_w"] = f(inp["router_w"])
    m["router_b"] = f(inp["router_b"])
    m["exp_w_up"] = f(inp["exp_w_up"])
    m["exp_w_down"] = f(inp["exp_w_down"])
    m["bup"] = f(inp["exp_b_up"].reshape(2, NE, 16, 128).transpose(3, 0, 1, 2).reshape(128, 2 * NE * 16))
    m["exp_b_down"] = f(inp["exp_b_down"])
    m["post_ln_g"] = f(inp["post_ln_g"])
    m["post_ln_b"] = f(inp["post_ln_b"])
    return m


def kernel(**inputs):
    inp = {k: np.asarray(v) for k, v in inputs.items()}
    stages = ("ab", "moe0", "cf", "moe1")
    nc = build_program(stages)
    wm = host_weights(inp, stages)
    x = inp["x"].reshape(8, NTOK, D)
    in_maps = []
    for c in range(8):
        m = dict(wm)
        m["x"] = np.ascontiguousarray(x[c])
        in_maps.append(m)
    res = run_bass_kernel_spmd(nc, in_maps, core_ids=list(range(8)))
    out = np.stack([np.asarray(r["out"]) for r in res.results], axis=0)
    return out.reshape(16, SEQ, D).astype(np.float32)
```

```python
import contextlib
import math
import os
import numpy as np
import concourse.bass as bass
import concourse.mybir as mybir
from concourse.bass_utils import run_bass_kernel_spmd
from concourse.alu_op_type import AluOpType as ALU

F32 = mybir.dt.float32
BF16 = mybir.dt.bfloat16
I32 = mybir.dt.int32
U32 = mybir.dt.uint32
AF = mybir.ActivationFunctionType

D = 1024
NTOK = 4096
NTILE = 32
SEQ = 2048
NE = 32
CAP = 640
NTC = CAP // 128
NSLOT = NE * CAP
ALPHA = 4.0 ** 0.25
LN_EPS = 1e-5
HN_EPS = 1e-6


class Buf:
    __slots__ = ("name", "writer", "readers", "sem", "dcount")

    def __init__(self, name):
        self.name = name
        self.writer = None
        self.readers = []
        self.sem = None
        self.dcount = 0


class T:
    __slots__ = ("t", "b")

    def __init__(self, t, b):
        self.t = t
        self.b = b


class Sched:
    def __init__(self, nc):
        self.nc = nc
        self.engs = {"pe": nc.tensor, "act": nc.scalar, "dve": nc.vector, "pool": nc.gpsimd, "sp": nc.sync}
        self.sems = {}
        self.count = {k: 0 for k in self.engs}
        self.seen = {k: {} for k in self.engs}
        self._ctx = []
        self.nsem = 0
        self.dma_bufs = []
        for k in self.engs:
            self.sems[k] = self.new_sem("s_" + k)

    def new_sem(self, name):
        cm = self.nc.semaphore("%s_%d" % (name, self.nsem))
        s = cm.__enter__()
        self._ctx.append(cm)
        self.nsem += 1
        return s

    def close(self):
        for cm in reversed(self._ctx):
            cm.__exit__(None, None, None)

    def _wait(self, eng, tok):
        if tok is None:
            return
        sem, val, pe = tok
        key = id(sem)
        if self.seen[eng].get(key, 0) >= val:
            return
        if pe == "pe" and eng == "pe":
            return
        self.engs[eng].wait_ge(sem, val)
        self.seen[eng][key] = val

    def _deps(self, eng, reads, writes, is_dma=False, after=()):
        toks = list(after)
        for b in reads:
            toks.append(b.writer)
        for b in writes:
            if b.writer is not None and not (is_dma and b.writer[2] == "dma"):
                toks.append(b.writer)
            toks.extend(b.readers)
        best = {}
        for t in toks:
            if t is None:
                continue
            k = id(t[0])
            if k not in best or best[k][1] < t[1]:
                best[k] = t
        for t in best.values():
            self._wait(eng, t)

    def _commit(self, tok, reads, writes):
        for b in writes:
            b.writer = tok
            b.readers = []
        for b in reads:
            b.readers.append(tok)
            if len(b.readers) > 16:
                last = {}
                for r in b.readers:
                    last[id(r[0])] = r
                b.readers = list(last.values())

    def op(self, eng, fn, reads=(), writes=(), after=(), sig=True):
        self._deps(eng, reads, writes, after=after)
        inst = fn(self.engs[eng])
        if not sig:
            return None
        self.count[eng] += 1
        inst.then_inc(self.sems[eng], 1)
        tok = (self.sems[eng], self.count[eng], eng)
        self._commit(tok, reads, writes)
        return tok

    def dma(self, q, fn, reads=(), writes=(), after=(), sembuf=None):
        sb = sembuf if sembuf is not None else writes[0]
        if sb.sem is None:
            sb.sem = self.new_sem("d_" + sb.name)
            self.dma_bufs.append(sb)
        self._deps(q, reads, writes, is_dma=True, after=after)
        inst = fn(self.engs[q])
        sb.dcount += 16
        inst.then_inc(sb.sem, 16)
        tok = (sb.sem, sb.dcount, "dma")
        self._commit(tok, reads, writes)
        return tok

    def barrier(self, engines=None):
        for e in (engines or self.engs):
            for k in self.engs:
                if k != e and self.count[k] > 0:
                    self._wait(e, (self.sems[k], self.count[k], k))
            for b in self.dma_bufs:
                if b.dcount > 0:
                    self._wait(e, (b.sem, b.dcount, "dma"))


class Ctx:
    pass


def build_program(stages=("ab", "moe0", "cf", "moe1"), debug=False):
    nc = bass.Bass("TRN2", target_bir_lowering=False)
    S = Sched(nc)
    G = Ctx()
    es_top = contextlib.ExitStack()

    def din(name, shape, dt=F32):
        return nc.dram_tensor(name, list(shape), dt, kind="ExternalInput").ap()

    def dscratch(name, shape, dt=F32, out=False):
        kind = "ExternalOutput" if out else "Internal"
        return nc.dram_tensor(name, list(shape), dt, kind=kind).ap()

    first = stages[0]
    x0 = din("x", [NTOK, D]) if first == "ab" else None
    W = {}
    if "ab" in stages:
        W["w_in"] = din("ab_w_in", [D, 3080])
        W["w_out"] = din("ab_w_out", [D, D])
        W["ab_bif"] = din("ab_bif", [8])
        W["ab_conv"] = din("ab_conv", [128, 12])
        W["ab_hg"] = din("ab_hg", [512])
    if "cf" in stages:
        W["pw1"] = din("cf_w_pw1", [D, 2048])
        W["pw2"] = din("cf_w_pw2", [D, D])
        W["cf_dw"] = din("cf_dw", [128, 31 * 8])
        W["cf_vec"] = din("cf_vec", [128, 24])
    W["router_w"] = din("router_w", [2, D, NE])
    W["router_b"] = din("router_b", [2, NE])
    W["up"] = din("exp_w_up", [2, NE, D, 2 * D])
    W["dn"] = din("exp_w_down", [2, NE, D, D])
    W["bup"] = din("bup", [128, 2 * NE * 16])
    W["bdn"] = din("exp_b_down", [2, NE, D])
    W["lng"] = din("post_ln_g", [2, 2, D])
    W["lnb"] = din("post_ln_b", [2, 2, D])

    last = stages[-1]
    xs = {}
    names = {"ab": "x1", "moe0": "x2", "cf": "x3", "moe1": "out"}
    for st in ("ab", "moe0", "cf", "moe1"):
        nm = names[st]
        if st in stages:
            xs[nm] = T(dscratch(nm, [NTOK, D], F32, out=(debug or st == last)), Buf(nm))
    prev = {"moe0": "x1", "cf": "x2", "moe1": "x3", "route0": "x1", "route1": "x3"}
    if first != "ab":
        nm = prev[first]
        xs[nm] = T(din(nm, [NTOK, D]), Buf(nm))
    else:
        xs["x0"] = T(x0, Buf("x0"))
    xg = T(dscratch("xg", [NSLOT, D], BF16, out=debug), Buf("xg"))
    yg = T(dscratch("yg", [NSLOT, D], F32, out=debug), Buf("yg"))

    uid = [0]

    def sbt(es, name, shape, dt):
        uid[0] += 1
        name = "sb%d_%s" % (uid[0], name)
        return T(es.enter_context(nc.sbuf_tensor(name, list(shape), dt)), Buf(name))

    def pst(es, name, shape, dt):
        uid[0] += 1
        name = "ps%d_%s" % (uid[0], name)
        return T(es.enter_context(nc.psum_tensor(name, list(shape), dt)), Buf(name))

    ident_f = sbt(es_top, "ident_f", [128, 128], F32)
    ident_b = sbt(es_top, "ident_b", [128, 128], BF16)
    uincl_f = sbt(es_top, "uincl_f", [128, 128], F32)
    ustr_b = sbt(es_top, "ustr_b", [128, 128], BF16)
    ones_f = sbt(es_top, "ones_f", [128, 128], F32)
    ones_b = sbt(es_top, "ones_b", [128, 128], BF16)
    iota_e = sbt(es_top, "iota_e", [128, NE], F32)
    ecap = sbt(es_top, "ecap", [128, NE], F32)
    dest_i = sbt(es_top, "dest_i", [128, NTILE * 4], I32)
    gate_t = sbt(es_top, "gate_t", [128, NTILE * 4], F32)
    dest_b = [Buf("dest%d" % i) for i in range(NTILE)]
    gate_b = [Buf("gate%d" % i) for i in range(NTILE)]
    cbase = sbt(es_top, "cbase", [128, NE], F32)
    zero_b = sbt(es_top, "zero_b", [128, 1024], BF16)
    tmpc = sbt(es_top, "tmpc", [128, 128], F32)

    def consts():
        S.op("pool", lambda e: e.memset(ones_f.t[:], 1.0), writes=[ones_f.b])
        S.op("pool", lambda e: e.memset(ones_b.t[:], 1.0), writes=[ones_b.b])
        S.op("pool", lambda e: e.memset(zero_b.t[:], 0.0), writes=[zero_b.b])
        S.op("pool", lambda e: e.affine_select(out=ident_f.t[:], in_=ones_f.t[:], pattern=[[-1, 128]],
                                                compare_op=ALU.is_equal, fill=0.0, base=0, channel_multiplier=1),
             reads=[ones_f.b], writes=[ident_f.b])
        S.op("dve", lambda e: e.tensor_copy(out=ident_b.t[:], in_=ident_f.t[:]), reads=[ident_f.b], writes=[ident_b.b])
        S.op("pool", lambda e: e.affine_select(out=uincl_f.t[:], in_=ones_f.t[:], pattern=[[1, 128]],
                                                compare_op=ALU.is_ge, fill=0.0, base=0, channel_multiplier=-1),
             reads=[ones_f.b], writes=[uincl_f.b])
        S.op("pool", lambda e: e.affine_select(out=tmpc.t[:], in_=ones_f.t[:], pattern=[[1, 128]],
                                                compare_op=ALU.is_gt, fill=0.0, base=0, channel_multiplier=-1),
             reads=[ones_f.b], writes=[tmpc.b])
        S.op("dve", lambda e: e.tensor_copy(out=ustr_b.t[:], in_=tmpc.t[:]), reads=[tmpc.b], writes=[ustr_b.b])
        S.op("pool", lambda e: e.iota(iota_e.t[:], pattern=[[1, NE]], base=0, channel_multiplier=0,
                                      allow_small_or_imprecise_dtypes=True), writes=[iota_e.b])
        S.op("dve", lambda e: e.tensor_scalar(out=ecap.t[:], in0=iota_e.t[:], scalar1=float(CAP), scalar2=None,
                                              op0=ALU.mult), reads=[iota_e.b], writes=[ecap.b])

    consts()
    breg = nc.gpsimd.to_reg(NSLOT - 1)
    zf = None
    for i in range(NSLOT // 128):
        zf = S.dma("sp", lambda e, i=i: e.dma_start(
            out=xg.t[i * 128:(i + 1) * 128, :], in_=zero_b.t[:]),
            reads=[zero_b.b], writes=[xg.b])
    G.xg_zero_tok = zf

    def make_epi(es, layer, which, nbuf=2):
        E = Ctx()
        E.lng = sbt(es, "lng%d%d" % (layer, which), [128, D], F32)
        E.lnb = sbt(es, "lnb%d%d" % (layer, which), [128, D], F32)
        S.dma("sp", lambda e: e.dma_start(out=E.lng.t[:], in_=W["lng"][layer, which].partition_broadcast(128)), writes=[E.lng.b])
        S.dma("sp", lambda e: e.dma_start(out=E.lnb.t[:], in_=W["lnb"][layer, which].partition_broadcast(128)), writes=[E.lnb.b])
        E.xt = [sbt(es, "epx%d%d_%d" % (layer, which, i), [128, D], F32) for i in range(nbuf)]
        E.r = [sbt(es, "epr%d%d_%d" % (layer, which, i), [128, D], F32) for i in range(nbuf)]
        E.nbuf = nbuf
        E.sts = [sbt(es, "epst%d%d" % (layer, which), [128, 12], F32) for i in range(nbuf)]
        E.mvs = [sbt(es, "epmv%d%d" % (layer, which), [128, 2], F32) for i in range(nbuf)]
        E.eps = sbt(es, "epeps%d%d" % (layer, which), [128, 1], F32)
        S.op("dve", lambda e: e.memset(E.eps.t[:], LN_EPS), writes=[E.eps.b])
        return E

    def epi_load(E, i, xin, slot=None):
        sl = (i if slot is None else slot) % E.nbuf
        xt = E.xt[sl]
        S.dma("sp", lambda e: e.dma_start(out=xt.t[:], in_=xin.t[i * 128:(i + 1) * 128, :]), reads=[xin.b], writes=[xt.b])

    def epilogue(E, i, xin, xout, add_fn, slot=None, preloaded=False, store_q="pool"):
        sl = (i if slot is None else slot) % E.nbuf
        xt = E.xt[sl]
        r = E.r[sl]
        est, emv = E.sts[sl], E.mvs[sl]
        if not preloaded:
            S.dma("sp", lambda e: e.dma_start(out=xt.t[:], in_=xin.t[i * 128:(i + 1) * 128, :]), reads=[xin.b], writes=[xt.b])
        src = add_fn(r, xt)
        if src is None:
            src, h0, h1, flat = r, r.t[:, 0:512], r.t[:, 512:1024], r.t[:]
        else:
            h0, h1, flat = src.t[:, 0, :], src.t[:, 1, :], src.t[:].rearrange("p a b -> p (a b)")
        S.op("dve", lambda e: e.bn_stats(out=est.t[:, 0:6], in_=h0), reads=[src.b], writes=[est.b])
        S.op("dve", lambda e: e.bn_stats(out=est.t[:, 6:12], in_=h1), reads=[src.b], writes=[est.b])
        S.op("dve", lambda e: e.bn_aggr(out=emv.t[:], in_=est.t[:]), reads=[est.b], writes=[emv.b])
        S.op("act", lambda e: e.activation(out=emv.t[:, 1:2], in_=emv.t[:, 1:2], func=AF.Ln, bias=E.eps.t[:, 0:1], scale=1.0),
             reads=[emv.b, E.eps.b], writes=[emv.b])
        S.op("act", lambda e: e.activation(out=emv.t[:, 1:2], in_=emv.t[:, 1:2], func=AF.Exp, scale=-0.5),
             reads=[emv.b], writes=[emv.b])
        S.op("dve", lambda e: e.tensor_scalar(out=r.t[:], in0=flat, scalar1=emv.t[:, 0:1], scalar2=emv.t[:, 1:2],
                                              op0=ALU.subtract, op1=ALU.mult), reads=[src.b, r.b, emv.b], writes=[r.b])
        S.op("dve", lambda e: e.tensor_tensor(out=r.t[:], in0=r.t[:], in1=E.lng.t[:], op=ALU.mult),
             reads=[r.b, E.lng.b], writes=[r.b])
        S.op("dve", lambda e: e.tensor_tensor(out=r.t[:], in0=r.t[:], in1=E.lnb.t[:], op=ALU.add),
             reads=[r.b, E.lnb.b], writes=[r.b])
        S.dma(store_q, lambda e: e.dma_start(out=xout.t[i * 128:(i + 1) * 128, :], in_=r.t[:]), reads=[r.b], writes=[xout.b], sembuf=r.b)
        return r

    def make_router(es, layer, pbanks, ptr_f, nbuf=2):
        R = Ctx()
        R.rw = sbt(es, "rw%d" % layer, [128, 8, NE], F32)
        R.rb = sbt(es, "rb%d" % layer, [128, NE], F32)
        S.dma("sp", lambda e: e.dma_start(out=R.rw.t[:], in_=W["router_w"][layer].rearrange("(ko p) n -> p ko n", p=128)),
              writes=[R.rw.b])
        S.dma("sp", lambda e: e.dma_start(out=R.rb.t[:], in_=W["router_b"][layer].partition_broadcast(128)), writes=[R.rb.b])
        S.op("dve", lambda e: e.memset(cbase.t[:], 0.0), writes=[cbase.b])
        R.xT = sbt(es, "rxT%d" % layer, [128, 8, 128], F32)
        R.xb = [sbt(es, "rxb%d_%d" % (layer, i), [128, D], BF16) for i in range(nbuf)]
        R.nbuf = nbuf
        R.lg = sbt(es, "rlg%d" % layer, [128, NE], F32)
        R.v8 = sbt(es, "rv8%d" % layer, [128, 8], F32)
        R.i8 = sbt(es, "ri8%d" % layer, [128, 8], U32)
        R.i8f = sbt(es, "ri8f%d" % layer, [128, 8], F32)
        R.mk = sbt(es, "rmk%d" % layer, [128, 4, NE], F32)
        R.msum = sbt(es, "rms%d" % layer, [128, NE], F32)
        R.msb = sbt(es, "rmsb%d" % layer, [128, NE], BF16)
        R.pos = sbt(es, "rpos%d" % layer, [128, NE], F32)
        R.junk = sbt(es, "rjk%d" % layer, [128, NE], F32)
        R.dst = sbt(es, "rdst%d" % layer, [128, 4], F32)
        R.val = sbt(es, "rval%d" % layer, [128, 4], F32)
        R.ex = sbt(es, "rex%d" % layer, [128, 4], F32)
        R.nv0 = sbt(es, "rnv%d" % layer, [128, 1], F32)
        R.ssum = sbt(es, "rss%d" % layer, [128, 1], F32)
        R.pT = ptr_f
        R.pS = pbanks
        return R

    def route_tile(R, i, r):
        for ko in range(8):
            S.op("pe", lambda e, ko=ko: e.transpose(R.pT.t[:, ko * 128:(ko + 1) * 128], r.t[:, ko * 128:(ko + 1) * 128], ident_f.t[:]),
                 reads=[r.b, ident_f.b], writes=[R.pT.b], sig=(ko == 7))
        S.op("act", lambda e: e.copy(out=R.xT.t[:].rearrange("p a b -> p (a b)"), in_=R.pT.t[:]), reads=[R.pT.b], writes=[R.xT.b])
        for ko in range(8):
            S.op("pe", lambda e, ko=ko: e.matmul(R.pS.t[:, 0:NE], lhsT=R.xT.t[:, ko, :], rhs=R.rw.t[:, ko, :], start=(ko == 0), stop=(ko == 7)),
                 reads=[R.xT.b, R.rw.b], writes=[R.pS.b], sig=(ko == 7))
        S.op("dve", lambda e: e.tensor_tensor(out=R.lg.t[:], in0=R.pS.t[:, 0:NE], in1=R.rb.t[:], op=ALU.add),
             reads=[R.pS.b, R.rb.b], writes=[R.lg.b])
        S.op("dve", lambda e: e.max(out=R.v8.t[:], in_=R.lg.t[:]), reads=[R.lg.b], writes=[R.v8.b])
        S.op("dve", lambda e: e.max_index(out=R.i8.t[:], in_max=R.v8.t[:], in_values=R.lg.t[:]), reads=[R.lg.b, R.v8.b], writes=[R.i8.b])
        S.op("dve", lambda e: e.tensor_copy(out=R.i8f.t[:], in_=R.i8.t[:]), reads=[R.i8.b], writes=[R.i8f.b])
        S.op("dve", lambda e: e.tensor_tensor(out=R.mk.t[:], in0=iota_e.t[:].unsqueeze(1).to_broadcast([128, 4, NE]),
                                              in1=R.i8f.t[:, 0:4].unsqueeze(2).to_broadcast([128, 4, NE]), op=ALU.is_equal),
             reads=[iota_e.b, R.i8f.b], writes=[R.mk.b])
        S.op("dve", lambda e: e.tensor_reduce(out=R.msum.t[:], in_=R.mk.t[:].rearrange("p k e -> p e k"), axis=mybir.AxisListType.X, op=ALU.add),
             reads=[R.mk.b], writes=[R.msum.b])
        S.op("dve", lambda e: e.tensor_copy(out=R.msb.t[:], in_=R.msum.t[:]), reads=[R.msum.b], writes=[R.msb.b])
        S.op("pe", lambda e: e.matmul(R.pS.t[:, 64:64 + NE], lhsT=ustr_b.t[:], rhs=R.msb.t[:], start=True, stop=True),
             reads=[ustr_b.b, R.msb.b, R.lg.b], writes=[R.pS.b], sig=False)
        S.op("pe", lambda e: e.matmul(R.pS.t[:, 128:128 + NE], lhsT=ones_b.t[:], rhs=R.msb.t[:], start=True, stop=True),
             reads=[ones_b.b, R.msb.b, R.lg.b], writes=[R.pS.b])
        S.op("dve", lambda e: e.tensor_tensor(out=R.pos.t[:], in0=R.pS.t[:, 64:64 + NE], in1=cbase.t[:], op=ALU.add),
             reads=[R.pS.b, cbase.b], writes=[R.pos.b])
        S.op("dve", lambda e: e.tensor_tensor(out=cbase.t[:], in0=R.pS.t[:, 128:128 + NE], in1=cbase.t[:], op=ALU.add),
             reads=[R.pS.b, cbase.b], writes=[cbase.b])
        S.op("dve", lambda e: e.tensor_scalar(out=R.junk.t[:], in0=R.pos.t[:], scalar1=float(CAP), scalar2=1.0e6,
                                              op0=ALU.is_ge, op1=ALU.mult), reads=[R.pos.b], writes=[R.junk.b])
        S.op("dve", lambda e: e.tensor_tensor(out=R.pos.t[:], in0=R.pos.t[:], in1=R.junk.t[:], op=ALU.add),
             reads=[R.pos.b, R.junk.b], writes=[R.pos.b])
        S.op("dve", lambda e: e.tensor_tensor(out=R.pos.t[:], in0=R.pos.t[:], in1=ecap.t[:], op=ALU.add),
             reads=[R.pos.b, ecap.b], writes=[R.pos.b])
        S.op("dve", lambda e: e.tensor_tensor(out=R.mk.t[:], in0=R.mk.t[:], in1=R.pos.t[:].unsqueeze(1).to_broadcast([128, 4, NE]), op=ALU.mult),
             reads=[R.mk.b, R.pos.b], writes=[R.mk.b])
        S.op("dve", lambda e: e.tensor_reduce(out=R.dst.t[:], in_=R.mk.t[:], axis=mybir.AxisListType.X, op=ALU.add),
             reads=[R.mk.b], writes=[R.dst.b])
        S.op("dve", lambda e: e.tensor_copy(out=dest_i.t[:, i * 4:(i + 1) * 4], in_=R.dst.t[:]), reads=[R.dst.b], writes=[dest_b[i]])
        S.op("dve", lambda e: e.tensor_scalar(out=R.nv0.t[:], in0=R.v8.t[:, 0:1], scalar1=-1.0, scalar2=None, op0=ALU.mult),
             reads=[R.v8.b], writes=[R.nv0.b])
        S.op("act", lambda e: e.activation(out=R.ex.t[:], in_=R.v8.t[:, 0:4], func=AF.Exp, bias=R.nv0.t[:, 0:1], scale=1.0),
             reads=[R.v8.b, R.nv0.b], writes=[R.ex.b])
        S.op("dve", lambda e: e.reduce_sum(out=R.ssum.t[:], in_=R.ex.t[:], axis=mybir.AxisListType.X), reads=[R.ex.b], writes=[R.ssum.b])
        S.op("dve", lambda e: e.reciprocal(out=R.ssum.t[:], in_=R.ssum.t[:]), reads=[R.ssum.b], writes=[R.ssum.b])
        S.op("dve", lambda e: e.tensor_scalar(out=R.val.t[:], in0=R.dst.t[:], scalar1=float(NSLOT), scalar2=None, op0=ALU.is_lt),
             reads=[R.dst.b], writes=[R.val.b])
        S.op("dve", lambda e: e.tensor_scalar(out=R.ex.t[:], in0=R.ex.t[:], scalar1=R.ssum.t[:, 0:1], scalar2=None, op0=ALU.mult),
             reads=[R.ex.b, R.ssum.b], writes=[R.ex.b])
        S.op("dve", lambda e: e.tensor_tensor(out=gate_t.t[:, i * 4:(i + 1) * 4], in0=R.ex.t[:], in1=R.val.t[:], op=ALU.mult),
             reads=[R.ex.b, R.val.b], writes=[gate_b[i]])
        xb = R.xb[i % R.nbuf]
        S.op("act", lambda e: e.copy(out=xb.t[:], in_=r.t[:]), reads=[r.b], writes=[xb.b])
        for k in range(4):
            S.dma("pool", lambda e, k=k: e.indirect_dma_start(
                out=xg.t[:, :], out_offset=bass.IndirectOffsetOnAxis(ap=dest_i.t[:, i * 4 + k:i * 4 + k + 1], axis=0),
                in_=xb.t[:, :], in_offset=None, bounds_check=breg, oob_is_err=False),
                reads=[xb.b, dest_b[i]], writes=[xg.b], after=[G.xg_zero_tok], sembuf=xb.b)

    def moe_phase(layer, xin, xout):
        with contextlib.ExitStack() as es:
            NR = 10
            ring = [sbt(es, "ring%d" % i, [128, 8, 512], BF16) for i in range(NR)]
            stg = [sbt(es, "stg%d" % i, [128, 2, 512], F32) for i in range(8)]
            xl = [sbt(es, "xl%d" % i, [128, NTC, D], BF16) for i in range(2)]
            xgT = [sbt(es, "xgT%d" % i, [128, 8, CAP], BF16) for i in range(2)]
            actT = sbt(es, "actT", [128, 8, CAP], BF16)
            gsb = [sbt(es, "gsb%d" % i, [128, CAP], F32) for i in range(2)]
            sgb = [sbt(es, "sgb%d" % i, [128, CAP], F32) for i in range(2)]
            t1b = [sbt(es, "t1b%d" % i, [128, CAP], F32) for i in range(2)]
            ysb = [sbt(es, "ysb%d" % i, [128, D], F32) for i in range(2)]
            bdn = [sbt(es, "bdn%d" % i, [128, D], F32) for i in range(2)]
            bup = sbt(es, "bup", [128, NE * 16], F32)
            bup1 = sbt(es, "bup1", [128, NE * 16], F32)
            pg = pst(es, "pg", [128, 2, 512], F32)
            pl = pst(es, "pl", [128, 2, 512], F32)
            py = pst(es, "py", [128, 2, 512], F32)
            ptrs = [pst(es, "ptr%d" % i, [128, 8, 128], BF16) for i in range(2)]
            pyb = [Buf("py_h0"), Buf("py_h1")]

            S.dma("sp", lambda e: e.dma_start(out=bup.t[:], in_=W["bup"][:, layer * NE * 16:(layer + 1) * NE * 16]), writes=[bup.b])
            S.op("dve", lambda e: e.tensor_scalar(out=bup1.t[:], in0=bup.t[:], scalar1=1.0, scalar2=None, op0=ALU.add),
                 reads=[bup.b], writes=[bup1.b])

            corder = [("up", 0), ("up", 2), ("up", 1), ("up", 3), ("dn", 0), ("dn", 1)]
            nchunks = NE * 6
            pcount = [0]

            cast_q = []
            step = [0]

            def emit_casts(pred, limit=99):
                n = 0
                k = 0
                while k < len(cast_q) and n < limit:
                    if pred(cast_q[k]):
                        cast_q.pop(k)[3]()
                        n += 1
                    else:
                        break

            def tick():
                step[0] += 1
                emit_casts(lambda c: c[0] <= step[0], limit=2)

            def flush_chunk(j):
                emit_casts(lambda c: c[1] <= j)

            def load_chunk(j):
                if j >= nchunks:
                    return
                e_, c = divmod(j, 6)
                kind, gi = corder[c]
                src = W["up"][layer, e_] if kind == "up" else W["dn"][layer, e_]
                slot = ring[j % NR]
                for q in range(4):
                    si = pcount[0] % len(stg)
                    st = stg[si]
                    pcount[0] += 1
                    while any(c[2] == si for c in cast_q):
                        cast_q.pop(0)[3]()
                    S.dma("sp", lambda e: e.dma_start(
                        out=st.t[:], in_=src[q * 256:(q + 1) * 256, gi * 512:(gi + 1) * 512].rearrange("(ko p) f -> p ko f", p=128)),
                        writes=[st.b])

                    def emit(st=st, q=q, slot=slot):
                        S.op("act", lambda e: e.copy(out=slot.t[:, q * 2:(q + 1) * 2, :], in_=st.t[:]), reads=[st.b], writes=[slot.b])
                    cast_q.append([step[0] + 2 + q // 2, j, si, emit])

            def load_x(e_):
                if e_ >= NE:
                    return
                S.dma("sp", lambda e: e.dma_start(out=xl[e_ % 2].t[:], in_=xg.t[e_ * CAP:(e_ + 1) * CAP, :].rearrange("(t p) d -> p t d", p=128)),
                      reads=[xg.b], writes=[xl[e_ % 2].b])

            def load_bdn(e_):
                if e_ >= NE:
                    return
                S.dma("sp", lambda e: e.dma_start(out=bdn[e_ % 2].t[:], in_=W["bdn"][layer, e_].partition_broadcast(128)),
                      writes=[bdn[e_ % 2].b])

            def trans_tile(e_, tt):
                if e_ >= NE:
                    return
                src = xl[e_ % 2]
                dst = xgT[e_ % 2]
                ptr = ptrs[tt % 2]
                for ko in range(8):
                    S.op("pe", lambda e, ko=ko: e.transpose(ptr.t[:, ko, :], src.t[:, tt, ko * 128:(ko + 1) * 128], ident_b.t[:]),
                         reads=[src.b, ident_b.b], writes=[ptr.b], sig=(ko == 7))
                S.op("act", lambda e: e.copy(out=dst.t[:, :, tt * 128:(tt + 1) * 128], in_=ptr.t[:]),
                     reads=[ptr.b], writes=[dst.b])

            def transposes(e_):
                for tt in range(NTC):
                    trans_tile(e_, tt)

            def down_tile(e_, tt):
                base = e_ * 6
                wd = [ring[(base + 4) % NR], ring[(base + 5) % NR]]
                y_ = ysb[tt % 2]
                for half in range(2):
                    for fo in range(8):
                        S.op("pe", lambda e, fo=fo: e.matmul(
                            py.t[:, half, :], lhsT=actT.t[:, fo, tt * 128:(tt + 1) * 128], rhs=wd[half].t[:, fo, :],
                            start=(fo == 0), stop=(fo == 7)),
                            reads=[actT.b, wd[half].b], writes=[pyb[half]], sig=(fo == 7))
                    S.op("dve", lambda e: e.tensor_tensor(out=y_.t[:, half * 512:(half + 1) * 512], in0=py.t[:, half, :],
                                                          in1=bdn[e_ % 2].t[:, half * 512:(half + 1) * 512], op=ALU.add),
                         reads=[pyb[half], bdn[e_ % 2].b] + ([y_.b] if half else []), writes=[y_.b])
                S.dma("pool", lambda e: e.dma_start(out=yg.t[e_ * CAP + tt * 128:e_ * CAP + (tt + 1) * 128, :], in_=y_.t[:]),
                      reads=[y_.b], writes=[yg.b], sembuf=y_.b)

            for j in range(NR):
                load_chunk(j)
                flush_chunk(j)
            load_x(0)
            load_bdn(0)
            transposes(0)
            load_x(1)
            load_bdn(1)
            HN = CAP // 2
            for e_ in range(NE):
                xT = xgT[e_ % 2]
                base = e_ * 6
                flush_chunk(base + 1)
                for j in range(8):
                    if j == 4:
                        flush_chunk(base + 3)
                    tick()
                    cg = ring[(base + (0 if j < 4 else 2)) % NR]
                    cl = ring[(base + (1 if j < 4 else 3)) % NR]
                    col = (j % 4) * 128
                    for (pp, cw) in ((pg, cg), (pl, cl)):
                        for half in range(2):
                            for ko in range(8):
                                S.op("pe", lambda e, pp=pp, cw=cw, half=half, ko=ko: e.matmul(
                                    pp.t[:, half, 0:HN], lhsT=cw.t[:, ko, col:col + 128], rhs=xT.t[:, ko, half * HN:(half + 1) * HN],
                                    start=(ko == 0), stop=(ko == 7)),
                                    reads=[cw.b, xT.b], writes=[pp.b], sig=(half == 1 and ko == 7))
                    g_ = gsb[j % 2]
                    s_ = sgb[j % 2]
                    t_ = t1b[j % 2]
                    bcol = e_ * 16 + j
                    S.op("dve", lambda e, g_=g_, bcol=bcol: e.tensor_scalar(
                        out=g_.t[:].rearrange("p (a b) -> p a b", a=2), in0=pg.t[:, :, 0:HN], scalar1=bup.t[:, bcol:bcol + 1], scalar2=7.0,
                        op0=ALU.add, op1=ALU.min), reads=[pg.b, bup.b], writes=[g_.b])
                    S.op("act", lambda e, g_=g_, s_=s_: e.activation(out=s_.t[:], in_=g_.t[:], func=AF.Sigmoid, scale=1.702),
                         reads=[g_.b], writes=[s_.b])
                    S.op("dve", lambda e, t_=t_, bcol=bcol: e.tensor_scalar(
                        out=t_.t[:].rearrange("p (a b) -> p a b", a=2), in0=pl.t[:, :, 0:HN], scalar1=bup1.t[:, bcol + 8:bcol + 9], scalar2=-6.0,
                        op0=ALU.add, op1=ALU.max), reads=[pl.b, bup1.b], writes=[t_.b])
                    S.op("dve", lambda e, t_=t_, g_=g_: e.scalar_tensor_tensor(out=t_.t[:], in0=t_.t[:], scalar=8.0, in1=g_.t[:],
                                                                         op0=ALU.min, op1=ALU.mult), reads=[t_.b, g_.b], writes=[t_.b])
                    S.op("dve", lambda e, t_=t_, s_=s_, j=j: e.tensor_tensor(out=actT.t[:, j, :], in0=t_.t[:], in1=s_.t[:], op=ALU.mult),
                         reads=[t_.b, s_.b], writes=[actT.b])
                    if j == 3:
                        load_chunk(base + 0 + NR)
                        load_chunk(base + 1 + NR)
                    if j == 7:
                        load_chunk(base + 2 + NR)
                        load_chunk(base + 3 + NR)
                flush_chunk(base + 5)
                trans_tile(e_ + 1, 0)
                trans_tile(e_ + 1, 1)
                tick()
                down_tile(e_, 0)
                trans_tile(e_ + 1, 2)
                tick()
                down_tile(e_, 1)
                trans_tile(e_ + 1, 3)
                tick()
                down_tile(e_, 2)
                trans_tile(e_ + 1, 4)
                tick()
                down_tile(e_, 3)
                tick()
                down_tile(e_, 4)
                load_x(e_ + 2)
                load_chunk(base + 4 + NR)
                load_chunk(base + 5 + NR)
                load_bdn(e_ + 2)
            flush_chunk(nchunks)
        S.barrier()
        with contextlib.ExitStack() as es:
            E = make_epi(es, layer, 1)
            yk = [[sbt(es, "yk%d_%d" % (b, k), [128, D], F32) for k in range(4)] for b in range(2)]
            dg = [[sbt(es, "dg%d_%d" % (b, k), [128, 128], F32) for k in range(4)] for b in range(2)]
            dal = sbt(es, "dal", [128, 128], F32)
            S.op("dve", lambda e: e.tensor_scalar(out=dal.t[:], in0=ident_f.t[:], scalar1=ALPHA, scalar2=None, op0=ALU.mult),
                 reads=[ident_f.b], writes=[dal.b])
            pacc = [pst(es, "pacc%d" % b, [128, 2, 512], F32) for b in range(2)]

            def issue_gathers(i):
                b = i % 2
                for k in range(4):
                    if i < 2:
                        S.op("pool", lambda e, k=k: e.memset(yk[b][k].t[:], 0.0), writes=[yk[b][k].b])
                    S.dma("pool", lambda e, k=k: e.indirect_dma_start(
                        out=yk[b][k].t[:, :], out_offset=None, in_=yg.t[:, :],
                        in_offset=bass.IndirectOffsetOnAxis(ap=dest_i.t[:, i * 4 + k:i * 4 + k + 1], axis=0),
                        bounds_check=breg, oob_is_err=False), reads=[yg.b, dest_b[i]], writes=[yk[b][k].b])
                for k in range(4):
                    S.op("dve", lambda e, k=k: e.tensor_scalar(out=dg[b][k].t[:], in0=ident_f.t[:], scalar1=gate_t.t[:, i * 4 + k:i * 4 + k + 1],
                                                               scalar2=None, op0=ALU.mult), reads=[ident_f.b, gate_b[i]], writes=[dg[b][k].b])

            issue_gathers(0)
            epi_load(E, 0, xin)
            for i in range(NTILE):
                b = i % 2
                if i + 1 < NTILE:
                    issue_gathers(i + 1)
                    epi_load(E, i + 1, xin)

                def add_fn(r, xt, b=b, i=i):
                    ps = pacc[b]
                    for half in range(2):
                        cs = slice(half * 512, (half + 1) * 512)
                        S.op("pe", lambda e: e.matmul(ps.t[:, half, :], lhsT=dal.t[:], rhs=xt.t[:, cs], start=True, stop=False),
                             reads=[dal.b, xt.b], writes=[ps.b], sig=False)
                        for k in range(4):
                            S.op("pe", lambda e, k=k: e.matmul(ps.t[:, half, :], lhsT=dg[b][k].t[:], rhs=yk[b][k].t[:, cs], start=False, stop=(k == 3)),
                                 reads=[dal.b, xt.b] + [t_.b for t_ in dg[b]] + [t_.b for t_ in yk[b]], writes=[ps.b],
                                 sig=(half == 1 and k == 3))
                    return ps
                epilogue(E, i, xin, xout, add_fn, preloaded=True, store_q="act")
        S.barrier()

    G.S = S
    G.nc = nc
    G.es_top = es_top
    G.xs = xs
    G.W = W
    G.sbt = sbt
    G.pst = pst
    G.make_epi = make_epi
    G.epilogue = epilogue
    G.make_router = make_router
    G.route_tile = route_tile
    G.consts = dict(ident_f=ident_f, ident_b=ident_b, uincl_f=uincl_f, ones_f=ones_f, ones_b=ones_b)

    for st in stages:
        if st == "ab":
            mixer_ab_phase(G, xs["x0"], xs["x1"])
        elif st == "moe0":
            moe_phase(0, xs["x1"], xs["x2"])
        elif st == "cf":
            conformer_phase(G, xs["x2"], xs["x3"])
        elif st == "moe1":
            moe_phase(1, xs["x3"], xs["out"])
        elif st == "route0":
            route_only_phase(G, 0, xs["x1"])
        elif st == "route1":
            route_only_phase(G, 1, xs["x3"])

    S.barrier(engines=["sp"])
    es_top.close()
    S.close()
    return nc


def route_only_phase(G, layer, xin):
    S = G.S
    with contextlib.ExitStack() as es:
        pT = G.pst(es, "rpT", [128, 1024], F32)
        pS = G.pst(es, "rpS", [128, 512], F32)
        R = G.make_router(es, layer, pS, pT)
        rt = [G.sbt(es, "rot%d" % i, [128, D], F32) for i in range(2)]
        for i in range(NTILE):
            r = rt[i % 2]
            S.dma("sp", lambda e: e.dma_start(out=r.t[:], in_=xin.t[i * 128:(i + 1) * 128, :]), reads=[xin.b], writes=[r.b])
            G.route_tile(R, i, r)
    S.barrier()


def load_weight_bf16(G, es_stage, dst, src, ncols, nm):
    S = G.S
    stg = [G.sbt(es_stage, "wst_%s%d" % (nm, i), [128, ncols], F32) for i in range(2)]
    for ko in range(8):
        st = stg[ko % 2]
        S.dma("sp", lambda e: e.dma_start(out=st.t[:], in_=src[ko * 128:(ko + 1) * 128, :]), writes=[st.b])
        eng = "pool" if ko % 2 == 0 else "act"
        if eng == "pool":
            S.op("pool", lambda e: e.tensor_copy(out=dst.t[:, ko, :], in_=st.t[:]), reads=[st.b], writes=[dst.b])
        else:
            S.op("act", lambda e: e.copy(out=dst.t[:, ko, :], in_=st.t[:]), reads=[st.b], writes=[dst.b])


def build_xT(G, X, xin, blk):
    S = G.S
    c = G.consts
    for tl in range(4):
        i = blk * 4 + tl
        S.dma("sp", lambda e: e.dma_start(out=X.xl.t[:], in_=xin.t[i * 128:(i + 1) * 128, :]), reads=[xin.b], writes=[X.xl.b])
        S.op("act", lambda e: e.copy(out=X.xlb.t[:], in_=X.xl.t[:]), reads=[X.xl.b], writes=[X.xlb.b])
        for ko in range(8):
            S.op("pe", lambda e: e.transpose(X.PT.t[:, ko * 128:(ko + 1) * 128], X.xlb.t[:, ko * 128:(ko + 1) * 128], c["ident_b"].t[:]),
                 reads=[X.xlb.b, c["ident_b"].b], writes=[X.PT.b], sig=(ko == 7))
        S.op("dve", lambda e: e.tensor_copy(out=X.xT.t[:, :, tl * 128:(tl + 1) * 128], in_=X.PT.t[:].rearrange("p (a b) -> p a b", a=8)),
             reads=[X.PT.b], writes=[X.xT.b])


def mixer_ab_phase(G, xin, xout):
    S, W, sbt, pst = G.S, G.W, G.sbt, G.pst
    c = G.consts
    ident_b, uincl_f, ones_f = c["ident_b"], c["uincl_f"], c["ones_f"]
    with contextlib.ExitStack() as es:
        w_in = sbt(es, "w_in", [128, 8, 3080], BF16)
        w_out = sbt(es, "w_out", [128, 8, D], BF16)
        with contextlib.ExitStack() as es2:
            load_weight_bf16(G, es2, w_in, W["w_in"], 3080, "in")
            load_weight_bf16(G, es2, w_out, W["w_out"], D, "out")
            S.barrier()
        PAB = pst(es, "PAB", [128, 1024], F32)
        PC = pst(es, "PC", [128, 512], F32)
        PD = pst(es, "PD", [128, 512], F32)
        PT = pst(es, "PT", [128, 1024], BF16)
        PE_ = pst(es, "PE", [128, 512], F32)
        PF = pst(es, "PF", [128, 512], F32)
        PG = pst(es, "PG", [128, 512], F32)
        E = G.make_epi(es, 0, 0)
        bif = sbt(es, "bif", [128, 8], F32)
        cw = sbt(es, "cw", [128, 12], F32)
        hg = sbt(es, "hg", [128, 512], F32)
        S.dma("sp", lambda e: e.dma_start(out=bif.t[:], in_=W["ab_bif"].partition_broadcast(128)), writes=[bif.b])
        S.dma("sp", lambda e: e.dma_start(out=cw.t[:], in_=W["ab_conv"]), writes=[cw.b])
        S.dma("sp", lambda e: e.dma_start(out=hg.t[:], in_=W["ab_hg"].partition_broadcast(128)), writes=[hg.b])
        maskT = sbt(es, "maskT", [128, 512], F32)
        for h in range(4):
            S.op("pool", lambda e: e.tensor_copy(out=maskT.t[:, h * 128:(h + 1) * 128], in_=uincl_f.t[:]), reads=[uincl_f.b], writes=[maskT.b])
        tmpc = sbt(es, "tmpc", [128, 512], F32)
        y1 = sbt(es, "y1", [128, 512], F32)
        LNK = math.log(0.125)
        lnk = sbt(es, "lnk", [128, 1], F32)
        S.op("pool", lambda e: e.memset(lnk.t[:], LNK), writes=[lnk.b])
        PS = []
        for sq in range(2):
            P = Ctx()
            X = Ctx()
            X.xl = sbt(es, "xl", [128, D], F32)
            X.xlb = sbt(es, "xlb", [128, D], BF16)
            X.xT = sbt(es, "xT", [128, 8, 512], BF16)
            X.PT = PT
            P.X = X
            P.R = G.make_router(es, 0, PE_, PAB, nbuf=1)
            P.u = [sbt(es, "u%d" % i, [128, 514], F32) for i in range(4)]
            P.yT = sbt(es, "yT", [128, 4, 512], BF16)
            for nm, shp, dt in (("fi", [128, 8], F32), ("e1", [128, 4], F32), ("lf", [128, 4], F32), ("qsc", [128, 4], F32),
                                ("ksc", [128, 4], F32), ("t4", [128, 4], F32), ("eg", [128, 4], F32), ("qs", [128, 256], BF16),
                                ("ks", [128, 256], BF16), ("vp", [128, 4, 129], BF16), ("so", [128, 512], F32),
                                ("qkT", [64, 8, 128], BF16), ("SmT", [128, 512], BF16), ("Cst", [64, 4, 129], F32),
                                ("Cb", [64, 4, 129], BF16), ("dn", [128, 4], F32), ("hh", [128, 512], F32), ("hst", [128, 24], F32),
                                ("hmv", [128, 8], F32), ("hrs", [128, 4], F32), ("hA", [128, 512], BF16), ("hAT", [128, 4, 128], BF16)):
                setattr(P, nm, sbt(es, nm, shp, dt))
            S.op("pool", lambda e: e.memset(P.vp.t[:], 1.0), writes=[P.vp.b])
            PS.append(P)

        def stream(sq):
            P = PS[sq]
            X, R, u, yTb = P.X, P.R, P.u, P.yT
            fi, e1, lf, qsc, ksc, t4, eg, qs, ks, vp, so = P.fi, P.e1, P.lf, P.qsc, P.ksc, P.t4, P.eg, P.qs, P.ks, P.vp, P.so
            qkT, SmT, Cst, Cb, dn, hh, hst, hmv, hrs, hA, hAT = P.qkT, P.SmT, P.Cst, P.Cb, P.dn, P.hh, P.hst, P.hmv, P.hrs, P.hA, P.hAT
            for blk in range(sq * 4, sq * 4 + 4):
                if blk % 4 == 0:
                    for cc in range(4):
                        S.op("pool", lambda e: e.memset(u[cc].t[:, 0:2], 0.0), writes=[u[cc].b])
                    S.op("pool", lambda e: e.memset(Cst.t[:], 0.0), writes=[Cst.b])
                    S.op("pool", lambda e: e.memset(Cb.t[:], 0.0), writes=[Cb.b])
                build_xT(G, X, xin, blk)
                yield
                xT = X.xT
                for cc in range(4):
                    for (bank, c0) in ((PE_, 1544 + 512 + cc * 128), (PF, 1544 + 1024 + cc * 128), (PG, 1544 + cc * 128)):
                        for ko in range(8):
                            S.op("pe", lambda e: e.matmul(bank.t[:], lhsT=w_in.t[:, ko, c0:c0 + 128], rhs=xT.t[:, ko, :], start=(ko == 0), stop=(ko == 7)),
                                 reads=[w_in.b, xT.b], writes=[bank.b], sig=(ko == 7))
                    uu = u[cc]
                    S.op("act", lambda e: e.copy(out=tmpc.t[:], in_=PE_.t[:]), reads=[PE_.b], writes=[tmpc.b])
                    S.op("dve", lambda e: e.tensor_tensor(out=uu.t[:, 2:514], in0=tmpc.t[:], in1=PF.t[:], op=ALU.mult),
                         reads=[tmpc.b, PF.b], writes=[uu.b])
                    S.op("dve", lambda e: e.tensor_scalar(out=y1.t[:], in0=uu.t[:, 2:514], scalar1=cw.t[:, 8 + cc:9 + cc], scalar2=None, op0=ALU.mult),
                         reads=[uu.b, cw.b], writes=[y1.b])
                    S.op("dve", lambda e: e.scalar_tensor_tensor(out=y1.t[:], in0=uu.t[:, 1:513], scalar=cw.t[:, 4 + cc:5 + cc], in1=y1.t[:],
                                                                 op0=ALU.mult, op1=ALU.add), reads=[uu.b, cw.b, y1.b], writes=[y1.b])
                    S.op("dve", lambda e: e.scalar_tensor_tensor(out=y1.t[:], in0=uu.t[:, 0:512], scalar=cw.t[:, cc:cc + 1], in1=y1.t[:],
                                                                 op0=ALU.mult, op1=ALU.add), reads=[uu.b, cw.b, y1.b], writes=[y1.b])
                    S.op("dve", lambda e: e.tensor_tensor(out=yTb.t[:, cc, :], in0=y1.t[:], in1=PG.t[:], op=ALU.mult),
                         reads=[y1.b, PG.b], writes=[yTb.b])
                    S.op("dve", lambda e: e.tensor_copy(out=uu.t[:, 0:2], in_=uu.t[:, 512:514]), reads=[uu.b], writes=[uu.b])
                    yield
                yield
                for tl in range(4):
                    i = blk * 4 + tl
                    tc0 = tl * 128
                    for (out_ap, bank, c0, c1) in ((PAB.t[:, 0:512], PAB, 0, 512), (PAB.t[:, 512:1024], PAB, 512, 1024),
                                                   (PC.t[:], PC, 1024, 1536), (PD.t[:, 0:8], PD, 1536, 1544)):
                        for ko in range(8):
                            S.op("pe", lambda e: e.matmul(out_ap, lhsT=xT.t[:, ko, tc0:tc0 + 128], rhs=w_in.t[:, ko, c0:c1], start=(ko == 0), stop=(ko == 7)),
                                 reads=[w_in.b, xT.b], writes=[bank.b], sig=(ko == 7 and c0 != 0))
                    S.op("dve", lambda e: e.tensor_tensor(out=fi.t[:], in0=PD.t[:, 0:8], in1=bif.t[:], op=ALU.add), reads=[PD.b, bif.b], writes=[fi.b])
                    S.op("act", lambda e: e.activation(out=e1.t[:], in_=fi.t[:, 4:8], func=AF.Exp, scale=-1.0), reads=[fi.b], writes=[e1.b])
                    S.op("act", lambda e: e.activation(out=lf.t[:], in_=e1.t[:], func=AF.Ln, bias=1.0), reads=[e1.b], writes=[lf.b])
                    S.op("pe", lambda e: e.matmul(PD.t[:, 16:20], lhsT=uincl_f.t[:], rhs=lf.t[:], start=True, stop=True),
                         reads=[uincl_f.b, lf.b], writes=[PD.b], sig=False)
                    S.op("pe", lambda e: e.matmul(PD.t[:, 20:24], lhsT=ones_f.t[:], rhs=lf.t[:], start=True, stop=True),
                         reads=[ones_f.b, lf.b], writes=[PD.b])
                    S.op("act", lambda e: e.activation(out=qsc.t[:], in_=PD.t[:, 16:20], func=AF.Exp, scale=-1.0), reads=[PD.b], writes=[qsc.b])
                    S.op("dve", lambda e: e.tensor_tensor(out=t4.t[:], in0=PD.t[:, 16:20], in1=fi.t[:, 0:4], op=ALU.add), reads=[PD.b, fi.b], writes=[t4.b])
                    S.op("act", lambda e: e.activation(out=ksc.t[:], in_=t4.t[:], func=AF.Exp, bias=lnk.t[:, 0:1], scale=1.0), reads=[t4.b, lnk.b], writes=[ksc.b])
                    S.op("act", lambda e: e.activation(out=eg.t[:], in_=PD.t[:, 20:24], func=AF.Exp, scale=-1.0), reads=[PD.b], writes=[eg.b])
                    for h in range(4):
                        S.op("dve", lambda e: e.tensor_scalar(out=qs.t[:, h * 64:(h + 1) * 64], in0=PAB.t[:, h * 64:(h + 1) * 64], scalar1=qsc.t[:, h:h + 1],
                                                              scalar2=None, op0=ALU.mult), reads=[PAB.b, qsc.b], writes=[qs.b])
                        S.op("dve", lambda e: e.tensor_scalar(out=ks.t[:, h * 64:(h + 1) * 64], in0=PAB.t[:, 256 + h * 64:256 + (h + 1) * 64],
                                                              scalar1=ksc.t[:, h:h + 1], scalar2=None, op0=ALU.mult), reads=[PAB.b, ksc.b], writes=[ks.b])
                    S.op("act", lambda e: e.copy(out=vp.t[:, :, 0:128], in_=PAB.t[:, 512:1024].rearrange("p (a b) -> p a b", a=4)),
                         reads=[PAB.b], writes=[vp.b])
                    S.op("act", lambda e: e.activation(out=so.t[:], in_=PC.t[:], func=AF.Sigmoid), reads=[PC.b], writes=[so.b])
                    yield
                    PT64 = PT.t[0:64, :].rearrange("p (a b) -> p a b", a=8)
                    for h in range(4):
                        S.op("pe", lambda e: e.transpose(PT64[:, h, :], qs.t[:, h * 64:(h + 1) * 64], ident_b.t[:]),
                             reads=[qs.b, ident_b.b], writes=[PT.b], sig=False)
                    for h in range(4):
                        S.op("pe", lambda e: e.transpose(PT64[:, 4 + h, :], ks.t[:, h * 64:(h + 1) * 64], ident_b.t[:]),
                             reads=[ks.b, qs.b, ident_b.b], writes=[PT.b], sig=(h == 3))
                    S.op("act", lambda e: e.copy(out=qkT.t[:], in_=PT64), reads=[PT.b], writes=[qkT.b])
                    yield
                    for h in range(4):
                        S.op("pe", lambda e: e.matmul(PE_.t[:, h * 128:(h + 1) * 128], lhsT=qkT.t[:, 4 + h, :], rhs=qkT.t[:, h, :], start=True, stop=True),
                             reads=[qkT.b], writes=[PE_.b], sig=(h == 3))
                    S.op("dve", lambda e: e.tensor_tensor(out=SmT.t[:], in0=PE_.t[:], in1=maskT.t[:], op=ALU.mult), reads=[PE_.b, maskT.b], writes=[SmT.b])
                    yield
                    for h in range(4):
                        bank = PF if h < 2 else PG
                        o_ap = bank.t[:, (h % 2) * 129:(h % 2 + 1) * 129]
                        S.op("pe", lambda e: e.matmul(o_ap, lhsT=SmT.t[:, h * 128:(h + 1) * 128], rhs=vp.t[:, h, :], start=True, stop=False),
                             reads=[SmT.b, vp.b], writes=[bank.b], sig=False)
                        S.op("pe", lambda e: e.matmul(o_ap, lhsT=qkT.t[:, h, :], rhs=Cb.t[:, h, :], start=False, stop=True),
                             reads=[qkT.b, Cb.b, SmT.b, vp.b], writes=[bank.b], sig=(h % 2 == 1))
                    for h in range(4):
                        o_ap = PAB.t[0:64, (h // 2) * 512 + (h % 2) * 129:(h // 2) * 512 + (h % 2 + 1) * 129]
                        S.op("pe", lambda e: e.matmul(o_ap, lhsT=ks.t[:, h * 64:(h + 1) * 64], rhs=vp.t[:, h, :], start=True, stop=True),
                             reads=[ks.b, vp.b], writes=[PAB.b], sig=(h == 3))
                    for h in range(4):
                        o_ap = PAB.t[0:64, (h // 2) * 512 + (h % 2) * 129:(h // 2) * 512 + (h % 2 + 1) * 129]
                        S.op("dve", lambda e: e.tensor_scalar(out=Cst.t[:, h, :], in0=Cst.t[:, h, :], scalar1=eg.t[0:64, h:h + 1], scalar2=None, op0=ALU.mult),
                             reads=[Cst.b, eg.b], writes=[Cst.b])
                        S.op("dve", lambda e: e.scalar_tensor_tensor(out=Cst.t[:, h, :], in0=o_ap, scalar=eg.t[0:64, h:h + 1], in1=Cst.t[:, h, :],
                                                                     op0=ALU.mult, op1=ALU.add), reads=[PAB.b, eg.b, Cst.b], writes=[Cst.b])
                    S.op("act", lambda e: e.copy(out=Cb.t[:], in_=Cst.t[:]), reads=[Cst.b], writes=[Cb.b])
                    for bi, bank in enumerate((PF, PG)):
                        den = bank.t[:, 0:258].rearrange("p (h c) -> p h c", c=129)[:, :, 128]
                        S.op("act", lambda e: e.activation(out=dn.t[:, 2 * bi:2 * bi + 2], in_=den, func=AF.Abs), reads=[bank.b], writes=[dn.b])
                    S.op("dve", lambda e: e.tensor_scalar(out=dn.t[:], in0=dn.t[:], scalar1=1.0, scalar2=None, op0=ALU.max), reads=[dn.b], writes=[dn.b])
                    S.op("dve", lambda e: e.reciprocal(out=dn.t[:], in_=dn.t[:]), reads=[dn.b], writes=[dn.b])
                    for h in range(4):
                        bank = PF if h < 2 else PG
                        S.op("dve", lambda e: e.tensor_scalar(out=hh.t[:, h * 128:(h + 1) * 128], in0=bank.t[:, (h % 2) * 129:(h % 2) * 129 + 128],
                                                              scalar1=dn.t[:, h:h + 1], scalar2=None, op0=ALU.mult), reads=[bank.b, dn.b], writes=[hh.b])
                    for h in range(4):
                        S.op("dve", lambda e: e.bn_stats(out=hst.t[:, h * 6:(h + 1) * 6], in_=hh.t[:, h * 128:(h + 1) * 128]), reads=[hh.b], writes=[hst.b])
                    for h in range(4):
                        S.op("dve", lambda e: e.bn_aggr(out=hmv.t[:, h * 2:(h + 1) * 2], in_=hst.t[:, h * 6:(h + 1) * 6]), reads=[hst.b], writes=[hmv.b])
                    S.op("act", lambda e: e.activation(out=hrs.t[:], in_=hmv.t[:].rearrange("p (h c) -> p h c", c=2)[:, :, 1], func=AF.Sqrt, bias=HN_EPS),
                         reads=[hmv.b], writes=[hrs.b])
                    S.op("dve", lambda e: e.reciprocal(out=hrs.t[:], in_=hrs.t[:]), reads=[hrs.b], writes=[hrs.b])
                    for h in range(4):
                        S.op("dve", lambda e: e.tensor_scalar(out=hh.t[:, h * 128:(h + 1) * 128], in0=hh.t[:, h * 128:(h + 1) * 128],
                                                              scalar1=hmv.t[:, 2 * h:2 * h + 1], scalar2=hrs.t[:, h:h + 1], op0=ALU.subtract, op1=ALU.mult),
                             reads=[hh.b, hmv.b, hrs.b], writes=[hh.b])
                    S.op("dve", lambda e: e.tensor_tensor(out=hh.t[:], in0=hh.t[:], in1=hg.t[:], op=ALU.mult), reads=[hh.b, hg.b], writes=[hh.b])
                    S.op("dve", lambda e: e.tensor_tensor(out=hA.t[:], in0=hh.t[:], in1=so.t[:], op=ALU.mult), reads=[hh.b, so.b], writes=[hA.b])
                    for cq in range(4):
                        S.op("pe", lambda e: e.transpose(PT.t[:, cq * 128:(cq + 1) * 128], hA.t[:, cq * 128:(cq + 1) * 128], ident_b.t[:]),
                             reads=[hA.b, ident_b.b], writes=[PT.b], sig=(cq == 3))
                    S.op("act", lambda e: e.copy(out=hAT.t[:], in_=PT.t[:, 0:512].rearrange("p (a b) -> p a b", a=4)), reads=[PT.b], writes=[hAT.b])
                    yield
                    for half, bank in enumerate((PC, PD)):
                        for cq in range(8):
                            lhsT = hAT.t[:, cq, :] if cq < 4 else yTb.t[:, cq - 4, tc0:tc0 + 128]
                            S.op("pe", lambda e: e.matmul(bank.t[:], lhsT=lhsT, rhs=w_out.t[:, cq, half * 512:(half + 1) * 512], start=(cq == 0), stop=(cq == 7)),
                                 reads=[hAT.b, yTb.b, w_out.b], writes=[bank.b], sig=(cq == 7))

                    def add_fn(r, xt):
                        S.op("dve", lambda e: e.scalar_tensor_tensor(out=r.t[:, 0:512], in0=xt.t[:, 0:512], scalar=ALPHA, in1=PC.t[:], op0=ALU.mult, op1=ALU.add),
                             reads=[xt.b, PC.b], writes=[r.b])
                        S.op("dve", lambda e: e.scalar_tensor_tensor(out=r.t[:, 512:1024], in0=xt.t[:, 512:1024], scalar=ALPHA, in1=PD.t[:], op0=ALU.mult, op1=ALU.add),
                             reads=[xt.b, PD.b, r.b], writes=[r.b])
                    r = G.epilogue(E, i, xin, xout, add_fn, slot=sq)
                    yield
                    G.route_tile(R, i, r)
                    yield

        gens = [stream(0), stream(1)]
        live = [True, True]
        while any(live):
            for q in range(2):
                if live[q]:
                    try:
                        next(gens[q])
                    except StopIteration:
                        live[q] = False
    S.barrier()


def conformer_phase(G, xin, xout):
    S, W, sbt, pst = G.S, G.W, G.sbt, G.pst
    c = G.consts
    ident_b, ident_f, ones_f = c["ident_b"], c["ident_f"], c["ones_f"]
    with contextlib.ExitStack() as es:
        pw1 = sbt(es, "pw1", [128, 8, 2048], BF16)
        pw2 = sbt(es, "pw2", [128, 8, D], BF16)
        with contextlib.ExitStack() as es2:
            load_weight_bf16(G, es2, pw1, W["pw1"], 2048, "p1")
            load_weight_bf16(G, es2, pw2, W["pw2"], D, "p2")
            S.barrier()
        cfdw = sbt(es, "cfdw", [128, 248], F32)
        cfv = sbt(es, "cfv", [128, 24], F32)
        S.dma("sp", lambda e: e.dma_start(out=cfdw.t[:], in_=W["cf_dw"]), writes=[cfdw.b])
        S.dma("sp", lambda e: e.dma_start(out=cfv.t[:], in_=W["cf_vec"]), writes=[cfv.b])
        diag = sbt(es, "diag", [128, 248, 128], BF16)
        for q in range(248):
            S.op("dve", lambda e: e.tensor_scalar(out=diag.t[:, q, :], in0=ident_f.t[:], scalar1=cfdw.t[:, q:q + 1], scalar2=None, op0=ALU.mult),
                 reads=[ident_f.b, cfdw.b], writes=[diag.b])
        onesm = sbt(es, "onesm", [128, 128], F32)
        S.op("pool", lambda e: e.memset(onesm.t[:], 1.0 / 1024.0), writes=[onesm.b])
        X = Ctx()
        X.xl = sbt(es, "xl", [128, D], F32)
        X.xlb = sbt(es, "xlb", [128, D], BF16)
        X.xT = sbt(es, "xT", [128, 8, 512], BF16)
        PAB = pst(es, "PAB", [128, 1024], F32)
        PC = pst(es, "PC", [128, 512], F32)
        PD = pst(es, "PD", [128, 512], F32)
        PT = pst(es, "PT", [128, 1024], BF16)
        PE_ = pst(es, "PE", [128, 512], F32)
        PF = pst(es, "PF", [128, 512], F32)
        PG = pst(es, "PG", [128, 512], F32)
        X.PT = PT
        E = G.make_epi(es, 1, 0, nbuf=2)
        R = G.make_router(es, 1, PE_, PAB, nbuf=1)
        ub = [sbt(es, "ub%d" % i, [128, 542], BF16) for i in range(8)]
        cv = [sbt(es, "cv%d" % i, [128, 512], F32) for i in range(8)]
        sg = sbt(es, "sg", [128, 512], F32)
        sq = sbt(es, "sq", [128, 512], F32)
        m2 = sq
        rstd = sbt(es, "rstd", [128, 512], F32)
        nmr = sbt(es, "nmr", [128, 512], F32)
        nn = sg
        vT = sbt(es, "vT", [128, 8, 512], BF16)
        print("CF sbuf bytes remaining", G.nc.sbuf_bytes_remaining)
        for blk in range(8):
            if blk % 4 == 0:
                for cc in range(8):
                    S.op("pool", lambda e: e.memset(ub[cc].t[:, 0:30], 0.0), writes=[ub[cc].b])
            build_xT(G, X, xin, blk)
            xT = X.xT
            def ag(cc):
                ba, bg = (PE_, PF) if cc % 2 == 0 else (PC, PD)
                for (bank, c0) in ((ba, cc * 128), (bg, 1024 + cc * 128)):
                    for ko in range(8):
                        S.op("pe", lambda e: e.matmul(bank.t[:], lhsT=pw1.t[:, ko, c0:c0 + 128], rhs=xT.t[:, ko, :], start=(ko == 0), stop=(ko == 7)),
                             reads=[pw1.b, xT.b], writes=[bank.b], sig=(ko == 7))

            def rest(cc):
                ba, bg = (PE_, PF) if cc % 2 == 0 else (PC, PD)
                S.op("act", lambda e: e.activation(out=sg.t[:], in_=bg.t[:], func=AF.Sigmoid), reads=[bg.b], writes=[sg.b])
                S.op("dve", lambda e: e.tensor_tensor(out=ub[cc].t[:, 30:542], in0=ba.t[:], in1=sg.t[:], op=ALU.mult),
                     reads=[ba.b, sg.b], writes=[ub[cc].b])
                for j in range(31):
                    S.op("pe", lambda e: e.matmul(PG.t[:], lhsT=diag.t[:, j * 8 + cc, :], rhs=ub[cc].t[:, j:j + 512], start=(j == 0), stop=(j == 30)),
                         reads=[diag.b, ub[cc].b], writes=[PG.b], sig=(j == 30))
                S.op("act", lambda e: e.activation(out=cv[cc].t[:], in_=PG.t[:], func=AF.Identity, bias=cfv.t[:, cc:cc + 1], scale=1.0),
                     reads=[PG.b, cfv.b], writes=[cv[cc].b])
                S.op("act", lambda e: e.activation(out=sq.t[:], in_=cv[cc].t[:], func=AF.Square), reads=[cv[cc].b], writes=[sq.b])
                S.op("pe", lambda e: e.matmul(PAB.t[:, 0:512], lhsT=onesm.t[:], rhs=cv[cc].t[:], start=(cc == 0), stop=(cc == 7)),
                     reads=[onesm.b, cv[cc].b], writes=[PAB.b])
                S.op("pe", lambda e: e.matmul(PAB.t[:, 512:1024], lhsT=onesm.t[:], rhs=sq.t[:], start=(cc == 0), stop=(cc == 7)),
                     reads=[onesm.b, sq.b], writes=[PAB.b])
                S.op("dve", lambda e: e.tensor_copy(out=ub[cc].t[:, 0:30], in_=ub[cc].t[:, 512:542]), reads=[ub[cc].b], writes=[ub[cc].b])

            ag(0)
            for cc in range(8):
                if cc + 1 < 8:
                    ag(cc + 1)
                rest(cc)
            S.op("act", lambda e: e.activation(out=m2.t[:], in_=PAB.t[:, 0:512], func=AF.Square), reads=[PAB.b], writes=[m2.b])
            S.op("dve", lambda e: e.tensor_tensor(out=m2.t[:], in0=PAB.t[:, 512:1024], in1=m2.t[:], op=ALU.subtract), reads=[PAB.b, m2.b], writes=[m2.b])
            S.op("act", lambda e: e.activation(out=m2.t[:], in_=m2.t[:], func=AF.Sqrt, bias=LN_EPS), reads=[m2.b], writes=[m2.b])
            S.op("dve", lambda e: e.reciprocal(out=rstd.t[:], in_=m2.t[:]), reads=[m2.b], writes=[rstd.b])
            S.op("dve", lambda e: e.scalar_tensor_tensor(out=nmr.t[:], in0=PAB.t[:, 0:512], scalar=-1.0, in1=rstd.t[:], op0=ALU.mult, op1=ALU.mult),
                 reads=[PAB.b, rstd.b], writes=[nmr.b])
            for cc in range(8):
                S.op("dve", lambda e: e.tensor_tensor(out=nn.t[:], in0=cv[cc].t[:], in1=rstd.t[:], op=ALU.mult), reads=[cv[cc].b, rstd.b], writes=[nn.b])
                S.op("dve", lambda e: e.tensor_tensor(out=nn.t[:], in0=nn.t[:], in1=nmr.t[:], op=ALU.add), reads=[nn.b, nmr.b], writes=[nn.b])
                S.op("dve", lambda e: e.tensor_scalar(out=nn.t[:], in0=nn.t[:], scalar1=cfv.t[:, 8 + cc:9 + cc], scalar2=cfv.t[:, 16 + cc:17 + cc],
                                                      op0=ALU.mult, op1=ALU.add), reads=[nn.b, cfv.b], writes=[nn.b])
                S.op("act", lambda e: e.activation(out=vT.t[:, cc, :], in_=nn.t[:], func=AF.Silu), reads=[nn.b], writes=[vT.b])
            rts = {}

            def outproj_epi(tl):
                i = blk * 4 + tl
                tc0 = tl * 128
                for half, bank in enumerate((PC, PD)):
                    for cq in range(8):
                        S.op("pe", lambda e: e.matmul(bank.t[:], lhsT=vT.t[:, cq, tc0:tc0 + 128], rhs=pw2.t[:, cq, half * 512:(half + 1) * 512],
                                                      start=(cq == 0), stop=(cq == 7)), reads=[vT.b, pw2.b], writes=[bank.b], sig=(cq == 7))

                def add_fn(r, xt):
                    S.op("dve", lambda e: e.scalar_tensor_tensor(out=r.t[:, 0:512], in0=xt.t[:, 0:512], scalar=ALPHA, in1=PC.t[:], op0=ALU.mult, op1=ALU.add),
                         reads=[xt.b, PC.b], writes=[r.b])
                    S.op("dve", lambda e: e.scalar_tensor_tensor(out=r.t[:, 512:1024], in0=xt.t[:, 512:1024], scalar=ALPHA, in1=PD.t[:], op0=ALU.mult, op1=ALU.add),
                         reads=[xt.b, PD.b, r.b], writes=[r.b])
                rts[tl] = G.epilogue(E, i, xin, xout, add_fn)

            outproj_epi(0)
            for tl in range(4):
                if tl + 1 < 4:
                    outproj_epi(tl + 1)
                G.route_tile(R, blk * 4 + tl, rts[tl])
    S.barrier()


def host_weights(inp, stages):
    f = np.ascontiguousarray
    m = {}
    if "ab" in stages:
        m["ab_w_in"] = f(inp["ab_w_in"][0])
        m["ab_w_out"] = f(inp["ab_w_out"][0])
        m["ab_bif"] = f(np.concatenate([inp["ab_b_igate"][0], inp["ab_b_fgate"][0]]))
        m["ab_conv"] = f(inp["ab_conv_w"][0].reshape(3, 4, 128).transpose(2, 0, 1).reshape(128, 12))
        m["ab_hg"] = f(inp["ab_head_gain"][0])
    if "cf" in stages:
        m["cf_w_pw1"] = f(inp["cf_w_pw1"][0])
        m["cf_w_pw2"] = f(inp["cf_w_pw2"][0])
        m["cf_dw"] = f(inp["cf_w_dw"][0].reshape(31, 8, 128).transpose(2, 0, 1).reshape(128, 248))
        vec = np.stack([inp["cf_b_dw"][0], inp["cf_ln_g"][0], inp["cf_ln_b"][0]])
        m["cf_vec"] = f(vec.reshape(3, 8, 128).transpose(2, 0, 1).reshape(128, 24))
    m["router_w"] = f(inp["router_w"])
    m["router_b"] = f(inp["router_b"])
    m["exp_w_up"] = f(inp["exp_w_up"])
    m["exp_w_down"] = f(inp["exp_w_down"])
    m["bup"] = f(inp["exp_b_up"].reshape(2, NE, 16, 128).transpose(3, 0, 1, 2).reshape(128, 2 * NE * 16))
    m["exp_b_down"] = f(inp["exp_b_down"])
    m["post_ln_g"] = f(inp["post_ln_g"])
    m["post_ln_b"] = f(inp["post_ln_b"])
    return m


def kernel(**inputs):
    inp = {k: np.asarray(v) for k, v in inputs.items()}
    stages = ("ab", "moe0", "cf", "moe1")
    nc = build_program(stages)
    wm = host_weights(inp, stages)
    x = inp["x"].reshape(8, NTOK, D)
    in_maps = []
    for c in range(8):
        m = dict(wm)
        m["x"] = np.ascontiguousarray(x[c])
        in_maps.append(m)
    res = run_bass_kernel_spmd(nc, in_maps, core_ids=list(range(8)))
    out = np.stack([np.asarray(r["out"]) for r in res.results], axis=0)
    return out.reshape(16, SEQ, D).astype(np.float32)
```

```python
import contextlib
import math
import os
import numpy as np
import concourse.bass as bass
import concourse.mybir as mybir
from concourse.bass_utils import run_bass_kernel_spmd
from concourse.alu_op_type import AluOpType as ALU

F32 = mybir.dt.float32
BF16 = mybir.dt.bfloat16
I32 = mybir.dt.int32
U32 = mybir.dt.uint32
AF = mybir.ActivationFunctionType

D = 1024
NTOK = 4096
NTILE = 32
SEQ = 2048
NE = 32
CAP = 640
NTC = CAP // 128
NSLOT = NE * CAP
ALPHA = 4.0 ** 0.25
LN_EPS = 1e-5
HN_EPS = 1e-6


class Buf:
    __slots__ = ("name", "writer", "readers", "sem", "dcount")

    def __init__(self, name):
        self.name = name
        self.writer = None
        self.readers = []
        self.sem = None
        self.dcount = 0


class T:
    __slots__ = ("t", "b")

    def __init__(self, t, b):
        self.t = t
        self.b = b


class Sched:
    def __init__(self, nc):
        self.nc = nc
        self.engs = {"pe": nc.tensor, "act": nc.scalar, "dve": nc.vector, "pool": nc.gpsimd, "sp": nc.sync}
        self.sems = {}
        self.count = {k: 0 for k in self.engs}
        self.seen = {k: {} for k in self.engs}
        self._ctx = []
        self.nsem = 0
        self.dma_bufs = []
        for k in self.engs:
            self.sems[k] = self.new_sem("s_" + k)

    def new_sem(self, name):
        cm = self.nc.semaphore("%s_%d" % (name, self.nsem))
        s = cm.__enter__()
        self._ctx.append(cm)
        self.nsem += 1
        return s

    def close(self):
        for cm in reversed(self._ctx):
            cm.__exit__(None, None, None)

    def _wait(self, eng, tok):
        if tok is None:
            return
        sem, val, pe = tok
        key = id(sem)
        if self.seen[eng].get(key, 0) >= val:
            return
        if pe == "pe" and eng == "pe":
            return
        self.engs[eng].wait_ge(sem, val)
        self.seen[eng][key] = val

    def _deps(self, eng, reads, writes, is_dma=False, after=()):
        toks = list(after)
        for b in reads:
            toks.append(b.writer)
        for b in writes:
            if b.writer is not None and not (is_dma and b.writer[2] == "dma"):
                toks.append(b.writer)
            toks.extend(b.readers)
        best = {}
        for t in toks:
            if t is None:
                continue
            k = id(t[0])
            if k not in best or best[k][1] < t[1]:
                best[k] = t
        for t in best.values():
            self._wait(eng, t)

    def _commit(self, tok, reads, writes):
        for b in writes:
            b.writer = tok
            b.readers = []
        for b in reads:
            b.readers.append(tok)
            if len(b.readers) > 16:
                last = {}
                for r in b.readers:
                    last[id(r[0])] = r
                b.readers = list(last.values())

    def op(self, eng, fn, reads=(), writes=(), after=(), sig=True):
        self._deps(eng, reads, writes, after=after)
        inst = fn(self.engs[eng])
        if not sig:
            return None
        self.count[eng] += 1
        inst.then_inc(self.sems[eng], 1)
        tok = (self.sems[eng], self.count[eng], eng)
        self._commit(tok, reads, writes)
        return tok

    def dma(self, q, fn, reads=(), writes=(), after=(), sembuf=None):
        sb = sembuf if sembuf is not None else writes[0]
        if sb.sem is None:
            sb.sem = self.new_sem("d_" + sb.name)
            self.dma_bufs.append(sb)
        self._deps(q, reads, writes, is_dma=True, after=after)
        inst = fn(self.engs[q])
        sb.dcount += 16
        inst.then_inc(sb.sem, 16)
        tok = (sb.sem, sb.dcount, "dma")
        self._commit(tok, reads, writes)
        return tok

    def barrier(self, engines=None):
        for e in (engines or self.engs):
            for k in self.engs:
                if k != e and self.count[k] > 0:
                    self._wait(e, (self.sems[k], self.count[k], k))
            for b in self.dma_bufs:
                if b.dcount > 0:
                    self._wait(e, (b.sem, b.dcount, "dma"))


class Ctx:
    pass


def build_program(stages=("ab", "moe0", "cf", "moe1"), debug=False):
    nc = bass.Bass("TRN2", target_bir_lowering=False)
    S = Sched(nc)
    G = Ctx()
    es_top = contextlib.ExitStack()

    def din(name, shape, dt=F32):
        return nc.dram_tensor(name, list(shape), dt, kind="ExternalInput").ap()

    def dscratch(name, shape, dt=F32, out=False):
        kind = "ExternalOutput" if out else "Internal"
        return nc.dram_tensor(name, list(shape), dt, kind=kind).ap()

    first = stages[0]
    x0 = din("x", [NTOK, D]) if first == "ab" else None
    W = {}
    if "ab" in stages:
        W["w_in"] = din("ab_w_in", [D, 3080])
        W["w_out"] = din("ab_w_out", [D, D])
        W["ab_bif"] = din("ab_bif", [8])
        W["ab_conv"] = din("ab_conv", [128, 12])
        W["ab_hg"] = din("ab_hg", [512])
    if "cf" in stages:
        W["pw1"] = din("cf_w_pw1", [D, 2048])
        W["pw2"] = din("cf_w_pw2", [D, D])
        W["cf_dw"] = din("cf_dw", [128, 31 * 8])
        W["cf_vec"] = din("cf_vec", [128, 24])
    W["router_w"] = din("router_w", [2, D, NE])
    W["router_b"] = din("router_b", [2, NE])
    W["up"] = din("exp_w_up", [2, NE, D, 2 * D])
    W["dn"] = din("exp_w_down", [2, NE, D, D])
    W["bup"] = din("bup", [128, 2 * NE * 16])
    W["bdn"] = din("exp_b_down", [2, NE, D])
    W["lng"] = din("post_ln_g", [2, 2, D])
    W["lnb"] = din("post_ln_b", [2, 2, D])

    last = stages[-1]
    xs = {}
    names = {"ab": "x1", "moe0": "x2", "cf": "x3", "moe1": "out"}
    for st in ("ab", "moe0", "cf", "moe1"):
        nm = names[st]
        if st in stages:
            xs[nm] = T(dscratch(nm, [NTOK, D], F32, out=(debug or st == last)), Buf(nm))
    prev = {"moe0": "x1", "cf": "x2", "moe1": "x3", "route0": "x1", "route1": "x3"}
    if first != "ab":
        nm = prev[first]
        xs[nm] = T(din(nm, [NTOK, D]), Buf(nm))
    else:
        xs["x0"] = T(x0, Buf("x0"))
    xg = T(dscratch("xg", [NSLOT, D], BF16, out=debug), Buf("xg"))
    yg = T(dscratch("yg", [NSLOT, D], F32, out=debug), Buf("yg"))

    uid = [0]

    def sbt(es, name, shape, dt):
        uid[0] += 1
        name = "sb%d_%s" % (uid[0], name)
        return T(es.enter_context(nc.sbuf_tensor(name, list(shape), dt)), Buf(name))

    def pst(es, name, shape, dt):
        uid[0] += 1
        name = "ps%d_%s" % (uid[0], name)
        return T(es.enter_context(nc.psum_tensor(name, list(shape), dt)), Buf(name))

    ident_f = sbt(es_top, "ident_f", [128, 128], F32)
    ident_b = sbt(es_top, "ident_b", [128, 128], BF16)
    uincl_f = sbt(es_top, "uincl_f", [128, 128], F32)
    ustr_b = sbt(es_top, "ustr_b", [128, 128], BF16)
    ones_f = sbt(es_top, "ones_f", [128, 128], F32)
    ones_b = sbt(es_top, "ones_b", [128, 128], BF16)
    iota_e = sbt(es_top, "iota_e", [128, NE], F32)
    ecap = sbt(es_top, "ecap", [128, NE], F32)
    dest_i = sbt(es_top, "dest_i", [128, NTILE * 4], I32)
    gate_t = sbt(es_top, "gate_t", [128, NTILE * 4], F32)
    dest_b = [Buf("dest%d" % i) for i in range(NTILE)]
    gate_b = [Buf("gate%d" % i) for i in range(NTILE)]
    cbase = sbt(es_top, "cbase", [128, NE], F32)
    zero_b = sbt(es_top, "zero_b", [128, 1024], BF16)
    tmpc = sbt(es_top, "tmpc", [128, 128], F32)

    def consts():
        S.op("pool", lambda e: e.memset(ones_f.t[:], 1.0), writes=[ones_f.b])
        S.op("pool", lambda e: e.memset(ones_b.t[:], 1.0), writes=[ones_b.b])
        S.op("pool", lambda e: e.memset(zero_b.t[:], 0.0), writes=[zero_b.b])
        S.op("pool", lambda e: e.affine_select(out=ident_f.t[:], in_=ones_f.t[:], pattern=[[-1, 128]],
                                                compare_op=ALU.is_equal, fill=0.0, base=0, channel_multiplier=1),
             reads=[ones_f.b], writes=[ident_f.b])
        S.op("dve", lambda e: e.tensor_copy(out=ident_b.t[:], in_=ident_f.t[:]), reads=[ident_f.b], writes=[ident_b.b])
        S.op("pool", lambda e: e.affine_select(out=uincl_f.t[:], in_=ones_f.t[:], pattern=[[1, 128]],
                                                compare_op=ALU.is_ge, fill=0.0, base=0, channel_multiplier=-1),
             reads=[ones_f.b], writes=[uincl_f.b])
        S.op("pool", lambda e: e.affine_select(out=tmpc.t[:], in_=ones_f.t[:], pattern=[[1, 128]],
                                                compare_op=ALU.is_gt, fill=0.0, base=0, channel_multiplier=-1),
             reads=[ones_f.b], writes=[tmpc.b])
        S.op("dve", lambda e: e.tensor_copy(out=ustr_b.t[:], in_=tmpc.t[:]), reads=[tmpc.b], writes=[ustr_b.b])
        S.op("pool", lambda e: e.iota(iota_e.t[:], pattern=[[1, NE]], base=0, channel_multiplier=0,
                                      allow_small_or_imprecise_dtypes=True), writes=[iota_e.b])
        S.op("dve", lambda e: e.tensor_scalar(out=ecap.t[:], in0=iota_e.t[:], scalar1=float(CAP), scalar2=None,
                                              op0=ALU.mult), reads=[iota_e.b], writes=[ecap.b])

    consts()
    breg = nc.gpsimd.to_reg(NSLOT - 1)
    zf = None
    for i in range(NSLOT // 128):
        zf = S.dma("sp", lambda e, i=i: e.dma_start(
            out=xg.t[i * 128:(i + 1) * 128, :], in_=zero_b.t[:]),
            reads=[zero_b.b], writes=[xg.b])
    G.xg_zero_tok = zf

    def make_epi(es, layer, which, nbuf=2):
        E = Ctx()
        E.lng = sbt(es, "lng%d%d" % (layer, which), [128, D], F32)
        E.lnb = sbt(es, "lnb%d%d" % (layer, which), [128, D], F32)
        S.dma("sp", lambda e: e.dma_start(out=E.lng.t[:], in_=W["lng"][layer, which].partition_broadcast(128)), writes=[E.lng.b])
        S.dma("sp", lambda e: e.dma_start(out=E.lnb.t[:], in_=W["lnb"][layer, which].partition_broadcast(128)), writes=[E.lnb.b])
        E.xt = [sbt(es, "epx%d%d_%d" % (layer, which, i), [128, D], F32) for i in range(nbuf)]
        E.r = [sbt(es, "epr%d%d_%d" % (layer, which, i), [128, D], F32) for i in range(nbuf)]
        E.nbuf = nbuf
        E.sts = [sbt(es, "epst%d%d" % (layer, which), [128, 12], F32) for i in range(nbuf)]
        E.mvs = [sbt(es, "epmv%d%d" % (layer, which), [128, 2], F32) for i in range(nbuf)]
        E.eps = sbt(es, "epeps%d%d" % (layer, which), [128, 1], F32)
        S.op("dve", lambda e: e.memset(E.eps.t[:], LN_EPS), writes=[E.eps.b])
        return E

    def epi_load(E, i, xin, slot=None):
        sl = (i if slot is None else slot) % E.nbuf
        xt = E.xt[sl]
        S.dma("sp", lambda e: e.dma_start(out=xt.t[:], in_=xin.t[i * 128:(i + 1) * 128, :]), reads=[xin.b], writes=[xt.b])

    def epilogue(E, i, xin, xout, add_fn, slot=None, preloaded=False, store_q="pool"):
        sl = (i if slot is None else slot) % E.nbuf
        xt = E.xt[sl]
        r = E.r[sl]
        est, emv = E.sts[sl], E.mvs[sl]
        if not preloaded:
            S.dma("sp", lambda e: e.dma_start(out=xt.t[:], in_=xin.t[i * 128:(i + 1) * 128, :]), reads=[xin.b], writes=[xt.b])
        src = add_fn(r, xt)
        if src is None:
            src, h0, h1, flat = r, r.t[:, 0:512], r.t[:, 512:1024], r.t[:]
        else:
            h0, h1, flat = src.t[:, 0, :], src.t[:, 1, :], src.t[:].rearrange("p a b -> p (a b)")
        S.op("dve", lambda e: e.bn_stats(out=est.t[:, 0:6], in_=h0), reads=[src.b], writes=[est.b])
        S.op("dve", lambda e: e.bn_stats(out=est.t[:, 6:12], in_=h1), reads=[src.b], writes=[est.b])
        S.op("dve", lambda e: e.bn_aggr(out=emv.t[:], in_=est.t[:]), reads=[est.b], writes=[emv.b])
        S.op("act", lambda e: e.activation(out=emv.t[:, 1:2], in_=emv.t[:, 1:2], func=AF.Ln, bias=E.eps.t[:, 0:1], scale=1.0),
             reads=[emv.b, E.eps.b], writes=[emv.b])
        S.op("act", lambda e: e.activation(out=emv.t[:, 1:2], in_=emv.t[:, 1:2], func=AF.Exp, scale=-0.5),
             reads=[emv.b], writes=[emv.b])
        S.op("dve", lambda e: e.tensor_scalar(out=r.t[:], in0=flat, scalar1=emv.t[:, 0:1], scalar2=emv.t[:, 1:2],
                                              op0=ALU.subtract, op1=ALU.mult), reads=[src.b, r.b, emv.b], writes=[r.b])
        S.op("dve", lambda e: e.tensor_tensor(out=r.t[:], in0=r.t[:], in1=E.lng.t[:], op=ALU.mult),
             reads=[r.b, E.lng.b], writes=[r.b])
        S.op("dve", lambda e: e.tensor_tensor(out=r.t[:], in0=r.t[:], in1=E.lnb.t[:], op=ALU.add),
             reads=[r.b, E.lnb.b], writes=[r.b])
        S.dma(store_q, lambda e: e.dma_start(out=xout.t[i * 128:(i + 1) * 128, :], in_=r.t[:]), reads=[r.b], writes=[xout.b], sembuf=r.b)
        return r

    def make_router(es, layer, pbanks, ptr_f, nbuf=2):
        R = Ctx()
        R.rw = sbt(es, "rw%d" % layer, [128, 8, NE], F32)
        R.rb = sbt(es, "rb%d" % layer, [128, NE], F32)
        S.dma("sp", lambda e: e.dma_start(out=R.rw.t[:], in_=W["router_w"][layer].rearrange("(ko p) n -> p ko n", p=128)),
              writes=[R.rw.b])
        S.dma("sp", lambda e: e.dma_start(out=R.rb.t[:], in_=W["router_b"][layer].partition_broadcast(128)), writes=[R.rb.b])
        S.op("dve", lambda e: e.memset(cbase.t[:], 0.0), writes=[cbase.b])
        R.xT = sbt(es, "rxT%d" % layer, [128, 8, 128], F32)
        R.xb = [sbt(es, "rxb%d_%d" % (layer, i), [128, D], BF16) for i in range(nbuf)]
        R.nbuf = nbuf
        R.lg = sbt(es, "rlg%d" % layer, [128, NE], F32)
        R.v8 = sbt(es, "rv8%d" % layer, [128, 8], F32)
        R.i8 = sbt(es, "ri8%d" % layer, [128, 8], U32)
        R.i8f = sbt(es, "ri8f%d" % layer, [128, 8], F32)
        R.mk = sbt(es, "rmk%d" % layer, [128, 4, NE], F32)
        R.msum = sbt(es, "rms%d" % layer, [128, NE], F32)
        R.msb = sbt(es, "rmsb%d" % layer, [128, NE], BF16)
        R.pos = sbt(es, "rpos%d" % layer, [128, NE], F32)
        R.junk = sbt(es, "rjk%d" % layer, [128, NE], F32)
        R.dst = sbt(es, "rdst%d" % layer, [128, 4], F32)
        R.val = sbt(es, "rval%d" % layer, [128, 4], F32)
        R.ex = sbt(es, "rex%d" % layer, [128, 4], F32)
        R.nv0 = sbt(es, "rnv%d" % layer, [128, 1], F32)
        R.ssum = sbt(es, "rss%d" % layer, [128, 1], F32)
        R.pT = ptr_f
        R.pS = pbanks
        return R

    def route_tile(R, i, r):
        for ko in range(8):
            S.op("pe", lambda e, ko=ko: e.transpose(R.pT.t[:, ko * 128:(ko + 1) * 128], r.t[:, ko * 128:(ko + 1) * 128], ident_f.t[:]),
                 reads=[r.b, ident_f.b], writes=[R.pT.b], sig=(ko == 7))
        S.op("act", lambda e: e.copy(out=R.xT.t[:].rearrange("p a b -> p (a b)"), in_=R.pT.t[:]), reads=[R.pT.b], writes=[R.xT.b])
        for ko in range(8):
            S.op("pe", lambda e, ko=ko: e.matmul(R.pS.t[:, 0:NE], lhsT=R.xT.t[:, ko, :], rhs=R.rw.t[:, ko, :], start=(ko == 0), stop=(ko == 7)),
                 reads=[R.xT.b, R.rw.b], writes=[R.pS.b], sig=(ko == 7))
        S.op("dve", lambda e: e.tensor_tensor(out=R.lg.t[:], in0=R.pS.t[:, 0:NE], in1=R.rb.t[:], op=ALU.add),
             reads=[R.pS.b, R.rb.b], writes=[R.lg.b])
        S.op("dve", lambda e: e.max(out=R.v8.t[:], in_=R.lg.t[:]), reads=[R.lg.b], writes=[R.v8.b])
        S.op("dve", lambda e: e.max_index(out=R.i8.t[:], in_max=R.v8.t[:], in_values=R.lg.t[:]), reads=[R.lg.b, R.v8.b], writes=[R.i8.b])
        S.op("dve", lambda e: e.tensor_copy(out=R.i8f.t[:], in_=R.i8.t[:]), reads=[R.i8.b], writes=[R.i8f.b])
        S.op("dve", lambda e: e.tensor_tensor(out=R.mk.t[:], in0=iota_e.t[:].unsqueeze(1).to_broadcast([128, 4, NE]),
                                              in1=R.i8f.t[:, 0:4].unsqueeze(2).to_broadcast([128, 4, NE]), op=ALU.is_equal),
             reads=[iota_e.b, R.i8f.b], writes=[R.mk.b])
        S.op("dve", lambda e: e.tensor_reduce(out=R.msum.t[:], in_=R.mk.t[:].rearrange("p k e -> p e k"), axis=mybir.AxisListType.X, op=ALU.add),
             reads=[R.mk.b], writes=[R.msum.b])
        S.op("dve", lambda e: e.tensor_copy(out=R.msb.t[:], in_=R.msum.t[:]), reads=[R.msum.b], writes=[R.msb.b])
        S.op("pe", lambda e: e.matmul(R.pS.t[:, 64:64 + NE], lhsT=ustr_b.t[:], rhs=R.msb.t[:], start=True, stop=True),
             reads=[ustr_b.b, R.msb.b, R.lg.b], writes=[R.pS.b], sig=False)
        S.op("pe", lambda e: e.matmul(R.pS.t[:, 128:128 + NE], lhsT=ones_b.t[:], rhs=R.msb.t[:], start=True, stop=True),
             reads=[ones_b.b, R.msb.b, R.lg.b], writes=[R.pS.b])
        S.op("dve", lambda e: e.tensor_tensor(out=R.pos.t[:], in0=R.pS.t[:, 64:64 + NE], in1=cbase.t[:], op=ALU.add),
             reads=[R.pS.b, cbase.b], writes=[R.pos.b])
        S.op("dve", lambda e: e.tensor_tensor(out=cbase.t[:], in0=R.pS.t[:, 128:128 + NE], in1=cbase.t[:], op=ALU.add),
             reads=[R.pS.b, cbase.b], writes=[cbase.b])
        S.op("dve", lambda e: e.tensor_scalar(out=R.junk.t[:], in0=R.pos.t[:], scalar1=float(CAP), scalar2=1.0e6,
                                              op0=ALU.is_ge, op1=ALU.mult), reads=[R.pos.b], writes=[R.junk.b])
        S.op("dve", lambda e: e.tensor_tensor(out=R.pos.t[:], in0=R.pos.t[:], in1=R.junk.t[:], op=ALU.add),
             reads=[R.pos.b, R.junk.b], writes=[R.pos.b])
        S.op("dve", lambda e: e.tensor_tensor(out=R.pos.t[:], in0=R.pos.t[:], in1=ecap.t[:], op=ALU.add),
             reads=[R.pos.b, ecap.b], writes=[R.pos.b])
        S.op("dve", lambda e: e.tensor_tensor(out=R.mk.t[:], in0=R.mk.t[:], in1=R.pos.t[:].unsqueeze(1).to_broadcast([128, 4, NE]), op=ALU.mult),
             reads=[R.mk.b, R.pos.b], writes=[R.mk.b])
        S.op("dve", lambda e: e.tensor_reduce(out=R.dst.t[:], in_=R.mk.t[:], axis=mybir.AxisListType.X, op=ALU.add),
             reads=[R.mk.b], writes=[R.dst.b])
        S.op("dve", lambda e: e.tensor_copy(out=dest_i.t[:, i * 4:(i + 1) * 4], in_=R.dst.t[:]), reads=[R.dst.b], writes=[dest_b[i]])
        S.op("dve", lambda e: e.tensor_scalar(out=R.nv0.t[:], in0=R.v8.t[:, 0:1], scalar1=-1.0, scalar2=None, op0=ALU.mult),
             reads=[R.v8.b], writes=[R.nv0.b])
        S.op("act", lambda e: e.activation(out=R.ex.t[:], in_=R.v8.t[:, 0:4], func=AF.Exp, bias=R.nv0.t[:, 0:1], scale=1.0),
             reads=[R.v8.b, R.nv0.b], writes=[R.ex.b])
        S.op("dve", lambda e: e.reduce_sum(out=R.ssum.t[:], in_=R.ex.t[:], axis=mybir.AxisListType.X), reads=[R.ex.b], writes=[R.ssum.b])
        S.op("dve", lambda e: e.reciprocal(out=R.ssum.t[:], in_=R.ssum.t[:]), reads=[R.ssum.b], writes=[R.ssum.b])
        S.op("dve", lambda e: e.tensor_scalar(out=R.val.t[:], in0=R.dst.t[:], scalar1=float(NSLOT), scalar2=None, op0=ALU.is_lt),
             reads=[R.dst.b], writes=[R.val.b])
        S.op("dve", lambda e: e.tensor_scalar(out=R.ex.t[:], in0=R.ex.t[:], scalar1=R.ssum.t[:, 0:1], scalar2=None, op0=ALU.mult),
             reads=[R.ex.b, R.ssum.b], writes=[R.ex.b])
        S.op("dve", lambda e: e.tensor_tensor(out=gate_t.t[:, i * 4:(i + 1) * 4], in0=R.ex.t[:], in1=R.val.t[:], op=ALU.mult),
             reads=[R.ex.b, R.val.b], writes=[gate_b[i]])
        xb = R.xb[i % R.nbuf]
        S.op("act", lambda e: e.copy(out=xb.t[:], in_=r.t[:]), reads=[r.b], writes=[xb.b])
        for k in range(4):
            S.dma("pool", lambda e, k=k: e.indirect_dma_start(
                out=xg.t[:, :], out_offset=bass.IndirectOffsetOnAxis(ap=dest_i.t[:, i * 4 + k:i * 4 + k + 1], axis=0),
                in_=xb.t[:, :], in_offset=None, bounds_check=breg, oob_is_err=False),
                reads=[xb.b, dest_b[i]], writes=[xg.b], after=[G.xg_zero_tok], sembuf=xb.b)

    def moe_phase(layer, xin, xout):
        with contextlib.ExitStack() as es:
            NR = 10
            ring = [sbt(es, "ring%d" % i, [128, 8, 512], BF16) for i in range(NR)]
            stg = [sbt(es, "stg%d" % i, [128, 2, 512], F32) for i in range(8)]
            xl = [sbt(es, "xl%d" % i, [128, NTC, D], BF16) for i in range(2)]
            xgT = [sbt(es, "xgT%d" % i, [128, 8, CAP], BF16) for i in range(2)]
            actT = sbt(es, "actT", [128, 8, CAP], BF16)
            gsb = [sbt(es, "gsb%d" % i, [128, CAP], F32) for i in range(2)]
            sgb = [sbt(es, "sgb%d" % i, [128, CAP], F32) for i in range(2)]
            t1b = [sbt(es, "t1b%d" % i, [128, CAP], F32) for i in range(2)]
            ysb = [sbt(es, "ysb%d" % i, [128, D], F32) for i in range(2)]
            bdn = [sbt(es, "bdn%d" % i, [128, D], F32) for i in range(2)]
            bup = sbt(es, "bup", [128, NE * 16], F32)
            bup1 = sbt(es, "bup1", [128, NE * 16], F32)
            pg = pst(es, "pg", [128, 2, 512], F32)
            pl = pst(es, "pl", [128, 2, 512], F32)
            py = pst(es, "py", [128, 2, 512], F32)
            ptrs = [pst(es, "ptr%d" % i, [128, 8, 128], BF16) for i in range(2)]
            pyb = [Buf("py_h0"), Buf("py_h1")]

            S.dma("sp", lambda e: e.dma_start(out=bup.t[:], in_=W["bup"][:, layer * NE * 16:(layer + 1) * NE * 16]), writes=[bup.b])
            S.op("dve", lambda e: e.tensor_scalar(out=bup1.t[:], in0=bup.t[:], scalar1=1.0, scalar2=None, op0=ALU.add),
                 reads=[bup.b], writes=[bup1.b])

            corder = [("up", 0), ("up", 2), ("up", 1), ("up", 3), ("dn", 0), ("dn", 1)]
            nchunks = NE * 6
            pcount = [0]

            cast_q = []
            step = [0]

            def emit_casts(pred, limit=99):
                n = 0
                k = 0
                while k < len(cast_q) and n < limit:
                    if pred(cast_q[k]):
                        cast_q.pop(k)[3]()
                        n += 1
                    else:
                        break

            def tick():
                step[0] += 1
                emit_casts(lambda c: c[0] <= step[0], limit=2)

            def flush_chunk(j):
                emit_casts(lambda c: c[1] <= j)

            def load_chunk(j):
                if j >= nchunks:
                    return
                e_, c = divmod(j, 6)
                kind, gi = corder[c]
                src = W["up"][layer, e_] if kind == "up" else W["dn"][layer, e_]
                slot = ring[j % NR]
                for q in range(4):
                    si = pcount[0] % len(stg)
                    st = stg[si]
                    pcount[0] += 1
                    while any(c[2] == si for c in cast_q):
                        cast_q.pop(0)[3]()
                    S.dma("sp", lambda e: e.dma_start(
                        out=st.t[:], in_=src[q * 256:(q + 1) * 256, gi * 512:(gi + 1) * 512].rearrange("(ko p) f -> p ko f", p=128)),
                        writes=[st.b])

                    def emit(st=st, q=q, slot=slot):
                        S.op("act", lambda e: e.copy(out=slot.t[:, q * 2:(q + 1) * 2, :], in_=st.t[:]), reads=[st.b], writes=[slot.b])
                    cast_q.append([step[0] + 2 + q // 2, j, si, emit])

            def load_x(e_):
                if e_ >= NE:
                    return
                S.dma("sp", lambda e: e.dma_start(out=xl[e_ % 2].t[:], in_=xg.t[e_ * CAP:(e_ + 1) * CAP, :].rearrange("(t p) d -> p t d", p=128)),
                      reads=[xg.b], writes=[xl[e_ % 2].b])

            def load_bdn(e_):
                if e_ >= NE:
                    return
                S.dma("sp", lambda e: e.dma_start(out=bdn[e_ % 2].t[:], in_=W["bdn"][layer, e_].partition_broadcast(128)),
                      writes=[bdn[e_ % 2].b])

            def trans_tile(e_, tt):
                if e_ >= NE:
                    return
                src = xl[e_ % 2]
                dst = xgT[e_ % 2]
                ptr = ptrs[tt % 2]
                for ko in range(8):
                    S.op("pe", lambda e, ko=ko: e.transpose(ptr.t[:, ko, :], src.t[:, tt, ko * 128:(ko + 1) * 128], ident_b.t[:]),
                         reads=[src.b, ident_b.b], writes=[ptr.b], sig=(ko == 7))
                S.op("act", lambda e: e.copy(out=dst.t[:, :, tt * 128:(tt + 1) * 128], in_=ptr.t[:]),
                     reads=[ptr.b], writes=[dst.b])

            def transposes(e_):
                for tt in range(NTC):
                    trans_tile(e_, tt)

            def down_tile(e_, tt):
                base = e_ * 6
                wd = [ring[(base + 4) % NR], ring[(base + 5) % NR]]
                y_ = ysb[tt % 2]
                for half in range(2):
                    for fo in range(8):
                        S.op("pe", lambda e, fo=fo: e.matmul(
                            py.t[:, half, :], lhsT=actT.t[:, fo, tt * 128:(tt + 1) * 128], rhs=wd[half].t[:, fo, :],
                            start=(fo == 0), stop=(fo == 7)),
                            reads=[actT.b, wd[half].b], writes=[pyb[half]], sig=(fo == 7))
                    S.op("dve", lambda e: e.tensor_tensor(out=y_.t[:, half * 512:(half + 1) * 512], in0=py.t[:, half, :],
                                                          in1=bdn[e_ % 2].t[:, half * 512:(half + 1) * 512], op=ALU.add),
                         reads=[pyb[half], bdn[e_ % 2].b] + ([y_.b] if half else []), writes=[y_.b])
                S.dma("pool", lambda e: e.dma_start(out=yg.t[e_ * CAP + tt * 128:e_ * CAP + (tt + 1) * 128, :], in_=y_.t[:]),
                      reads=[y_.b], writes=[yg.b], sembuf=y_.b)

            for j in range(NR):
                load_chunk(j)
                flush_chunk(j)
            load_x(0)
            load_bdn(0)
            transposes(0)
            load_x(1)
            load_bdn(1)
            HN = CAP // 2
            for e_ in range(NE):
                xT = xgT[e_ % 2]
                base = e_ * 6
                flush_chunk(base + 1)
                for j in range(8):
                    if j == 4:
                        flush_chunk(base + 3)
                    tick()
                    cg = ring[(base + (0 if j < 4 else 2)) % NR]
                    cl = ring[(base + (1 if j < 4 else 3)) % NR]
                    col = (j % 4) * 128
                    for (pp, cw) in ((pg, cg), (pl, cl)):
                        for half in range(2):
                            for ko in range(8):
                                S.op("pe", lambda e, pp=pp, cw=cw, half=half, ko=ko: e.matmul(
                                    pp.t[:, half, 0:HN], lhsT=cw.t[:, ko, col:col + 128], rhs=xT.t[:, ko, half * HN:(half + 1) * HN],
                                    start=(ko == 0), stop=(ko == 7)),
                                    reads=[cw.b, xT.b], writes=[pp.b], sig=(half == 1 and ko == 7))
                    g_ = gsb[j % 2]
                    s_ = sgb[j % 2]
                    t_ = t1b[j % 2]
                    bcol = e_ * 16 + j
                    S.op("dve", lambda e, g_=g_, bcol=bcol: e.tensor_scalar(
                        out=g_.t[:].rearrange("p (a b) -> p a b", a=2), in0=pg.t[:, :, 0:HN], scalar1=bup.t[:, bcol:bcol + 1], scalar2=7.0,
                        op0=ALU.add, op1=ALU.min), reads=[pg.b, bup.b], writes=[g_.b])
                    S.op("act", lambda e, g_=g_, s_=s_: e.activation(out=s_.t[:], in_=g_.t[:], func=AF.Sigmoid, scale=1.702),
                         reads=[g_.b], writes=[s_.b])
                    S.op("dve", lambda e, t_=t_, bcol=bcol: e.tensor_scalar(
                        out=t_.t[:].rearrange("p (a b) -> p a b", a=2), in0=pl.t[:, :, 0:HN], scalar1=bup1.t[:, bcol + 8:bcol + 9], scalar2=-6.0,
                        op0=ALU.add, op1=ALU.max), reads=[pl.b, bup1.b], writes=[t_.b])
                    S.op("dve", lambda e, t_=t_, g_=g_: e.scalar_tensor_tensor(out=t_.t[:], in0=t_.t[:], scalar=8.0, in1=g_.t[:],
                                                                         op0=ALU.min, op1=ALU.mult), reads=[t_.b, g_.b], writes=[t_.b])
                    S.op("dve", lambda e, t_=t_, s_=s_, j=j: e.tensor_tensor(out=actT.t[:, j, :], in0=t_.t[:], in1=s_.t[:], op=ALU.mult),
                         reads=[t_.b, s_.b], writes=[actT.b])
                    if j == 3:
                        load_chunk(base + 0 + NR)
                        load_chunk(base + 1 + NR)
                    if j == 7:
                        load_chunk(base + 2 + NR)
                        load_chunk(base + 3 + NR)
                flush_chunk(base + 5)
                trans_tile(e_ + 1, 0)
                trans_tile(e_ + 1, 1)
                tick()
                down_tile(e_, 0)
                trans_tile(e_ + 1, 2)
                tick()
                down_tile(e_, 1)
                trans_tile(e_ + 1, 3)
                tick()
                down_tile(e_, 2)
                trans_tile(e_ + 1, 4)
                tick()
                down_tile(e_, 3)
                tick()
                down_tile(e_, 4)
                load_x(e_ + 2)
                load_chunk(base + 4 + NR)
                load_chunk(base + 5 + NR)
                load_bdn(e_ + 2)
            flush_chunk(nchunks)
        S.barrier()
        with contextlib.ExitStack() as es:
            E = make_epi(es, layer, 1)
            yk = [[sbt(es, "yk%d_%d" % (b, k), [128, D], F32) for k in range(4)] for b in range(2)]
            dg = [[sbt(es, "dg%d_%d" % (b, k), [128, 128], F32) for k in range(4)] for b in range(2)]
            dal = sbt(es, "dal", [128, 128], F32)
            S.op("dve", lambda e: e.tensor_scalar(out=dal.t[:], in0=ident_f.t[:], scalar1=ALPHA, scalar2=None, op0=ALU.mult),
                 reads=[ident_f.b], writes=[dal.b])
            pacc = [pst(es, "pacc%d" % b, [128, 2, 512], F32) for b in range(2)]

            def issue_gathers(i):
                b = i % 2
                for k in range(4):
                    if i < 2:
                        S.op("pool", lambda e, k=k: e.memset(yk[b][k].t[:], 0.0), writes=[yk[b][k].b])
                    S.dma("pool", lambda e, k=k: e.indirect_dma_start(
                        out=yk[b][k].t[:, :], out_offset=None, in_=yg.t[:, :],
                        in_offset=bass.IndirectOffsetOnAxis(ap=dest_i.t[:, i * 4 + k:i * 4 + k + 1], axis=0),
                        bounds_check=breg, oob_is_err=False), reads=[yg.b, dest_b[i]], writes=[yk[b][k].b])
                for k in range(4):
                    S.op("dve", lambda e, k=k: e.tensor_scalar(out=dg[b][k].t[:], in0=ident_f.t[:], scalar1=gate_t.t[:, i * 4 + k:i * 4 + k + 1],
                                                               scalar2=None, op0=ALU.mult), reads=[ident_f.b, gate_b[i]], writes=[dg[b][k].b])

            issue_gathers(0)
            epi_load(E, 0, xin)
            for i in range(NTILE):
                b = i % 2
                if i + 1 < NTILE:
                    issue_gathers(i + 1)
                    epi_load(E, i + 1, xin)

                def add_fn(r, xt, b=b, i=i):
                    ps = pacc[b]
                    for half in range(2):
                        cs = slice(half * 512, (half + 1) * 512)
                        S.op("pe", lambda e: e.matmul(ps.t[:, half, :], lhsT=dal.t[:], rhs=xt.t[:, cs], start=True, stop=False),
                             reads=[dal.b, xt.b], writes=[ps.b], sig=False)
                        for k in range(4):
                            S.op("pe", lambda e, k=k: e.matmul(ps.t[:, half, :], lhsT=dg[b][k].t[:], rhs=yk[b][k].t[:, cs], start=False, stop=(k == 3)),
                                 reads=[dal.b, xt.b] + [t_.b for t_ in dg[b]] + [t_.b for t_ in yk[b]], writes=[ps.b],
                                 sig=(half == 1 and k == 3))
                    return ps
                epilogue(E, i, xin, xout, add_fn, preloaded=True, store_q="act")
        S.barrier()

    G.S = S
    G.nc = nc
    G.es_top = es_top
    G.xs = xs
    G.W = W
    G.sbt = sbt
    G.pst = pst
    G.make_epi = make_epi
    G.epilogue = epilogue
    G.make_router = make_router
    G.route_tile = route_tile
    G.consts = dict(ident_f=ident_f, ident_b=ident_b, uincl_f=uincl_f, ones_f=ones_f, ones_b=ones_b)

    for st in stages:
        if st == "ab":
            mixer_ab_phase(G, xs["x0"], xs["x1"])
        elif st == "moe0":
            moe_phase(0, xs["x1"], xs["x2"])
        elif st == "cf":
            conformer_phase(G, xs["x2"], xs["x3"])
        elif st == "moe1":
            moe_phase(1, xs["x3"], xs["out"])
        elif st == "route0":
            route_only_phase(G, 0, xs["x1"])
        elif st == "route1":
            route_only_phase(G, 1, xs["x3"])

    S.barrier(engines=["sp"])
    es_top.close()
    S.close()
    return nc


def route_only_phase(G, layer, xin):
    S = G.S
    with contextlib.ExitStack() as es:
        pT = G.pst(es, "rpT", [128, 1024], F32)
        pS = G.pst(es, "rpS", [128, 512], F32)
        R = G.make_router(es, layer, pS, pT)
        rt = [G.sbt(es, "rot%d" % i, [128, D], F32) for i in range(2)]
        for i in range(NTILE):
            r = rt[i % 2]
            S.dma("sp", lambda e: e.dma_start(out=r.t[:], in_=xin.t[i * 128:(i + 1) * 128, :]), reads=[xin.b], writes=[r.b])
            G.route_tile(R, i, r)
    S.barrier()


def load_weight_bf16(G, es_stage, dst, src, ncols, nm, engines=("dve", "act")):
    S = G.S
    stg = [G.sbt(es_stage, "wst_%s%d" % (nm, i), [128, ncols], F32) for i in range(2)]
    for ko in range(8):
        st = stg[ko % 2]
        S.dma("sp", lambda e: e.dma_start(out=st.t[:], in_=src[ko * 128:(ko + 1) * 128, :]), writes=[st.b])
        if engines[ko % 2] == "dve":
            S.op("dve", lambda e: e.tensor_copy(out=dst.t[:, ko, :], in_=st.t[:]), reads=[st.b], writes=[dst.b])
        else:
            S.op("act", lambda e: e.copy(out=dst.t[:, ko, :], in_=st.t[:]), reads=[st.b], writes=[dst.b])


def build_xT(G, X, xin, blk):
    S = G.S
    c = G.consts
    for tl in range(4):
        i = blk * 4 + tl
        S.dma("sp", lambda e: e.dma_start(out=X.xl.t[:], in_=xin.t[i * 128:(i + 1) * 128, :]), reads=[xin.b], writes=[X.xl.b])
        S.op("act", lambda e: e.copy(out=X.xlb.t[:], in_=X.xl.t[:]), reads=[X.xl.b], writes=[X.xlb.b])
        for ko in range(8):
            S.op("pe", lambda e: e.transpose(X.PT.t[:, ko * 128:(ko + 1) * 128], X.xlb.t[:, ko * 128:(ko + 1) * 128], c["ident_b"].t[:]),
                 reads=[X.xlb.b, c["ident_b"].b], writes=[X.PT.b], sig=(ko == 7))
        S.op("dve", lambda e: e.tensor_copy(out=X.xT.t[:, :, tl * 128:(tl + 1) * 128], in_=X.PT.t[:].rearrange("p (a b) -> p a b", a=8)),
             reads=[X.PT.b], writes=[X.xT.b])


def mixer_ab_phase(G, xin, xout):
    S, W, sbt, pst = G.S, G.W, G.sbt, G.pst
    c = G.consts
    ident_b, uincl_f, ones_f = c["ident_b"], c["uincl_f"], c["ones_f"]
    with contextlib.ExitStack() as es:
        w_in = sbt(es, "w_in", [128, 8, 3080], BF16)
        w_out = sbt(es, "w_out", [128, 8, D], BF16)
        with contextlib.ExitStack() as es2:
            load_weight_bf16(G, es2, w_in, W["w_in"], 3080, "in")
            load_weight_bf16(G, es2, w_out, W["w_out"], D, "out")
            S.barrier()
        PAB = pst(es, "PAB", [128, 1024], F32)
        PC = pst(es, "PC", [128, 512], F32)
        PD = pst(es, "PD", [128, 512], F32)
        PT = pst(es, "PT", [128, 1024], BF16)
        PE_ = pst(es, "PE", [128, 512], F32)
        PF = pst(es, "PF", [128, 512], F32)
        PG = pst(es, "PG", [128, 512], F32)
        E = G.make_epi(es, 0, 0)
        bif = sbt(es, "bif", [128, 8], F32)
        cw = sbt(es, "cw", [128, 12], F32)
        hg = sbt(es, "hg", [128, 512], F32)
        S.dma("sp", lambda e: e.dma_start(out=bif.t[:], in_=W["ab_bif"].partition_broadcast(128)), writes=[bif.b])
        S.dma("sp", lambda e: e.dma_start(out=cw.t[:], in_=W["ab_conv"]), writes=[cw.b])
        S.dma("sp", lambda e: e.dma_start(out=hg.t[:], in_=W["ab_hg"].partition_broadcast(128)), writes=[hg.b])
        maskT = sbt(es, "maskT", [128, 512], F32)
        for h in range(4):
            S.op("pool", lambda e: e.tensor_copy(out=maskT.t[:, h * 128:(h + 1) * 128], in_=uincl_f.t[:]), reads=[uincl_f.b], writes=[maskT.b])
        tmpc = sbt(es, "tmpc", [128, 512], F32)
        y1 = sbt(es, "y1", [128, 512], F32)
        LNK = math.log(0.125)
        lnk = sbt(es, "lnk", [128, 1], F32)
        S.op("pool", lambda e: e.memset(lnk.t[:], LNK), writes=[lnk.b])
        hneps = sbt(es, "hneps", [128, 1], F32)
        S.op("pool", lambda e: e.memset(hneps.t[:], HN_EPS), writes=[hneps.b])
        PS = []
        for sq in range(2):
            P = Ctx()
            X = Ctx()
            X.xl = sbt(es, "xl", [128, D], F32)
            X.xlb = sbt(es, "xlb", [128, D], BF16)
            X.xT = sbt(es, "xT", [128, 8, 512], BF16)
            X.PT = PT
            P.X = X
            P.R = G.make_router(es, 0, PE_, PAB, nbuf=1)
            P.u = [sbt(es, "u%d" % i, [128, 514], F32) for i in range(4)]
            P.yT = sbt(es, "yT", [128, 4, 512], BF16)
            for nm, shp, dt in (("fi", [128, 8], F32), ("e1", [128, 4], F32), ("lf", [128, 4], F32), ("qsc", [128, 4], F32),
                                ("ksc", [128, 4], F32), ("t4", [128, 4], F32), ("eg", [128, 4], F32), ("qs", [128, 256], BF16),
                                ("ks", [128, 256], BF16), ("vp", [128, 4, 129], BF16), ("so", [128, 512], F32),
                                ("qkT", [64, 8, 128], BF16), ("SmT", [128, 512], BF16), ("Cst", [64, 4, 129], F32),
                                ("Cb", [64, 4, 129], BF16), ("dn", [128, 4], F32), ("hh", [128, 512], F32), ("hst", [128, 24], F32),
                                ("hmv", [128, 8], F32), ("hrs", [128, 4], F32), ("hA", [128, 512], BF16), ("hAT", [128, 4, 128], BF16)):
                setattr(P, nm, sbt(es, nm, shp, dt))
            S.op("pool", lambda e: e.memset(P.vp.t[:], 1.0), writes=[P.vp.b])
            PS.append(P)

        def stream(sq):
            P = PS[sq]
            X, R, u, yTb = P.X, P.R, P.u, P.yT
            fi, e1, lf, qsc, ksc, t4, eg, qs, ks, vp, so = P.fi, P.e1, P.lf, P.qsc, P.ksc, P.t4, P.eg, P.qs, P.ks, P.vp, P.so
            qkT, SmT, Cst, Cb, dn, hh, hst, hmv, hrs, hA, hAT = P.qkT, P.SmT, P.Cst, P.Cb, P.dn, P.hh, P.hst, P.hmv, P.hrs, P.hA, P.hAT
            for blk in range(sq * 4, sq * 4 + 4):
                if blk % 4 == 0:
                    for cc in range(4):
                        S.op("pool", lambda e: e.memset(u[cc].t[:, 0:2], 0.0), writes=[u[cc].b])
                    S.op("pool", lambda e: e.memset(Cst.t[:], 0.0), writes=[Cst.b])
                    S.op("pool", lambda e: e.memset(Cb.t[:], 0.0), writes=[Cb.b])
                build_xT(G, X, xin, blk)
                yield
                xT = X.xT
                for cc in range(4):
                    for (bank, c0) in ((PE_, 1544 + 512 + cc * 128), (PF, 1544 + 1024 + cc * 128), (PG, 1544 + cc * 128)):
                        for ko in range(8):
                            S.op("pe", lambda e: e.matmul(bank.t[:], lhsT=w_in.t[:, ko, c0:c0 + 128], rhs=xT.t[:, ko, :], start=(ko == 0), stop=(ko == 7)),
                                 reads=[w_in.b, xT.b], writes=[bank.b], sig=(ko == 7))
                    uu = u[cc]
                    S.op("act", lambda e: e.copy(out=tmpc.t[:], in_=PE_.t[:]), reads=[PE_.b], writes=[tmpc.b])
                    S.op("dve", lambda e: e.tensor_tensor(out=uu.t[:, 2:514], in0=tmpc.t[:], in1=PF.t[:], op=ALU.mult),
                         reads=[tmpc.b, PF.b], writes=[uu.b])
                    S.op("dve", lambda e: e.tensor_scalar(out=y1.t[:], in0=uu.t[:, 2:514], scalar1=cw.t[:, 8 + cc:9 + cc], scalar2=None, op0=ALU.mult),
                         reads=[uu.b, cw.b], writes=[y1.b])
                    S.op("dve", lambda e: e.scalar_tensor_tensor(out=y1.t[:], in0=uu.t[:, 1:513], scalar=cw.t[:, 4 + cc:5 + cc], in1=y1.t[:],
                                                                 op0=ALU.mult, op1=ALU.add), reads=[uu.b, cw.b, y1.b], writes=[y1.b])
                    S.op("dve", lambda e: e.scalar_tensor_tensor(out=y1.t[:], in0=uu.t[:, 0:512], scalar=cw.t[:, cc:cc + 1], in1=y1.t[:],
                                                                 op0=ALU.mult, op1=ALU.add), reads=[uu.b, cw.b, y1.b], writes=[y1.b])
                    S.op("dve", lambda e: e.tensor_tensor(out=yTb.t[:, cc, :], in0=y1.t[:], in1=PG.t[:], op=ALU.mult),
                         reads=[y1.b, PG.b], writes=[yTb.b])
                    S.op("dve", lambda e: e.tensor_copy(out=uu.t[:, 0:2], in_=uu.t[:, 512:514]), reads=[uu.b], writes=[uu.b])
                    yield
                yield
                for tl in range(4):
                    i = blk * 4 + tl
                    tc0 = tl * 128
                    for (out_ap, bank, c0, c1) in ((PAB.t[:, 0:512], PAB, 0, 512), (PAB.t[:, 512:1024], PAB, 512, 1024),
                                                   (PC.t[:], PC, 1024, 1536), (PD.t[:, 0:8], PD, 1536, 1544)):
                        for ko in range(8):
                            S.op("pe", lambda e: e.matmul(out_ap, lhsT=xT.t[:, ko, tc0:tc0 + 128], rhs=w_in.t[:, ko, c0:c1], start=(ko == 0), stop=(ko == 7)),
                                 reads=[w_in.b, xT.b], writes=[bank.b], sig=(ko == 7 and c0 != 0))
                    S.op("dve", lambda e: e.tensor_tensor(out=fi.t[:], in0=PD.t[:, 0:8], in1=bif.t[:], op=ALU.add), reads=[PD.b, bif.b], writes=[fi.b])
                    S.op("act", lambda e: e.activation(out=e1.t[:], in_=fi.t[:, 4:8], func=AF.Exp, scale=-1.0), reads=[fi.b], writes=[e1.b])
                    S.op("act", lambda e: e.activation(out=lf.t[:], in_=e1.t[:], func=AF.Ln, bias=1.0), reads=[e1.b], writes=[lf.b])
                    S.op("pe", lambda e: e.matmul(PD.t[:, 16:20], lhsT=uincl_f.t[:], rhs=lf.t[:], start=True, stop=True),
                         reads=[uincl_f.b, lf.b], writes=[PD.b], sig=False)
                    S.op("pe", lambda e: e.matmul(PD.t[:, 20:24], lhsT=ones_f.t[:], rhs=lf.t[:], start=True, stop=True),
                         reads=[ones_f.b, lf.b], writes=[PD.b])
                    S.op("act", lambda e: e.activation(out=qsc.t[:], in_=PD.t[:, 16:20], func=AF.Exp, scale=-1.0), reads=[PD.b], writes=[qsc.b])
                    S.op("dve", lambda e: e.tensor_tensor(out=t4.t[:], in0=PD.t[:, 16:20], in1=fi.t[:, 0:4], op=ALU.add), reads=[PD.b, fi.b], writes=[t4.b])
                    S.op("act", lambda e: e.activation(out=ksc.t[:], in_=t4.t[:], func=AF.Exp, bias=lnk.t[:, 0:1], scale=1.0), reads=[t4.b, lnk.b], writes=[ksc.b])
                    S.op("act", lambda e: e.activation(out=eg.t[:], in_=PD.t[:, 20:24], func=AF.Exp, scale=-1.0), reads=[PD.b], writes=[eg.b])
                    S.op("dve", lambda e: e.tensor_tensor(out=qs.t[:].rearrange("p (h d) -> p h d", h=4),
                                                          in0=PAB.t[:, 0:256].rearrange("p (h d) -> p h d", h=4),
                                                          in1=qsc.t[:, 0:4].unsqueeze(2).to_broadcast([128, 4, 64]), op=ALU.mult),
                         reads=[PAB.b, qsc.b], writes=[qs.b])
                    S.op("dve", lambda e: e.tensor_tensor(out=ks.t[:].rearrange("p (h d) -> p h d", h=4),
                                                          in0=PAB.t[:, 256:512].rearrange("p (h d) -> p h d", h=4),
                                                          in1=ksc.t[:, 0:4].unsqueeze(2).to_broadcast([128, 4, 64]), op=ALU.mult),
                         reads=[PAB.b, ksc.b], writes=[ks.b])
                    S.op("act", lambda e: e.copy(out=vp.t[:, :, 0:128], in_=PAB.t[:, 512:1024].rearrange("p (a b) -> p a b", a=4)),
                         reads=[PAB.b], writes=[vp.b])
                    S.op("act", lambda e: e.activation(out=so.t[:], in_=PC.t[:], func=AF.Sigmoid), reads=[PC.b], writes=[so.b])
                    yield
                    PT64 = PT.t[0:64, :].rearrange("p (a b) -> p a b", a=8)
                    for h in range(4):
                        S.op("pe", lambda e: e.transpose(PT64[:, h, :], qs.t[:, h * 64:(h + 1) * 64], ident_b.t[:]),
                             reads=[qs.b, ident_b.b], writes=[PT.b], sig=False)
                    for h in range(4):
                        S.op("pe", lambda e: e.transpose(PT64[:, 4 + h, :], ks.t[:, h * 64:(h + 1) * 64], ident_b.t[:]),
                             reads=[ks.b, qs.b, ident_b.b], writes=[PT.b], sig=(h == 3))
                    S.op("act", lambda e: e.copy(out=qkT.t[:], in_=PT64), reads=[PT.b], writes=[qkT.b])
                    yield
                    for h in range(4):
                        S.op("pe", lambda e: e.matmul(PE_.t[:, h * 128:(h + 1) * 128], lhsT=qkT.t[:, 4 + h, :], rhs=qkT.t[:, h, :], start=True, stop=True),
                             reads=[qkT.b], writes=[PE_.b], sig=(h == 3))
                    S.op("dve", lambda e: e.tensor_tensor(out=SmT.t[:], in0=PE_.t[:], in1=maskT.t[:], op=ALU.mult), reads=[PE_.b, maskT.b], writes=[SmT.b])
                    yield
                    for h in range(4):
                        bank = PF if h < 2 else PG
                        o_ap = bank.t[:, (h % 2) * 129:(h % 2 + 1) * 129]
                        S.op("pe", lambda e: e.matmul(o_ap, lhsT=SmT.t[:, h * 128:(h + 1) * 128], rhs=vp.t[:, h, :], start=True, stop=False),
                             reads=[SmT.b, vp.b], writes=[bank.b], sig=False)
                        S.op("pe", lambda e: e.matmul(o_ap, lhsT=qkT.t[:, h, :], rhs=Cb.t[:, h, :], start=False, stop=True),
                             reads=[qkT.b, Cb.b, SmT.b, vp.b], writes=[bank.b], sig=(h % 2 == 1))
                    for h in range(4):
                        o_ap = PAB.t[0:64, (h // 2) * 512 + (h % 2) * 129:(h // 2) * 512 + (h % 2 + 1) * 129]
                        S.op("pe", lambda e: e.matmul(o_ap, lhsT=ks.t[:, h * 64:(h + 1) * 64], rhs=vp.t[:, h, :], start=True, stop=True),
                             reads=[ks.b, vp.b], writes=[PAB.b], sig=(h == 3))
                    dCv = PAB.t[0:64, :].rearrange("p (b x) -> p b x", b=2)[:, :, 0:258].rearrange("p b (h c) -> p b h c", c=129)
                    S.op("dve", lambda e: e.tensor_tensor(out=Cst.t[:].rearrange("p (b h) c -> p b h c", b=2),
                                                          in0=Cst.t[:].rearrange("p (b h) c -> p b h c", b=2), in1=dCv, op=ALU.add),
                         reads=[PAB.b, Cst.b], writes=[Cst.b])
                    S.op("dve", lambda e: e.tensor_tensor(out=Cst.t[:], in0=Cst.t[:], in1=eg.t[0:64, 0:4].unsqueeze(2).to_broadcast([64, 4, 129]), op=ALU.mult),
                         reads=[Cst.b, eg.b], writes=[Cst.b])
                    S.op("act", lambda e: e.copy(out=Cb.t[:], in_=Cst.t[:]), reads=[Cst.b], writes=[Cb.b])
                    for bi, bank in enumerate((PF, PG)):
                        den = bank.t[:, 0:258].rearrange("p (h c) -> p h c", c=129)[:, :, 128]
                        S.op("act", lambda e: e.activation(out=dn.t[:, 2 * bi:2 * bi + 2], in_=den, func=AF.Abs), reads=[bank.b], writes=[dn.b])
                    S.op("dve", lambda e: e.tensor_scalar(out=dn.t[:], in0=dn.t[:], scalar1=1.0, scalar2=None, op0=ALU.max), reads=[dn.b], writes=[dn.b])
                    S.op("dve", lambda e: e.reciprocal(out=dn.t[:], in_=dn.t[:]), reads=[dn.b], writes=[dn.b])
                    for bi, bank in enumerate((PF, PG)):
                        S.op("dve", lambda e: e.tensor_tensor(out=hh.t[:, bi * 256:(bi + 1) * 256].rearrange("p (h d) -> p h d", h=2),
                                                              in0=bank.t[:, 0:258].rearrange("p (h c) -> p h c", c=129)[:, :, 0:128],
                                                              in1=dn.t[:, 2 * bi:2 * bi + 2].unsqueeze(2).to_broadcast([128, 2, 128]), op=ALU.mult),
                             reads=[bank.b, dn.b], writes=[hh.b])
                    for h in range(4):
                        S.op("dve", lambda e: e.bn_stats(out=hst.t[:, h * 6:(h + 1) * 6], in_=hh.t[:, h * 128:(h + 1) * 128]), reads=[hh.b], writes=[hst.b])
                    for h in range(4):
                        S.op("dve", lambda e: e.bn_aggr(out=hmv.t[:, h * 2:(h + 1) * 2], in_=hst.t[:, h * 6:(h + 1) * 6]), reads=[hst.b], writes=[hmv.b])
                    S.op("act", lambda e: e.activation(out=hrs.t[:], in_=hmv.t[:].rearrange("p (h c) -> p h c", c=2)[:, :, 1], func=AF.Ln, bias=hneps.t[:, 0:1], scale=1.0),
                         reads=[hmv.b, hneps.b], writes=[hrs.b])
                    S.op("act", lambda e: e.activation(out=hrs.t[:], in_=hrs.t[:], func=AF.Exp, scale=-0.5), reads=[hrs.b], writes=[hrs.b])
                    hh3 = hh.t[:].rearrange("p (h d) -> p h d", h=4)
                    S.op("dve", lambda e: e.tensor_tensor(out=hh3, in0=hh3, in1=hmv.t[:].rearrange("p (h c) -> p h c", c=2)[:, :, 0:1].to_broadcast([128, 4, 128]),
                                                          op=ALU.subtract), reads=[hh.b, hmv.b], writes=[hh.b])
                    S.op("dve", lambda e: e.tensor_tensor(out=hh3, in0=hh3, in1=hrs.t[:, 0:4].unsqueeze(2).to_broadcast([128, 4, 128]), op=ALU.mult),
                         reads=[hh.b, hrs.b], writes=[hh.b])
                    S.op("dve", lambda e: e.tensor_tensor(out=hh.t[:], in0=hh.t[:], in1=hg.t[:], op=ALU.mult), reads=[hh.b, hg.b], writes=[hh.b])
                    S.op("dve", lambda e: e.tensor_tensor(out=hA.t[:], in0=hh.t[:], in1=so.t[:], op=ALU.mult), reads=[hh.b, so.b], writes=[hA.b])
                    for cq in range(4):
                        S.op("pe", lambda e: e.transpose(PT.t[:, cq * 128:(cq + 1) * 128], hA.t[:, cq * 128:(cq + 1) * 128], ident_b.t[:]),
                             reads=[hA.b, ident_b.b], writes=[PT.b], sig=(cq == 3))
                    S.op("act", lambda e: e.copy(out=hAT.t[:], in_=PT.t[:, 0:512].rearrange("p (a b) -> p a b", a=4)), reads=[PT.b], writes=[hAT.b])
                    yield
                    for half, bank in enumerate((PC, PD)):
                        for cq in range(8):
                            lhsT = hAT.t[:, cq, :] if cq < 4 else yTb.t[:, cq - 4, tc0:tc0 + 128]
                            S.op("pe", lambda e: e.matmul(bank.t[:], lhsT=lhsT, rhs=w_out.t[:, cq, half * 512:(half + 1) * 512], start=(cq == 0), stop=(cq == 7)),
                                 reads=[hAT.b, yTb.b, w_out.b], writes=[bank.b], sig=(cq == 7))

                    def add_fn(r, xt):
                        S.op("dve", lambda e: e.scalar_tensor_tensor(out=r.t[:, 0:512], in0=xt.t[:, 0:512], scalar=ALPHA, in1=PC.t[:], op0=ALU.mult, op1=ALU.add),
                             reads=[xt.b, PC.b], writes=[r.b])
                        S.op("dve", lambda e: e.scalar_tensor_tensor(out=r.t[:, 512:1024], in0=xt.t[:, 512:1024], scalar=ALPHA, in1=PD.t[:], op0=ALU.mult, op1=ALU.add),
                             reads=[xt.b, PD.b, r.b], writes=[r.b])
                    r = G.epilogue(E, i, xin, xout, add_fn, slot=sq)
                    yield
                    G.route_tile(R, i, r)
                    yield

        gens = [stream(0), stream(1)]
        live = [True, True]
        while any(live):
            for q in range(2):
                if live[q]:
                    try:
                        next(gens[q])
                    except StopIteration:
                        live[q] = False
    S.barrier()


def conformer_phase(G, xin, xout):
    S, W, sbt, pst = G.S, G.W, G.sbt, G.pst
    c = G.consts
    ident_b, ident_f, ones_f = c["ident_b"], c["ident_f"], c["ones_f"]
    with contextlib.ExitStack() as es:
        pw1 = sbt(es, "pw1", [128, 8, 2048], BF16)
        pw2 = sbt(es, "pw2", [128, 8, D], BF16)
        cfdw = sbt(es, "cfdw", [128, 248], F32)
        cfv = sbt(es, "cfv", [128, 24], F32)
        S.dma("sp", lambda e: e.dma_start(out=cfdw.t[:], in_=W["cf_dw"]), writes=[cfdw.b])
        S.dma("sp", lambda e: e.dma_start(out=cfv.t[:], in_=W["cf_vec"]), writes=[cfv.b])
        diag = sbt(es, "diag", [128, 248, 128], BF16)
        for q in range(248):
            S.op("dve", lambda e: e.tensor_scalar(out=diag.t[:, q, :], in0=ident_f.t[:], scalar1=cfdw.t[:, q:q + 1], scalar2=None, op0=ALU.mult),
                 reads=[ident_f.b, cfdw.b], writes=[diag.b])
        with contextlib.ExitStack() as es2:
            load_weight_bf16(G, es2, pw1, W["pw1"], 2048, "p1", engines=("act", "act"))
            load_weight_bf16(G, es2, pw2, W["pw2"], D, "p2", engines=("act", "act"))
            S.barrier()
        onesm = sbt(es, "onesm", [128, 128], F32)
        S.op("pool", lambda e: e.memset(onesm.t[:], 1.0 / 1024.0), writes=[onesm.b])
        X = Ctx()
        X.xl = sbt(es, "xl", [128, D], F32)
        X.xlb = sbt(es, "xlb", [128, D], BF16)
        X.xT = sbt(es, "xT", [128, 8, 512], BF16)
        PAB = pst(es, "PAB", [128, 1024], F32)
        PC = pst(es, "PC", [128, 512], F32)
        PD = pst(es, "PD", [128, 512], F32)
        PT = pst(es, "PT", [128, 1024], BF16)
        PE_ = pst(es, "PE", [128, 512], F32)
        PF = pst(es, "PF", [128, 512], F32)
        PG = pst(es, "PG", [128, 512], F32)
        X.PT = PT
        E = G.make_epi(es, 1, 0, nbuf=2)
        R = G.make_router(es, 1, PE_, PAB, nbuf=1)
        ub = [sbt(es, "ub%d" % i, [128, 542], BF16) for i in range(8)]
        cv = [sbt(es, "cv%d" % i, [128, 512], F32) for i in range(8)]
        sg = sbt(es, "sg", [128, 512], F32)
        sq = sbt(es, "sq", [128, 512], F32)
        m2 = sq
        rstd = sbt(es, "rstd", [128, 512], F32)
        nmr = sbt(es, "nmr", [128, 512], F32)
        nn = sg
        vT = sbt(es, "vT", [128, 8, 512], BF16)
        print("CF sbuf bytes remaining", G.nc.sbuf_bytes_remaining)
        for blk in range(8):
            if blk % 4 == 0:
                for cc in range(8):
                    S.op("pool", lambda e: e.memset(ub[cc].t[:, 0:30], 0.0), writes=[ub[cc].b])
            build_xT(G, X, xin, blk)
            xT = X.xT
            def ag(cc):
                ba, bg = (PE_, PF) if cc % 2 == 0 else (PC, PD)
                for (bank, c0) in ((ba, cc * 128), (bg, 1024 + cc * 128)):
                    for ko in range(8):
                        S.op("pe", lambda e: e.matmul(bank.t[:], lhsT=pw1.t[:, ko, c0:c0 + 128], rhs=xT.t[:, ko, :], start=(ko == 0), stop=(ko == 7)),
                             reads=[pw1.b, xT.b], writes=[bank.b], sig=(ko == 7))

            def rest(cc):
                ba, bg = (PE_, PF) if cc % 2 == 0 else (PC, PD)
                S.op("act", lambda e: e.activation(out=sg.t[:], in_=bg.t[:], func=AF.Sigmoid), reads=[bg.b], writes=[sg.b])
                S.op("dve", lambda e: e.tensor_tensor(out=ub[cc].t[:, 30:542], in0=ba.t[:], in1=sg.t[:], op=ALU.mult),
                     reads=[ba.b, sg.b], writes=[ub[cc].b])
                for j in range(31):
                    S.op("pe", lambda e: e.matmul(PG.t[:], lhsT=diag.t[:, j * 8 + cc, :], rhs=ub[cc].t[:, j:j + 512], start=(j == 0), stop=(j == 30)),
                         reads=[diag.b, ub[cc].b], writes=[PG.b], sig=(j == 30))
                S.op("act", lambda e: e.activation(out=cv[cc].t[:], in_=PG.t[:], func=AF.Identity, bias=cfv.t[:, cc:cc + 1], scale=1.0),
                     reads=[PG.b, cfv.b], writes=[cv[cc].b])
                S.op("act", lambda e: e.activation(out=sq.t[:], in_=cv[cc].t[:], func=AF.Square), reads=[cv[cc].b], writes=[sq.b])
                S.op("pe", lambda e: e.matmul(PAB.t[:, 0:512], lhsT=onesm.t[:], rhs=cv[cc].t[:], start=(cc == 0), stop=(cc == 7)),
                     reads=[onesm.b, cv[cc].b], writes=[PAB.b])
                S.op("pe", lambda e: e.matmul(PAB.t[:, 512:1024], lhsT=onesm.t[:], rhs=sq.t[:], start=(cc == 0), stop=(cc == 7)),
                     reads=[onesm.b, sq.b], writes=[PAB.b])
                S.op("dve", lambda e: e.tensor_copy(out=ub[cc].t[:, 0:30], in_=ub[cc].t[:, 512:542]), reads=[ub[cc].b], writes=[ub[cc].b])

            ag(0)
            for cc in range(8):
                if cc + 1 < 8:
                    ag(cc + 1)
                rest(cc)
            S.op("act", lambda e: e.activation(out=m2.t[:], in_=PAB.t[:, 0:512], func=AF.Square), reads=[PAB.b], writes=[m2.b])
            S.op("dve", lambda e: e.tensor_tensor(out=m2.t[:], in0=PAB.t[:, 512:1024], in1=m2.t[:], op=ALU.subtract), reads=[PAB.b, m2.b], writes=[m2.b])
            S.op("act", lambda e: e.activation(out=m2.t[:], in_=m2.t[:], func=AF.Sqrt, bias=LN_EPS), reads=[m2.b], writes=[m2.b])
            S.op("dve", lambda e: e.reciprocal(out=rstd.t[:], in_=m2.t[:]), reads=[m2.b], writes=[rstd.b])
            S.op("dve", lambda e: e.scalar_tensor_tensor(out=nmr.t[:], in0=PAB.t[:, 0:512], scalar=-1.0, in1=rstd.t[:], op0=ALU.mult, op1=ALU.mult),
                 reads=[PAB.b, rstd.b], writes=[nmr.b])
            for cc in range(8):
                S.op("dve", lambda e: e.tensor_tensor(out=nn.t[:], in0=cv[cc].t[:], in1=rstd.t[:], op=ALU.mult), reads=[cv[cc].b, rstd.b], writes=[nn.b])
                S.op("dve", lambda e: e.tensor_tensor(out=nn.t[:], in0=nn.t[:], in1=nmr.t[:], op=ALU.add), reads=[nn.b, nmr.b], writes=[nn.b])
                S.op("dve", lambda e: e.tensor_scalar(out=nn.t[:], in0=nn.t[:], scalar1=cfv.t[:, 8 + cc:9 + cc], scalar2=cfv.t[:, 16 + cc:17 + cc],
                                                      op0=ALU.mult, op1=ALU.add), reads=[nn.b, cfv.b], writes=[nn.b])
                S.op("act", lambda e: e.activation(out=vT.t[:, cc, :], in_=nn.t[:], func=AF.Silu), reads=[nn.b], writes=[vT.b])
            rts = {}

            def outproj_epi(tl):
                i = blk * 4 + tl
                tc0 = tl * 128
                for half, bank in enumerate((PC, PD)):
                    for cq in range(8):
                        S.op("pe", lambda e: e.matmul(bank.t[:], lhsT=vT.t[:, cq, tc0:tc0 + 128], rhs=pw2.t[:, cq, half * 512:(half + 1) * 512],
                                                      start=(cq == 0), stop=(cq == 7)), reads=[vT.b, pw2.b], writes=[bank.b], sig=(cq == 7))

                def add_fn(r, xt):
                    S.op("dve", lambda e: e.scalar_tensor_tensor(out=r.t[:, 0:512], in0=xt.t[:, 0:512], scalar=ALPHA, in1=PC.t[:], op0=ALU.mult, op1=ALU.add),
                         reads=[xt.b, PC.b], writes=[r.b])
                    S.op("dve", lambda e: e.scalar_tensor_tensor(out=r.t[:, 512:1024], in0=xt.t[:, 512:1024], scalar=ALPHA, in1=PD.t[:], op0=ALU.mult, op1=ALU.add),
                         reads=[xt.b, PD.b, r.b], writes=[r.b])
                rts[tl] = G.epilogue(E, i, xin, xout, add_fn)

            outproj_epi(0)
            for tl in range(4):
                if tl + 1 < 4:
                    outproj_epi(tl + 1)
                G.route_tile(R, blk * 4 + tl, rts[tl])
    S.barrier()


def host_weights(inp, stages):
    f = np.ascontiguousarray
    m = {}
    if "ab" in stages:
        m["ab_w_in"] = f(inp["ab_w_in"][0])
        m["ab_w_out"] = f(inp["ab_w_out"][0])
        m["ab_bif"] = f(np.concatenate([inp["ab_b_igate"][0], inp["ab_b_fgate"][0]]))
        m["ab_conv"] = f(inp["ab_conv_w"][0].reshape(3, 4, 128).transpose(2, 0, 1).reshape(128, 12))
        m["ab_hg"] = f(inp["ab_head_gain"][0])
    if "cf" in stages:
        m["cf_w_pw1"] = f(inp["cf_w_pw1"][0])
        m["cf_w_pw2"] = f(inp["cf_w_pw2"][0])
        m["cf_dw"] = f(inp["cf_w_dw"][0].reshape(31, 8, 128).transpose(2, 0, 1).reshape(128, 248))
        vec = np.stack([inp["cf_b_dw"][0], inp["cf_ln_g"][0], inp["cf_ln_b"][0]])
        m["cf_vec"] = f(vec.reshape(3, 8, 128).transpose(2, 0, 1).reshape(128, 24))
    m["router_w"] = f(inp["router_w"])
    m["router_b"] = f(inp["router_b"])
    m["exp_w_up"] = f(inp["exp_w_up"])
    m["exp_w_down"] = f(inp["exp_w_down"])
    m["bup"] = f(inp["exp_b_up"].reshape(2, NE, 16, 128).transpose(3, 0, 1, 2).reshape(128, 2 * NE * 16))
    m["exp_b_down"] = f(inp["exp_b_down"])
    m["post_ln_g"] = f(inp["post_ln_g"])
    m["post_ln_b"] = f(inp["post_ln_b"])
    return m


def kernel(**inputs):
    inp = {k: np.asarray(v) for k, v in inputs.items()}
    stages = ("ab", "moe0", "cf", "moe1")
    nc = build_program(stages)
    wm = host_weights(inp, stages)
    x = inp["x"].reshape(8, NTOK, D)
    in_maps = []
    for c in range(8):
        m = dict(wm)
        m["x"] = np.ascontiguousarray(x[c])
        in_maps.append(m)
    res = run_bass_kernel_spmd(nc, in_maps, core_ids=list(range(8)))
    out = np.stack([np.asarray(r["out"]) for r in res.results], axis=0)
    return out.reshape(16, SEQ, D).astype(np.float32)
```
